# Optimizing a Trainium2 kernel written in Bass

```python
import math
import jax
import jax.numpy as jnp
from jax import lax
import numpy as np

D_MODEL = 1024
BATCH = 4
SEQ = 4096
DEPTH = 1

GRID_W = 64
CTX_LEN = 256
H_RET = 4
RET_DK = 128
RET_DV = 128
RET_W = H_RET * RET_DV
H_GDN = 4
GDN_DK = 128
GDN_DV = 128
GDN_W = H_GDN * GDN_DV
SHORT_CONV = 3
CHUNK = 128
ROPE_BASE = 10000.0
N_STATE = 2 * RET_W + 2 * GDN_W + 4 * H_GDN
N_QUERY = 2 * RET_W + 2 * GDN_W + 2 * D_MODEL
N_IN = N_STATE + N_QUERY
N_EXPERTS = 64
TOP_K = 8
N_GROUPS = 8
TOPK_GROUPS = 4
D_EXPERT = 256
D_SHARED = 256
ROUTED_SCALE = 2.5
EXPERT_BLOCK = 128
EPS = 1e-6

kernel_name = 'hybrid_retention_gdn_moe_prefix_dit'


def _rmsnorm(t, g):
    tf = t.astype(jnp.float32)
    y = tf * lax.rsqrt(jnp.mean(tf * tf, axis=-1, keepdims=True) + EPS)
    return (y * g.astype(jnp.float32)).astype(t.dtype)


def _l2norm(t):
    return t * lax.rsqrt(jnp.sum(t * t, axis=-1, keepdims=True) + EPS)


def _group_norm(t):
    tc = t - jnp.mean(t, axis=-1, keepdims=True)
    return tc * lax.rsqrt(jnp.mean(tc * tc, axis=-1, keepdims=True) + EPS)


def _head_rms(t):
    return t * lax.rsqrt(jnp.mean(t * t, axis=-1, keepdims=True) + EPS)


def _split_heads(t, n_heads):
    b, l, w = t.shape
    return t.reshape(b, l, n_heads, w // n_heads).transpose(0, 2, 1, 3)


def _merge_heads(t):
    b, h, l, d = t.shape
    return t.transpose(0, 2, 1, 3).reshape(b, l, h * d)


def _dwconv(t, w):
    return lax.conv_general_dilated(t, w[:, None, :].astype(t.dtype), (1,), 'SAME',
                                    dimension_numbers=('NWC', 'WIO', 'NWC'),
                                    feature_group_count=t.shape[-1])


def _modulation(cvec, w, b, n):
    m = jax.nn.silu(cvec) @ w[:, : n * D_MODEL] + b[: n * D_MODEL]
    return jnp.split(m, n, axis=-1)


def _axial_rope(n):
    rows = n // GRID_W
    pos_r = jnp.repeat(jnp.arange(rows, dtype=jnp.float32), GRID_W)
    pos_c = jnp.tile(jnp.arange(GRID_W, dtype=jnp.float32), rows)
    n_freq = RET_DK // 4
    inv = ROPE_BASE ** (-jnp.arange(n_freq, dtype=jnp.float32) / n_freq)
    ang = jnp.concatenate([pos_r[:, None] * inv, pos_c[:, None] * inv], axis=-1)
    return jnp.cos(ang), jnp.sin(ang)


def _apply_rope(t, cos, sin):
    half = RET_DK // 2
    t1, t2 = t[..., :half], t[..., half:]
    return jnp.concatenate([t1 * cos - t2 * sin, t1 * sin + t2 * cos], axis=-1)


def _retention_scan(q, k, v, log_g, s0):
    b, h, l, dk = k.shape
    n = l // CHUNK
    kc = k.reshape(b, h, n, CHUNK, dk)
    vc = v.reshape(b, h, n, CHUNK, -1)
    lg = log_g.astype(jnp.float32)[:, None]
    idx = jnp.arange(CHUNK, dtype=jnp.float32)
    kv = jnp.einsum('bhncd,hc,bhnce->bhnde', kc, jnp.exp(lg * (CHUNK - 1 - idx)), vc)
    chunk_decay = jnp.exp(lg[:, 0] * CHUNK)[None, :, None, None]

    def step(s, kv_n):
        return chunk_decay * s + kv_n, (None if q is None else s)

    s_fin, s_prev = lax.scan(step, s0, jnp.moveaxis(kv, 2, 0))
    if q is None:
        return s_fin
    qc = q.reshape(b, h, n, CHUNK, dk)
    dist = idx[:, None] - idx[None, :]
    dmat = jnp.where(dist >= 0, jnp.exp(lg[:, :, None] * jnp.maximum(dist, 0.0)), 0.0)
    scores = jnp.einsum('bhnid,bhnjd->bhnij', qc, kc) * dmat[None, :, None]
    o = jnp.einsum('bhnij,bhnje->bhnie', scores, vc)
    o = o + jnp.einsum('bhnid,nbhde->bhnie', qc, s_prev) * jnp.exp(lg * (idx + 1.0))[None, :, None, :, None]
    return o.reshape(b, h, l, -1), s_fin


def _gdn_scan(q, k, v, log_a, beta, s0):
    b, h, l, dk = k.shape
    dv = v.shape[-1]
    n = l // CHUNK
    kc = k.reshape(b, h, n, CHUNK, dk)
    vc = v.reshape(b, h, n, CHUNK, dv)
    bc = beta.reshape(b, h, n, CHUNK, 1)
    g = jnp.cumsum(log_a.reshape(b, h, n, CHUNK), axis=-1)
    idx = jnp.arange(CHUNK)
    strict = idx[:, None] > idx[None, :]
    rel = g[..., :, None] - g[..., None, :]
    a = jnp.einsum('bhnid,bhnjd->bhnij', kc, kc) * bc * jnp.where(strict, jnp.exp(jnp.where(strict, rel, 0.0)), 0.0)
    rhs = jnp.concatenate([bc * vc, bc * jnp.exp(g)[..., None] * kc], axis=-1)
    sol = lax.linalg.triangular_solve(a + jnp.eye(CHUNK, dtype=a.dtype), rhs,
                                      left_side=True, lower=True, unit_diagonal=True)
    u, wk = sol[..., :dv], sol[..., dv:]
    g_last = g[..., -1]
    k_tail = kc * jnp.exp(g_last[..., None] - g)[..., None]

    def step(s, inp):
        u_n, w_n, kt_n, gl_n = inp
        v_new = u_n - jnp.einsum('bhck,bhkv->bhcv', w_n, s)
        s_next = jnp.exp(gl_n)[..., None, None] * s + jnp.einsum('bhck,bhcv->bhkv', kt_n, v_new)
        return s_next, (None if q is None else (v_new, s))

    xs = tuple(jnp.moveaxis(t, 2, 0) for t in (u, wk, k_tail, g_last))
    s_fin, ys = lax.scan(step, s0, xs)
    if q is None:
        return s_fin
    v_new, s_prev = ys
    qc = q.reshape(b, h, n, CHUNK, dk)
    incl = idx[:, None] >= idx[None, :]
    qk = jnp.einsum('bhnid,bhnjd->bhnij', qc, kc) * jnp.where(incl, jnp.exp(jnp.where(incl, rel, 0.0)), 0.0)
    o = jnp.einsum('bhnij,nbhjv->bhniv', qk, v_new)
    o = o + jnp.einsum('bhnik,nbhkv->bhniv', qc * jnp.exp(g)[..., None], s_prev)
    return o.reshape(b, h, l, -1), s_fin


def _state_features(h, w_in_l, conv_l, a_log_l, dt_bias_l):
    p = h @ w_in_l[:, :N_STATE]
    rk = _split_heads(p[..., :RET_W].astype(jnp.float32), H_RET) * RET_DK ** -0.5
    rv = _split_heads(p[..., RET_W:2 * RET_W].astype(jnp.float32), H_RET)
    gkv = jax.nn.silu(_dwconv(p[..., 2 * RET_W:2 * RET_W + 2 * GDN_W], conv_l[:, GDN_W:])).astype(jnp.float32)
    gk = _l2norm(_split_heads(gkv[..., :GDN_W], H_GDN))
    gv = _split_heads(gkv[..., GDN_W:], H_GDN)
    b, l = h.shape[:2]
    gab = p[..., 2 * RET_W + 2 * GDN_W:].astype(jnp.float32).reshape(b, l, 2, 2, H_GDN).transpose(2, 3, 0, 4, 1)
    log_a = -jnp.exp(a_log_l.astype(jnp.float32))[:, None, :, None] * jax.nn.softplus(
        gab[0] + dt_bias_l.astype(jnp.float32)[:, None, :, None])
    beta = jax.nn.sigmoid(gab[1])
    return rk, rv, gk, gv, log_a, beta


def _query_features(h, w_in_l, conv_l):
    p = h @ w_in_l[:, N_STATE:]
    rq = _split_heads(p[..., :RET_W].astype(jnp.float32), H_RET)
    rg = p[..., RET_W:2 * RET_W]
    gq = jax.nn.silu(_dwconv(p[..., 2 * RET_W:2 * RET_W + GDN_W], conv_l[:, :GDN_W])).astype(jnp.float32)
    gq = _l2norm(_split_heads(gq, H_GDN)) * GDN_DK ** -0.5
    gz = p[..., 2 * RET_W + GDN_W:2 * RET_W + 2 * GDN_W]
    gates = jax.nn.sigmoid(p[..., 2 * RET_W + 2 * GDN_W:].astype(jnp.float32))
    return rq, rg, gq, gz, gates


def _recurrent_mixers(sf, rq, gq, init, ret_log_decay_l):
    rk, rv, gk, gv, log_a, beta = sf
    flip = lambda t: jnp.flip(t, axis=2)
    r_f = _retention_scan(rq, rk, rv, ret_log_decay_l[0], init[0])
    r_b = _retention_scan(None if rq is None else flip(rq), flip(rk), flip(rv), ret_log_decay_l[1], init[1])
    g_f = _gdn_scan(gq, gk, gv, log_a[0], beta[0], init[2])
    g_b = _gdn_scan(None if gq is None else flip(gq), flip(gk), flip(gv), flip(log_a[1]), flip(beta[1]), init[3])
    if rq is None:
        return (r_f, r_b, g_f, g_b)
    return r_f[0] + flip(r_b[0]), g_f[0] + flip(g_b[0]), (r_f[1], r_b[1], g_f[1], g_b[1])


def _mixer_out(ret_o, gdn_o, rg, gz, gates, ret_gn_w_l, gdn_norm_w_l, w_ret_out_l, w_gdn_out_l, w_o_l):
    dt = w_o_l.dtype
    ret_y = _merge_heads(_group_norm(ret_o)) * ret_gn_w_l.astype(jnp.float32) * jax.nn.silu(rg.astype(jnp.float32))
    gdn_y = _merge_heads(_head_rms(gdn_o)) * gdn_norm_w_l.astype(jnp.float32) * jax.nn.silu(gz.astype(jnp.float32))
    merged = (gates[..., :D_MODEL] * (ret_y.astype(dt) @ w_ret_out_l)
              + gates[..., D_MODEL:] * (gdn_y.astype(dt) @ w_gdn_out_l))
    return merged.astype(dt) @ w_o_l


def _moe(h, w_router_l, bias_l, w_gate_l, w_up_l, w_down_l, w_sh_gate_l, w_sh_up_l, w_sh_down_l):
    b, l, d = h.shape
    t = b * l
    xt = h.reshape(t, d)
    scores = jax.nn.sigmoid((xt @ w_router_l).astype(jnp.float32))
    biased = scores + bias_l.astype(jnp.float32)
    per_group = N_EXPERTS // N_GROUPS
    grp_score = lax.top_k(biased.reshape(t, N_GROUPS, per_group), 2)[0].sum(-1)
    grp_keep = jax.nn.one_hot(lax.top_k(grp_score, TOPK_GROUPS)[1], N_GROUPS, dtype=jnp.float32).sum(1) > 0
    cand = jnp.where(jnp.repeat(grp_keep, per_group, axis=1), biased, -jnp.inf)
    sel = lax.top_k(cand, TOP_K)[1]
    wts = jnp.take_along_axis(scores, sel, axis=1)
    wts = wts / jnp.sum(wts, axis=-1, keepdims=True) * ROUTED_SCALE
    n_assign = t * TOP_K
    flat_e = sel.reshape(-1)
    order = jnp.argsort(flat_e)
    e_sorted = flat_e[order]
    sizes = jnp.bincount(flat_e, length=N_EXPERTS).astype(jnp.int32)
    padded = (sizes + EXPERT_BLOCK - 1) // EXPERT_BLOCK * EXPERT_BLOCK
    pad_end = jnp.cumsum(padded)
    slot = (pad_end - padded)[e_sorted] + jnp.arange(n_assign, dtype=jnp.int32) - (jnp.cumsum(sizes) - sizes)[e_sorted]
    n_blocks = -(-n_assign // EXPERT_BLOCK) + N_EXPERTS
    n_slots = n_blocks * EXPERT_BLOCK
    slot_tok = jnp.full((n_slots,), t, jnp.int32).at[slot].set((order // TOP_K).astype(jnp.int32))
    slot_w = jnp.zeros((n_slots,), jnp.float32).at[slot].set(wts.reshape(-1)[order])
    block_e = jnp.minimum(jnp.searchsorted(pad_end, jnp.arange(n_blocks, dtype=jnp.int32) * EXPERT_BLOCK, side='right'),
                          N_EXPERTS - 1)
    xb = jnp.concatenate([xt, jnp.zeros((1, d), xt.dtype)])[slot_tok].reshape(n_blocks, EXPERT_BLOCK, d)

    def expert_block(args):
        xe, e = args
        return (jax.nn.silu(xe @ w_gate_l[e]) * (xe @ w_up_l[e])) @ w_down_l[e]

    yb = lax.map(expert_block, (xb, block_e)).reshape(n_slots, d)
    routed = jax.ops.segment_sum(yb * slot_w[:, None].astype(yb.dtype), slot_tok, num_segments=t + 1)[:t]
    shared = (jax.nn.silu(xt @ w_sh_gate_l) * (xt @ w_sh_up_l)) @ w_sh_down_l
    return (routed + shared).reshape(b, l, d)


def setup_inputs(seed: int = 0) -> dict:
    key = jax.random.key(seed)
    ks = jax.random.split(key, 32)
    f32 = jnp.float32
    D = D_MODEL

    def nrm(k, shape, fan_in, scale=1.0):
        return jax.random.normal(k, shape, f32) * (scale * fan_in ** -0.5)

    def gain(k, shape):
        return 1.0 + 0.05 * jax.random.normal(k, shape, f32)

    base_decay = jnp.log1p(-jnp.power(2.0, -5.0 - jnp.arange(H_RET, dtype=f32)))
    dt = jnp.exp(jax.random.uniform(ks[12], (DEPTH, 2, H_GDN), f32, math.log(1e-3), math.log(1e-1)))
    return {
        'x': jax.random.normal(ks[0], (BATCH, SEQ, D), f32),
        'c': jax.random.normal(ks[1], (BATCH, D), f32),
        'ctx': jax.random.normal(ks[2], (BATCH, CTX_LEN, D), f32),
        'c_ctx': jax.random.normal(ks[3], (D,), f32),
        'w_mod': nrm(ks[4], (DEPTH, D, 6 * D), D, 0.3),
        'b_mod': 0.02 * jax.random.normal(ks[5], (DEPTH, 6 * D), f32),
        'norm_mix_pre': gain(ks[6], (DEPTH, D)),
        'norm_mix_post': gain(ks[7], (DEPTH, D)),
        'norm_ffn_pre': gain(ks[8], (DEPTH, D)),
        'norm_ffn_post': gain(ks[9], (DEPTH, D)),
        'w_in': nrm(ks[10], (DEPTH, D, N_IN), D),
        'gdn_conv': nrm(ks[11], (DEPTH, SHORT_CONV, 3 * GDN_W), SHORT_CONV),
        'ret_log_decay': base_decay * (1.0 + 0.05 * jax.random.normal(ks[13], (DEPTH, 2, H_RET), f32)),
        'gdn_a_log': jnp.log(jax.random.uniform(ks[14], (DEPTH, 2, H_GDN), f32, 1.0, 16.0)),
        'gdn_dt_bias': dt + jnp.log(-jnp.expm1(-dt)),
        'ret_gn_w': gain(ks[15], (DEPTH, RET_W)),
        'gdn_norm_w': gain(ks[16], (DEPTH, GDN_W)),
        'w_ret_out': nrm(ks[17], (DEPTH, RET_W, D), RET_W),
        'w_gdn_out': nrm(ks[18], (DEPTH, GDN_W, D), GDN_W),
        'w_o': nrm(ks[19], (DEPTH, D, D), D),
        'w_router': nrm(ks[20], (DEPTH, D, N_EXPERTS), D),
        'router_bias': 0.01 * jax.random.normal(ks[21], (DEPTH, N_EXPERTS), f32),
        'w_gate': nrm(ks[22], (DEPTH, N_EXPERTS, D, D_EXPERT), D),
        'w_up': nrm(ks[23], (DEPTH, N_EXPERTS, D, D_EXPERT), D),
        'w_down': nrm(ks[24], (DEPTH, N_EXPERTS, D_EXPERT, D), D_EXPERT),
        'w_sh_gate': nrm(ks[25], (DEPTH, D, D_SHARED), D),
        'w_sh_up': nrm(ks[26], (DEPTH, D, D_SHARED), D),
        'w_sh_down': nrm(ks[27], (DEPTH, D_SHARED, D), D_SHARED),
    }


def reference(x, c, ctx, c_ctx, w_mod, b_mod, norm_mix_pre, norm_mix_post, norm_ffn_pre, norm_ffn_post,
              w_in, gdn_conv, ret_log_decay, gdn_a_log, gdn_dt_bias, ret_gn_w, gdn_norm_w,
              w_ret_out, w_gdn_out, w_o, w_router, router_bias, w_gate, w_up, w_down,
              w_sh_gate, w_sh_up, w_sh_down):
    b, n, _ = x.shape
    cos, sin = _axial_rope(n)
    zr = jnp.zeros((b, H_RET, RET_DK, RET_DV), jnp.float32)
    zg = jnp.zeros((b, H_GDN, GDN_DK, GDN_DV), jnp.float32)
    zero_state = (zr, zr, zg, zg)
    for i in range(DEPTH):
        last = i == DEPTH - 1
        out_p = (ret_gn_w[i], gdn_norm_w[i], w_ret_out[i], w_gdn_out[i], w_o[i])
        moe_p = (w_router[i], router_bias[i], w_gate[i], w_up[i], w_down[i], w_sh_gate[i], w_sh_up[i], w_sh_down[i])
        sh1, sc1, g1, sh2, sc2, g2 = [m[:, None, :] for m in _modulation(c, w_mod[i], b_mod[i], 6)]
        cm = _modulation(c_ctx, w_mod[i], b_mod[i], 2 if last else 6)
        hc = _rmsnorm(ctx, norm_mix_pre[i]) * (1.0 + cm[1]) + cm[0]
        sf_c = _state_features(hc, w_in[i], gdn_conv[i], gdn_a_log[i], gdn_dt_bias[i])
        if last:
            init = _recurrent_mixers(sf_c, None, None, zero_state, ret_log_decay[i])
        else:
            rq_c, rg_c, gq_c, gz_c, gates_c = _query_features(hc, w_in[i], gdn_conv[i])
            ret_c, gdn_c, init = _recurrent_mixers(sf_c, rq_c, gq_c, zero_state, ret_log_decay[i])
            ctx = ctx + cm[2] * _rmsnorm(_mixer_out(ret_c, gdn_c, rg_c, gz_c, gates_c, *out_p), norm_mix_post[i])
            h2c = _rmsnorm(ctx, norm_ffn_pre[i]) * (1.0 + cm[4]) + cm[3]
            ctx = ctx + cm[5] * _rmsnorm(_moe(h2c, *moe_p), norm_ffn_post[i])
        hx = _rmsnorm(x, norm_mix_pre[i]) * (1.0 + sc1) + sh1
        rk, rv, gk, gv, log_a, beta = _state_features(hx, w_in[i], gdn_conv[i], gdn_a_log[i], gdn_dt_bias[i])
        rq, rg, gq, gz, gates = _query_features(hx, w_in[i], gdn_conv[i])
        sf_x = (_apply_rope(rk, cos, sin), rv, gk, gv, log_a, beta)
        ret_x, gdn_x, _ = _recurrent_mixers(sf_x, _apply_rope(rq, cos, sin), gq, init, ret_log_decay[i])
        x = x + g1 * _rmsnorm(_mixer_out(ret_x, gdn_x, rg, gz, gates, *out_p), norm_mix_post[i])
        h2 = _rmsnorm(x, norm_ffn_pre[i]) * (1.0 + sc2) + sh2
        x = x + g2 * _rmsnorm(_moe(h2, *moe_p), norm_ffn_post[i])
    return x
```

```python
from contextlib import ExitStack
import numpy as np
import concourse.bass as bass
import concourse.mybir as mybir
from concourse.bass_utils import run_bass_kernel_spmd

F32 = mybir.dt.float32
BF16 = mybir.dt.bfloat16
AF = mybir.ActivationFunctionType
ALU = mybir.AluOpType

D = 1024
NTOK = 2048
NLAT = 4096
NCTX = 256
NS = NLAT + NCTX
NT = NS // 128
EPS = 1e-6
NE = 65
QSC = float(128 ** -0.5)


class T:
    __slots__ = ("name", "w", "r")

    def __init__(self, name=""):
        self.name = name
        self.w = None
        self.r = {}


class KB:
    def __init__(self, nc):
        self.nc = nc
        self.eng = {"pe": nc.tensor, "act": nc.scalar, "dve": nc.vector, "pool": nc.gpsimd, "sp": nc.sync}
        self.sem = {k: nc.alloc_semaphore("sem_" + k) for k in ["pe", "act", "dve", "pool"]}
        self.cnt = {k: 0 for k in self.sem}
        self.waited = {k: {} for k in self.eng}
        self.ring = {}
        for q in ("sp", "pool"):
            self.ring[q] = dict(sems=[nc.alloc_semaphore(f"dq_{q}_{i}") for i in range(12)], cnt=[0] * 12, idx=0)

    def _wait(self, e, tok):
        semname, sem, val, owner = tok
        if owner == e and e == "pe":
            return
        w = self.waited[e]
        if w.get(semname, 0) >= val:
            return
        self.eng[e].wait_ge(sem, val)
        w[semname] = val

    def _deps(self, e, reads, writes):
        for t in reads:
            if t.w is not None:
                self._wait(e, t.w)
        for t in writes:
            if t.w is not None:
                self._wait(e, t.w)
            for tok in t.r.values():
                self._wait(e, tok)

    def _record(self, tok, reads, writes):
        for t in reads:
            t.r[tok[0]] = tok
        for t in writes:
            t.w = tok
            t.r = {}

    def op(self, e, fn, reads=(), writes=()):
        self._deps(e, reads, writes)
        ins = fn(self.eng[e])
        self.cnt[e] += 1
        ins.then_inc(self.sem[e], 1)
        tok = ("sem_" + e, self.sem[e], self.cnt[e], e)
        self._record(tok, reads, writes)
        return tok

    def dma(self, q, out, in_, reads=(), writes=(), **kw):
        rg = self.ring[q]
        i = rg["idx"] % len(rg["sems"])
        rg["idx"] += 1
        sem = rg["sems"][i]
        name = f"dq_{q}_{i}"
        if rg["cnt"][i] > 0:
            self._wait(q, (name, sem, rg["cnt"][i] * 16, "dma"))
        self._deps(q, reads, writes)
        self.eng[q].dma_start(out=out, in_=in_, **kw).then_inc(sem, 16)
        rg["cnt"][i] += 1
        tok = (name, sem, rg["cnt"][i] * 16, "dma")
        self._record(tok, reads, writes)
        return tok

    def all_tokens(self):
        toks = [("sem_" + e, self.sem[e], self.cnt[e], e) for e in self.sem if self.cnt[e] > 0]
        for q, rg in self.ring.items():
            for i, c in enumerate(rg["cnt"]):
                if c > 0:
                    toks.append((f"dq_{q}_{i}", rg["sems"][i], c * 16, "dma"))
        return toks

    def barrier(self):
        toks = self.all_tokens()
        for e in self.eng:
            for tok in toks:
                semname, sem, val, owner = tok
                w = self.waited[e]
                if w.get(semname, 0) >= val:
                    continue
                self.eng[e].wait_ge(sem, val)
                w[semname] = val

    def mm(self, out, lhsT, rhs, start, stop, reads, writes):
        return self.op("pe", lambda e: e.matmul(out, lhsT=lhsT, rhs=rhs, start=start, stop=stop), reads, writes)

    def tr(self, out, in_, ident, reads, writes):
        return self.op("pe", lambda e: e.transpose(out, in_, ident), reads, writes)

    def act(self, out, in_, func, reads, writes, **kw):
        return self.op("act", lambda e: e.activation(out=out, in_=in_, func=func, **kw), reads, writes)

    def ts(self, e, out, in0, s1, s2, op0, op1, reads, writes):
        return self.op(e, lambda g: g.tensor_scalar(out=out, in0=in0, scalar1=s1, scalar2=s2, op0=op0, op1=op1),
                       reads, writes)

    def tt(self, e, out, in0, in1, op, reads, writes):
        return self.op(e, lambda g: g.tensor_tensor(out=out, in0=in0, in1=in1, op=op), reads, writes)

    def stt(self, out, in0, scalar, in1, op0, op1, reads, writes):
        return self.op("dve", lambda g: g.scalar_tensor_tensor(out=out, in0=in0, scalar=scalar, in1=in1,
                                                                op0=op0, op1=op1), reads, writes)

    def cp(self, e, out, in_, reads, writes):
        if e == "act":
            return self.act(out, in_, AF.Copy, reads, writes)
        return self.op(e, lambda g: g.tensor_copy(out=out, in_=in_), reads, writes)

    def memset(self, e, ap, val, writes):
        return self.op(e, lambda g: g.memset(ap, val), (), writes)


def build(dbg=(), stop_after=None, noq=False):
    nc = bass.Bass("TRN2", target_bir_lowering=False)
    k = KB(nc)

    def din(name, shape, dt=F32):
        return nc.dram_tensor(name, list(shape), dt, kind="ExternalInput").ap()

    xl = din("xl", [NLAT, D])
    ctxl = din("ctxl", [NCTX, D])
    cvec = din("cvec", [128, 8, 2])
    w_mod = din("w_mod", [D, 6 * D])
    bmod_d = din("bmod", [128, 48])
    gains_d = din("gains", [128, 4, 8])
    cst_d = din("cst", [128, 11, 128])
    cstb_d = din("cstb", [128, 2, 128])
    rope_d = din("rope", [128, 4, NS])
    w_in_h = din("w_in_h", [4, D, 1280])
    w_gab_d = din("w_gab", [D, 16])
    w_gates_d = din("w_gates", [D, 2048])
    convw_d = din("convw", [128, 4, 3, 3])
    hp_d = din("hp", [128, 24])
    nw_d = din("nw", [128, 2, 4])
    w_ro_d = din("w_ro", [512, D])
    w_go_d = din("w_go", [512, D])
    w_o_d = din("w_o", [D, D])
    w_r_d = din("w_r", [D, 64])
    rb_d = din("rb", [128, 64])
    ne_decl = NE if stop_after is None else 1
    w_gu_d = din("w_gu", [ne_decl, D, 512])
    w_dn_d = din("w_dn", [ne_decl, 256, D])
    out_d = nc.dram_tensor("out", [NTOK, D], F32, kind="ExternalOutput").ap()
    hx_d = nc.dram_tensor("hx_d", [128, 8, NS], BF16, kind="Internal").ap()
    y_d = nc.dram_tensor("y_d", [8, 128, NTOK], BF16, kind="Internal").ap()
    x1_d = nc.dram_tensor("x1_d", [NTOK, D], F32, kind="Internal").ap()
    out_toks = []

    def dump(name, ap_sb, tls, shape, dt=F32):
        if name not in dbg:
            return
        d = nc.dram_tensor("dbg_" + name, list(shape), dt, kind="ExternalOutput").ap()
        out_toks.append(k.dma("sp", d, ap_sb, reads=tls))

    PS = [nc.alloc_psum_tensor(f"ps{i}", [128, 512], F32) for i in range(8)]
    PSB = [p[:].bitcast(BF16) for p in PS]
    PT = [T(f"ps{i}") for i in range(8)]

    def P(name, shape, dt=F32):
        return nc.alloc_sbuf_tensor("s_" + name, list(shape), dt)

    cst = P("cst", [128, 11, 128]); t_cst = T()
    k.dma("sp", cst[:], cst_d, writes=[t_cst])
    cstb = P("cstb", [128, 2, 128], BF16); t_cstb = T()
    k.dma("pool", cstb[:], cstb_d, writes=[t_cstb])
    ident = cst[:, 0, :]
    ones = cst[:, 1, :]
    L_st, L_in, U_st, U_in = cst[:, 2, :], cst[:, 3, :], cst[:, 4, :], cst[:, 5, :]
    identb = cstb[:, 0, :]
    onesb = cstb[:, 1, :]
    hp = P("hp", [128, 24]); t_hp = T()
    k.dma("sp", hp[:], hp_d, writes=[t_hp])
    nw = P("nw", [128, 2, 4]); t_nw = T()
    k.dma("sp", nw[:], nw_d, writes=[t_nw])
    convw = P("convw", [128, 4, 3, 3]); t_convw = T()
    k.dma("sp", convw[:], convw_d, writes=[t_convw])
    mv = P("mv", [128, 8, 8]); t_mv = T()
    gbc = [P(f"gbc{i}", [128, D]) for i in range(2)]
    t_gbc = [T() for _ in range(2)]
    ss = P("ss", [128, 64]); t_ss = T()
    rs = P("rs", [128, 64]); t_rs = T()
    junk = P("junk", [128, D], BF16); t_junk = T()
    g_la = P("g_la", [128, NT, 8]); g_beta = P("g_beta", [128, NT, 8]); g_nbeta = P("g_nbeta", [128, NT, 8])
    g_g = P("g_g", [128, NT, 8]); g_beg = P("g_beg", [128, NT, 8]); g_egl = P("g_egl", [128, NT, 8])
    g_kts = P("g_kts", [128, NT, 8])
    g_ng = P("g_ng", [128, NT, 8])
    t_gs = T()

    def sigmoid_to(out_ap, in_ap, reads, t_out):
        k.act(out_ap, in_ap, AF.Exp, reads, [t_out], scale=-1.0)
        k.act(out_ap, out_ap, AF.Ln, [t_out], [t_out], bias=1.0)
        k.act(out_ap, out_ap, AF.Exp, [t_out], [t_out], scale=-1.0)

    def rstd_to(out_ap, in_ap, scale, reads, t_out, post_scale=None):
        k.act(out_ap, in_ap, AF.Ln, reads, [t_out], scale=scale, bias=EPS)
        if post_scale is None:
            k.act(out_ap, out_ap, AF.Exp, [t_out], [t_out], scale=-0.5)
        else:
            k.act(out_ap, out_ap, AF.Exp, [t_out], [t_out], scale=-0.5, bias=float(np.log(post_scale)))

    def norm_transpose(es_xn, src_ap, tl_src, it, Acol, Bcol, dst_ap, t_dst, pbank, f32out=None, stage=0):
        xn_, t_xn_ = es_xn
        if stage != 2:
            k.act(junk[:], src_ap, AF.Square, [tl_src], [t_junk, t_ss], accum_out=ss[:, it:it + 1])
            rstd_to(rs[:, it:it + 1], ss[:, it:it + 1], 1.0 / D, [t_ss], t_rs)
            k.ts("dve", xn_[:], src_ap, rs[:, it:it + 1], None, ALU.mult, ALU.bypass, [tl_src, t_rs], [t_xn_])
            for kc in range(8):
                pb = pbank + kc // 4
                k.tr(PS[pb][:, (kc % 4) * 128:(kc % 4 + 1) * 128], xn_[:, kc * 128:(kc + 1) * 128], ident,
                     [t_xn_, t_cst], [PT[pb]])
        if stage != 1:
            for kc in range(8):
                pb = pbank + kc // 4
                o = dst_ap[:, kc, :] if f32out is None else f32out[0][:, kc, :]
                k.act(o, PS[pb][:, (kc % 4) * 128:(kc % 4 + 1) * 128], AF.Identity,
                      [PT[pb], t_mv], [t_dst if f32out is None else f32out[1]],
                      scale=mv[:, Acol, kc:kc + 1], bias=mv[:, Bcol, kc:kc + 1])
            if f32out is not None:
                k.cp("act", dst_ap, f32out[0][:], [f32out[1]], [t_dst])
        return
        k.act(junk[:], src_ap, AF.Square, [tl_src], [t_junk, t_ss], accum_out=ss[:, it:it + 1])
        rstd_to(rs[:, it:it + 1], ss[:, it:it + 1], 1.0 / D, [t_ss], t_rs)
        k.ts("dve", xn_[:], src_ap, rs[:, it:it + 1], None, ALU.mult, ALU.bypass, [tl_src, t_rs], [t_xn_])
        for kc in range(8):
            pb = pbank + kc // 4
            k.tr(PS[pb][:, (kc % 4) * 128:(kc % 4 + 1) * 128], xn_[:, kc * 128:(kc + 1) * 128], ident,
                 [t_xn_, t_cst], [PT[pb]])
        for kc in range(8):
            pb = pbank + kc // 4
            o = dst_ap[:, kc, :] if f32out is None else f32out[0][:, kc, :]
            k.act(o, PS[pb][:, (kc % 4) * 128:(kc % 4 + 1) * 128], AF.Identity,
                  [PT[pb], t_mv], [t_dst if f32out is None else f32out[1]],
                  scale=mv[:, Acol, kc:kc + 1], bias=mv[:, Bcol, kc:kc + 1])
        if f32out is not None:
            k.cp("act", dst_ap, f32out[0][:], [f32out[1]], [t_dst])

    with ExitStack() as es:
        A = lambda name, shape, dt=F32: es.enter_context(nc.sbuf_tensor("s_" + name, list(shape), dt))
        cv = A("cv", [128, 8, 2]); t_cv = T()
        k.dma("sp", cv[:], cvec, writes=[t_cv])
        bmod = A("bmod", [128, 48]); t_bmod = T()
        k.dma("sp", bmod[:], bmod_d, writes=[t_bmod])
        gains = A("gains", [128, 4, 8]); t_gains = T()
        k.dma("sp", gains[:], gains_d, writes=[t_gains])
        sg = A("sg", [128, 8, 2]); t_sg = T()
        scb = A("scb", [128, 8, 2], BF16); t_scb = T()
        sigmoid_to(sg[:], cv[:], [t_cv], t_sg)
        k.tt("dve", scb[:], sg[:], cv[:], ALU.mult, [t_sg, t_cv], [t_scb])
        wmf = [A(f"wmf{i}", [128, 8, 512]) for i in range(2)]
        t_wmf = [T() for _ in range(2)]
        wmb = [A(f"wmb{i}", [128, 8, 512], BF16) for i in range(2)]
        t_wmb = [T() for _ in range(2)]
        wm_v = w_mod.rearrange("(k p) n -> p k n", p=128)
        for j in range(12):
            b = j % 2
            k.dma("sp", wmf[b][:], wm_v[:, :, j * 512:(j + 1) * 512], writes=[t_wmf[b]])
            k.cp("dve" if j % 2 == 0 else "act", wmb[b][:], wmf[b][:], [t_wmf[b]], [t_wmb[b]])
            for m in range(4):
                jc = j * 4 + m
                for kc in range(8):
                    k.mm(PS[0][:, jc * 2:jc * 2 + 2], wmb[b][:, kc, m * 128:(m + 1) * 128], scb[:, kc, :],
                         kc == 0, kc == 7, [t_wmb[b], t_scb], [PT[0]])
        mod = A("mod", [128, 48, 2]); t_mod = T()
        k.tt("dve", mod[:], PS[0][:, 0:96].rearrange("p (j t) -> p j t", t=2),
             bmod[:].unsqueeze(2).broadcast_to([128, 48, 2]), ALU.add, [PT[0], t_bmod], [t_mod])
        k.stt(mv[:, 0, :], mod[:, 8:16, 0], 1.0, gains[:, 0, :], ALU.add, ALU.mult, [t_mod, t_gains], [t_mv])
        k.cp("dve", mv[:, 1, :], mod[:, 0:8, 0], [t_mod], [t_mv])
        k.stt(mv[:, 2, :], mod[:, 8:16, 1], 1.0, gains[:, 0, :], ALU.add, ALU.mult, [t_mod, t_gains], [t_mv])
        k.cp("dve", mv[:, 3, :], mod[:, 0:8, 1], [t_mod], [t_mv])
        k.stt(mv[:, 4, :], mod[:, 32:40, 0], 1.0, gains[:, 2, :], ALU.add, ALU.mult, [t_mod, t_gains], [t_mv])
        k.cp("dve", mv[:, 5, :], mod[:, 24:32, 0], [t_mod], [t_mv])
        k.tt("dve", mv[:, 6, :], mod[:, 16:24, 0], gains[:, 1, :], ALU.mult, [t_mod, t_gains], [t_mv])
        k.tt("dve", mv[:, 7, :], mod[:, 40:48, 0], gains[:, 3, :], ALU.mult, [t_mod, t_gains], [t_mv])
        dump("mv", mv[:], [t_mv], [128, 8, 8])
        dg = A("dg", [128, 8, 128]); t_dg = T()
        for gi in range(2):
            for kc in range(8):
                k.ts("dve", dg[:, kc, :], ident, mv[:, 6 + gi, kc:kc + 1], None, ALU.mult, ALU.bypass,
                     [t_cst, t_mv], [t_dg])
            for kc in range(8):
                pb = 1 + kc // 4
                k.mm(PS[pb][:, (kc % 4) * 128:(kc % 4 + 1) * 128], ones, dg[:, kc, :], True, True,
                     [t_cst, t_dg], [PT[pb]])
            k.cp("act", gbc[gi][:, 0:512], PS[1][:], [PT[1]], [t_gbc[gi]])
            k.cp("act", gbc[gi][:, 512:1024], PS[2][:], [PT[2]], [t_gbc[gi]])
        dump("gbc0", gbc[0][:], [t_gbc[0]], [128, D])

        xt = [A(f"xt{i}", [128, D]) for i in range(3)]; t_xt = [T() for _ in range(3)]
        xn = [A(f"xn{i}", [128, D]) for i in range(2)]; t_xn = [T() for _ in range(2)]
        hxo = [A(f"hxo{i}", [128, 8, 128], BF16) for i in range(2)]; t_hxo = [T() for _ in range(2)]
        def p1_stage(it, stage):
            b3, b2 = it % 3, it % 2
            lat = it < 32
            if stage == 1:
                src = xl[it * 128:(it + 1) * 128, :] if lat else ctxl[(it - 32) * 128:(it - 31) * 128, :]
                k.dma("sp", xt[b3][:], src, writes=[t_xt[b3]])
            norm_transpose((xn[b2], t_xn[b2]), xt[b3][:], t_xt[b3], it, 0 if lat else 2, 1 if lat else 3,
                           hxo[b2][:], t_hxo[b2], 3 + 2 * b2, stage=stage)
            if stage == 2:
                k.dma("sp", hx_d[:, :, it * 128:(it + 1) * 128], hxo[b2][:], reads=[t_hxo[b2]])

        p1_stage(0, 1)
        for it in range(NT):
            if it + 1 < NT:
                p1_stage(it + 1, 1)
            p1_stage(it, 2)
        k.barrier()
    if stop_after == "A":
        return finish(nc, k, out_toks, out_d)

    with ExitStack() as es:
        A = lambda name, shape, dt=F32: es.enter_context(nc.sbuf_tensor("s_" + name, list(shape), dt))
        wgab = A("wgab", [128, 8, 16], BF16); t_wgab = T()
        k.dma("pool", wgab[:], w_gab_d.rearrange("(k p) n -> p k n", p=128), writes=[t_wgab])
        hxb = [A(f"hxbg{i}", [128, 8, 512], BF16) for i in range(2)]; t_hxb = [T(), T()]
        gab = A("gab", [128, NT, 16]); t_gab = T()
        for bi in range(9):
            tok0 = bi * 512
            n = 512 if bi < 8 else 256
            hb = bi % 2
            k.dma("sp", hxb[hb][:, :, :n], hx_d[:, :, tok0:tok0 + n], writes=[t_hxb[hb]])
            pb = bi % 2
            for j in range(n // 128):
                for kc in range(8):
                    k.mm(PS[pb][:, j * 16:(j + 1) * 16], hxb[hb][:, kc, j * 128:(j + 1) * 128], wgab[:, kc, :],
                         kc == 0, kc == 7, [t_hxb[hb], t_wgab], [PT[pb]])
            k.cp("act", gab[:, bi * 4:bi * 4 + n // 128, :],
                 PS[pb][:, 0:(n // 128) * 16].rearrange("p (j c) -> p j c", c=16), [PT[pb]], [t_gab])
        dump("gab", gab[:], [t_gab], [128, NT, 16])
        negA = A("negA", [128, 8]); t_negA = T()
        k.act(negA[:], hp[:, 8:16], AF.Exp, [t_hp], [t_negA])
        k.ts("dve", negA[:], negA[:], -1.0, None, ALU.mult, ALU.bypass, [t_negA], [t_negA])
        z = A("z", [128, NT, 8]); t_z = T()
        k.tt("dve", z[:], gab[:, :, 0:8], hp[:, 16:24].unsqueeze(1).broadcast_to([128, NT, 8]), ALU.add,
             [t_gab, t_hp], [t_z])
        k.act(z[:], z[:], AF.Exp, [t_z], [t_z])
        k.act(z[:], z[:], AF.Ln, [t_z], [t_z], bias=1.0)
        k.tt("dve", g_la[:], z[:], negA[:].unsqueeze(1).broadcast_to([128, NT, 8]), ALU.mult, [t_z, t_negA], [t_gs])
        sigmoid_to(g_beta[:], gab[:, :, 8:16], [t_gab], t_gs)
        k.ts("dve", g_nbeta[:], g_beta[:], -1.0, None, ALU.mult, ALU.bypass, [t_gs], [t_gs])
        for ti in range(NT):
            pb = ti % 2
            k.mm(PS[pb][:, 0:4], U_in, g_la[:, ti, 0:4], True, True, [t_cst, t_gs], [PT[pb]])
            k.mm(PS[pb][:, 4:8], L_in, g_la[:, ti, 4:8], True, True, [t_cst, t_gs], [PT[pb]])
            k.mm(PS[pb][:, 8:16], ones, g_la[:, ti, :], True, True, [t_cst, t_gs], [PT[pb]])
            k.cp("act", g_g[:, ti, :], PS[pb][:, 0:8], [PT[pb]], [t_gs])
            k.act(g_egl[:, ti, :], PS[pb][:, 8:16], AF.Exp, [PT[pb]], [t_gs])
            k.tt("dve", g_kts[:, ti, :], PS[pb][:, 8:16], g_g[:, ti, :], ALU.subtract, [PT[pb], t_gs], [t_gs])
        k.act(g_kts[:], g_kts[:], AF.Exp, [t_gs], [t_gs])
        k.ts("dve", g_ng[:], g_g[:], -1.0, None, ALU.mult, ALU.bypass, [t_gs], [t_gs])
        k.act(g_beg[:], g_g[:], AF.Exp, [t_gs], [t_gs])
        k.tt("dve", g_beg[:], g_beg[:], g_beta[:], ALU.mult, [t_gs], [t_gs])
        dump("g_g", g_g[:], [t_gs], [128, NT, 8])
        dump("g_beta", g_beta[:], [t_gs], [128, NT, 8])
        k.barrier()
    if stop_after == "G":
        return finish(nc, k, out_toks, out_d)

    es_heads = ExitStack()
    whd2 = [es_heads.enter_context(nc.sbuf_tensor(f"s_whd{i}", [128, 8, 1280], BF16)) for i in range(2)]
    t_whd2 = [T(), T()]

    def load_whd(hh):
        wv_ = w_in_h[hh].rearrange("(k p) n -> p k n", p=128)
        for half in range(2):
            k.dma("pool", whd2[hh % 2][:, :, half * 640:(half + 1) * 640], wv_[:, :, half * 640:(half + 1) * 640],
                  writes=[t_whd2[hh % 2]])

    load_whd(0)
    for h in range(4):
        with ExitStack() as es:
            A = lambda name, shape, dt=F32: es.enter_context(nc.sbuf_tensor(f"s_{name}_{h}", list(shape), dt))
            esr = ExitStack()
            AR = lambda name, shape, dt=F32: esr.enter_context(nc.sbuf_tensor(f"s_{name}_{h}", list(shape), dt))
            whd = whd2[h % 2]; t_whd = t_whd2[h % 2]
            hxb = [A(f"hxb{i}", [128, 8, 512], BF16) for i in range(2)]; t_hxb = [T(), T()]
            rope1 = A("rope", [128, 4, 512]); rope = [rope1, rope1]; t_rope1 = T(); t_rope = [t_rope1, t_rope1]
            sgzT = A("sgzT", [128, NTOK], BF16); t_sgz = [T() for _ in range(4)]
            pck = A("pck", [128, NS + 6], BF16); t_pck = T()
            pcv = A("pcv", [128, NS + 6], BF16); t_pcv = T()
            pcq = A("pcq", [128, NTOK + 130], BF16); t_pcq = T()
            tmp1 = A("tmp1", [128, 512]); t_tmp1 = T()
            tmp2 = A("tmp2", [128, 512]); t_tmp2 = T()
            tmp3 = A("tmp3", [128, 512]); t_tmp3 = T()
            onb = [A(f"onb{i}", [128, 128], BF16) for i in range(2)]; t_onb = [T(), T()]
            ybuf = A("ybuf", [128, NTOK], BF16); t_ybuf = T()
            rkT = AR("rkT", [128, NTOK], BF16); t_rkT = [T() for _ in range(4)]
            rkblk = AR("rkblk", [128, 512], BF16); t_rkblk = T()
            rk_tm = AR("rk_tm", [128, NT, 128], BF16); t_rk_tm = [T() for _ in range(9)]
            rv_tm = AR("rv_tm", [128, NT, 128], BF16); t_rv_tm = [T() for _ in range(9)]
            rqT = AR("rqT", [128, NTOK], BF16); t_rqT = [T() for _ in range(4)]
            QF = AR("QF", [128, NTOK], BF16); QB = AR("QB", [128, NTOK], BF16); t_QFB = [T() for _ in range(4)]
            srgT = AR("srgT", [128, NTOK], BF16); t_srg = [T() for _ in range(4)]
            lgf = hp[:, h:h + 1]
            lgb = hp[:, 4 + h:5 + h]
            GF = AR("GF", [128, 128]); GB = AR("GB", [128, 128]); DmT = AR("DmT", [128, 128])
            E1 = AR("E1", [128, 128]); E2 = AR("E2", [128, 128]); wcol = AR("wcol", [128, 4])
            t_rc = T()
            k.act(GF[:], cst[:, 8, :], AF.Exp, [t_cst, t_hp], [t_rc], scale=lgf)
            k.act(GB[:], cst[:, 9, :], AF.Exp, [t_cst, t_hp], [t_rc], scale=lgb)
            k.act(wcol[:, 0:1], cst[:, 10, 1:2], AF.Exp, [t_cst, t_hp], [t_rc], scale=lgf)
            k.act(wcol[:, 1:2], cst[:, 10, 0:1], AF.Exp, [t_cst, t_hp], [t_rc], scale=lgb)
            k.act(wcol[:, 2:3], cst[:, 10, 2:3], AF.Exp, [t_cst, t_hp], [t_rc], scale=lgf)
            k.act(wcol[:, 3:4], cst[:, 10, 2:3], AF.Exp, [t_cst, t_hp], [t_rc], scale=lgb)
            k.act(E1[:], cst[:, 6, :], AF.Exp, [t_cst, t_hp], [t_rc], scale=lgf)
            k.act(E2[:], cst[:, 7, :], AF.Exp, [t_cst, t_hp], [t_rc], scale=lgb)
            k.tt("dve", E1[:], E1[:], U_st, ALU.mult, [t_rc, t_cst], [t_rc])
            k.tt("dve", E2[:], E2[:], L_st, ALU.mult, [t_rc, t_cst], [t_rc])
            k.tt("dve", E1[:], E1[:], E2[:], ALU.add, [t_rc], [t_rc])
            k.stt(DmT[:], ident, 2.0, E1[:], ALU.mult, ALU.add, [t_cst, t_rc], [t_rc])
            for (buf, tl, cols) in ((pck, t_pck, (0, 4097, 4098, 4355)), (pcv, t_pcv, (0, 4097, 4098, 4355)),
                                    (pcq, t_pcq, (0,))):
                for c in cols:
                    k.memset("pool", buf[:, c:c + 1], 0.0, [tl])

            def silu_psum_to(dst_ap, t_dst, pb, n):
                k.act(dst_ap, PS[pb][:, :n], AF.Silu, [PT[pb]], [t_dst])

            for bi in range(9):
                tok0 = bi * 512
                n = 512 if bi < 8 else 256
                own = bi < 4
                hb = bi % 2
                nt = n // 128
                k.dma("sp", hxb[hb][:, :, :n], hx_d[:, :, tok0:tok0 + n], writes=[t_hxb[hb]])
                k.dma("sp", rope[hb][:, :, :n], rope_d[:, :, tok0:tok0 + n], writes=[t_rope[hb]])

                def proj(cb, pb, ncols=n, c0=0):
                    for kc in range(8):
                        k.mm(PS[pb][:, :ncols], whd[:, kc, cb * 128:(cb + 1) * 128], hxb[hb][:, kc, c0:c0 + ncols],
                             kc == 0, kc == 7, [t_whd, t_hxb[hb]], [PT[pb]])

                def rope_to(dst_ap, t_dst, tc, ts_):
                    proj(tc[0], 0)
                    proj(tc[1], 1)
                    k.tt("dve", tmp1[:, :n], PS[0][:, :n], rope[hb][:, ts_, :n], ALU.mult, [PT[0], t_rope[hb]], [t_tmp1])
                    k.tt("dve", tmp2[:, :n], PS[1][:, :n], rope[hb][:, ts_ + 1, :n], ALU.mult, [PT[1], t_rope[hb]],
                         [t_tmp2])
                    k.tt("dve", dst_ap, tmp1[:, :n], tmp2[:, :n], ALU.add, [t_tmp1, t_tmp2], [t_dst])

                if own:
                    dst, t_dst = rkT[:, tok0:tok0 + n], t_rkT[bi]
                else:
                    dst, t_dst = rkblk[:, :n], t_rkblk
                rope_to(dst, t_dst, (0, 1), 0)
                for j in range(nt):
                    k.tr(PSB[2][:, j * 128:(j + 1) * 128], dst[:, j * 128:(j + 1) * 128], identb, [t_dst, t_cstb], [PT[2]])
                k.cp("act", rk_tm[:, bi * 4:bi * 4 + nt, :], PSB[2][:, :n].rearrange("p (j c) -> p j c", c=128),
                     [PT[2]], [t_rk_tm[bi]])
                for j in range(nt):
                    for kc in range(8):
                        k.mm(PS[3][:, j * 128:(j + 1) * 128], hxb[hb][:, kc, j * 128:(j + 1) * 128],
                             whd[:, kc, 256:384], kc == 0, kc == 7, [t_whd, t_hxb[hb]], [PT[3]])
                k.cp("act", rv_tm[:, bi * 4:bi * 4 + nt, :], PS[3][:, :n].rearrange("p (j c) -> p j c", c=128),
                     [PT[3]], [t_rv_tm[bi]])
                off = tok0 + 1 if bi < 8 else tok0 + 3
                proj(3, 4)
                k.cp("act", pck[:, off:off + n], PS[4][:, :n], [PT[4]], [t_pck])
                proj(4, 5)
                k.cp("act", pcv[:, off:off + n], PS[5][:, :n], [PT[5]], [t_pcv])
                if own:
                    rope_to(rqT[:, tok0:tok0 + n], t_rqT[bi], (5, 6), 2)
                    k.tt("pool", QF[:, tok0:tok0 + n].rearrange("p (c i) -> p c i", i=128),
                         rqT[:, tok0:tok0 + n].rearrange("p (c i) -> p c i", i=128),
                         GF[:].unsqueeze(1).broadcast_to([128, 4, 128]), ALU.mult, [t_rqT[bi], t_rc], [t_QFB[bi]])
                    k.tt("pool", QB[:, tok0:tok0 + n].rearrange("p (c i) -> p c i", i=128),
                         rqT[:, tok0:tok0 + n].rearrange("p (c i) -> p c i", i=128),
                         GB[:].unsqueeze(1).broadcast_to([128, 4, 128]), ALU.mult, [t_rqT[bi], t_rc], [t_QFB[bi]])
                    proj(7, 6)
                    silu_psum_to(srgT[:, tok0:tok0 + n], t_srg[bi], 6, n)
                    proj(9, 6)
                    silu_psum_to(sgzT[:, tok0:tok0 + n], t_sgz[bi], 6, n)
                    proj(8, 7)
                    k.cp("act", pcq[:, 1 + tok0:1 + tok0 + n], PS[7][:, :n], [PT[7]], [t_pcq])
                if bi == 4:
                    proj(8, 7, ncols=128)
                    k.cp("act", pcq[:, 2049:2049 + 128], PS[7][:, :128], [PT[7]], [t_pcq])
            if h + 1 < 4:
                load_whd(h + 1)
            if stop_after == f"H{h}a":
                k.barrier(); esr.close()
                return finish(nc, k, out_toks, out_d)
            dump(f"rkT{h}", rkT[:], t_rkT, [128, NTOK], BF16)
            dump(f"rv_tm{h}", rv_tm[:], t_rv_tm, [128, NT, 128], BF16)
            dump(f"rk_tm{h}", rk_tm[:], t_rk_tm, [128, NT, 128], BF16)
            dump(f"QF{h}", QF[:], t_QFB, [128, NTOK], BF16)
            dump(f"srgT{h}", srgT[:], t_srg, [128, NTOK], BF16)

            Sb32 = AR("Sb32", [128, 128]); t_Sb32 = T()
            Sf32 = AR("Sf32", [128, 128]); t_Sf32 = T()
            Sbst = AR("Sbst", [128, 16, 128], BF16); t_Sbst = [T() for _ in range(16)]
            Sfb = [AR(f"Sfb{i}", [128, 128], BF16) for i in range(2)]; t_Sfb = [T(), T()]
            wvb = [AR(f"wvb{i}", [128, 128], BF16) for i in range(2)]; t_wvb = [T(), T()]
            scm = [AR(f"scm{i}", [128, 128], BF16) for i in range(2)]; t_scm = [T(), T()]
            osb = AR("osb", [128, 16, 128]); t_osb = [T() for _ in range(16)]
            bst = AR("bst", [128, 16, 6]); t_bst = T()
            mvar = AR("mvar", [128, 16, 2]); t_mvar = T()
            rstd16 = AR("rstd16", [128, 16]); t_rstd16 = T()
            k.memset("dve", Sb32[:], 0.0, [t_Sb32])
            k.memset("dve", Sf32[:], 0.0, [t_Sf32])
            cnt = [0]

            def ret_update(S32, t_S32, ti, wc, dc):
                i = cnt[0] % 2
                cnt[0] += 1
                bi_ = ti // 4
                k.ts("dve", wvb[i][:], rv_tm[:, ti, :], wcol[:, wc:wc + 1], None, ALU.mult, ALU.bypass,
                     [t_rv_tm[bi_], t_rc], [t_wvb[i]])
                k.mm(PS[i][:, 0:128], rk_tm[:, ti, :], wvb[i][:], True, True, [t_rk_tm[bi_], t_wvb[i]], [PT[i]])
                k.stt(S32[:], S32[:], wcol[:, dc:dc + 1], PS[i][:, 0:128], ALU.mult, ALU.add,
                      [t_S32, t_rc, PT[i]], [t_S32])

            for ti in [33, 32] + list(range(31, 15, -1)) + list(range(15, -1, -1)):
                if ti < 16:
                    k.cp("act", Sbst[:, ti, :], Sb32[:], [t_Sb32], [t_Sbst[ti]])
                ret_update(Sb32, t_Sb32, ti, 1, 3)
            if stop_after == f"H{h}r2":
                k.barrier(); esr.close()
                return finish(nc, k, out_toks, out_d)
            for ti in (32, 33):
                ret_update(Sf32, t_Sf32, ti, 0, 2)
            for n_ in range(16):
                i = n_ % 2
                bi_ = n_ // 4
                cs = slice(n_ * 128, (n_ + 1) * 128)
                k.cp("act", Sfb[i][:], Sf32[:], [t_Sf32], [t_Sfb[i]])
                k.mm(PS[2 + i][:, 0:128], rkT[:, cs], rqT[:, cs], True, True, [t_rkT[bi_], t_rqT[bi_]], [PT[2 + i]])
                k.tt("dve", scm[i][:], PS[2 + i][:, 0:128], DmT[:], ALU.mult, [PT[2 + i], t_rc], [t_scm[i]])
                k.mm(PS[4 + i][:, 0:128], scm[i][:], rv_tm[:, n_, :], True, False, [t_scm[i], t_rv_tm[bi_]], [PT[4 + i]])
                k.mm(PS[4 + i][:, 0:128], QF[:, cs], Sfb[i][:], False, False, [t_QFB[bi_], t_Sfb[i]], [PT[4 + i]])
                k.mm(PS[4 + i][:, 0:128], QB[:, cs], Sbst[:, n_, :], False, True, [t_QFB[bi_], t_Sbst[n_]], [PT[4 + i]])
                k.act(osb[:, n_, :], PS[4 + i][:, 0:128], AF.Identity, [PT[4 + i]], [t_osb[n_], t_bst],
                      accum_out=bst[:, n_, 0:1])
                k.act(junk[:, 0:128], PS[4 + i][:, 0:128], AF.Square, [PT[4 + i]], [t_junk, t_bst],
                      accum_out=bst[:, n_, 1:2])
                ret_update(Sf32, t_Sf32, n_, 0, 2)
            if stop_after == f"H{h}r3":
                k.barrier(); esr.close()
                return finish(nc, k, out_toks, out_d)
            k.ts("dve", mvar[:, :, 0], bst[:, :, 0], 1.0 / 128, None, ALU.mult, ALU.bypass, [t_bst], [t_mvar])
            k.tt("dve", mvar[:, :, 1], mvar[:, :, 0], mvar[:, :, 0], ALU.mult, [t_mvar], [t_mvar])
            k.stt(mvar[:, :, 1], bst[:, :, 1], 1.0 / 128, mvar[:, :, 1], ALU.mult, ALU.subtract, [t_bst, t_mvar], [t_mvar])
            rstd_to(rstd16[:], mvar[:, :, 1], 1.0, [t_mvar], t_rstd16)
            dump(f"osb{h}", osb[:], t_osb, [128, 16, 128])
            for n_ in range(16):
                i = n_ % 2
                cs = slice(n_ * 128, (n_ + 1) * 128)
                k.ts("dve", onb[i][:], osb[:, n_, :], mvar[:, n_, 0:1], rstd16[:, n_:n_ + 1], ALU.subtract, ALU.mult,
                     [t_osb[n_], t_mvar, t_rstd16], [t_onb[i]])
                k.tr(PSB[6 + i][:, 0:128], onb[i][:], identb, [t_onb[i], t_cstb], [PT[6 + i]])
                k.stt(ybuf[:, cs], PSB[6 + i][:, 0:128], nw[:, 0, h:h + 1], srgT[:, cs], ALU.mult, ALU.mult,
                      [PT[6 + i], t_nw, t_srg[n_ // 4]], [t_ybuf])
            dump(f"yret{h}", ybuf[:], [t_ybuf], [128, NTOK], BF16)
            k.dma("sp", y_d[h], ybuf[:], reads=[t_ybuf])
            k.barrier()
            esr.close()
            if stop_after == f"H{h}b":
                return finish(nc, k, out_toks, out_d)

            gkT = A("gkT", [128, NS], BF16); t_gkT = T()
            gvT = A("gvT", [128, NS], BF16); t_gvT = T()
            gqT = A("gqT", [128, NTOK], BF16); t_gqT = T()
            sqb = A("sqb", [128, 512], BF16); t_sqb = T()

            def conv_piece(pc, t_pc, o, n, fam, dstT, t_dstT, d0, l2, qscale):
                w = convw[:, h, fam, :]
                k.ts("dve", tmp1[:, :n], pc[:, o - 1:o - 1 + n], w[:, 0:1], None, ALU.mult, ALU.bypass,
                     [t_pc, t_convw], [t_tmp1])
                k.stt(tmp1[:, :n], pc[:, o:o + n], w[:, 1:2], tmp1[:, :n], ALU.mult, ALU.add, [t_pc, t_convw, t_tmp1],
                      [t_tmp1])
                k.stt(tmp1[:, :n], pc[:, o + 1:o + 1 + n], w[:, 2:3], tmp1[:, :n], ALU.mult, ALU.add,
                      [t_pc, t_convw, t_tmp1], [t_tmp1])
                sigmoid_to(tmp3[:, :n], tmp1[:, :n], [t_tmp1], t_tmp3)
                if not l2:
                    k.tt("dve", dstT[:, d0:d0 + n], tmp1[:, :n], tmp3[:, :n], ALU.mult, [t_tmp1, t_tmp3], [t_dstT])
                    return
                k.tt("dve", tmp2[:, :n], tmp1[:, :n], tmp3[:, :n], ALU.mult, [t_tmp1, t_tmp3], [t_tmp2])
                k.act(sqb[:, :n], tmp2[:, :n], AF.Square, [t_tmp2], [t_sqb])
                k.mm(PS[7][:, :n], onesb, sqb[:, :n], True, True, [t_cstb, t_sqb], [PT[7]])
                rstd_to(tmp3[:, :n], PS[7][:, :n], 1.0, [PT[7]], t_tmp3, post_scale=qscale)
                k.tt("dve", dstT[:, d0:d0 + n], tmp2[:, :n], tmp3[:, :n], ALU.mult, [t_tmp2, t_tmp3], [t_dstT])

            for bi in range(9):
                tok0 = bi * 512
                n = 512 if bi < 8 else 256
                off = tok0 + 1 if bi < 8 else tok0 + 3
                conv_piece(pck, t_pck, off, n, 1, gkT, t_gkT, tok0, True, None)
                conv_piece(pcv, t_pcv, off, n, 2, gvT, t_gvT, tok0, False, None)
                if bi < 4:
                    conv_piece(pcq, t_pcq, tok0 + 1, n, 0, gqT, t_gqT, tok0, True, QSC)
            dump(f"gkT{h}", gkT[:], [t_gkT], [128, NS], BF16)
            dump(f"gvT{h}", gvT[:], [t_gvT], [128, NS], BF16)
            dump(f"gqT{h}", gqT[:], [t_gqT], [128, NTOK], BF16)

            if stop_after == f"H{h}g1":
                k.barrier()
                return finish(nc, k, out_toks, out_d)
            XAB = [A(f"XAB{s}", [128, 512]) for s in range(8)]; t_XAB = [T() for _ in range(8)]
            wkT = [A(f"wkT{s}", [128, 128], BF16) for s in range(8)]; t_wkT = [T() for _ in range(8)]
            ktl = [A(f"ktl{s}", [128, 128], BF16) for s in range(8)]; t_ktl = [T() for _ in range(8)]
            QKm = [A(f"QKm{s}", [128, 128], BF16) for s in range(8)]; t_QKm = [T() for _ in range(8)]
            QgT = [A(f"QgT{s}", [128, 128], BF16) for s in range(8)]; t_QgT = [T() for _ in range(8)]
            Fm = [A(f"Fm{s}", [128, 128]) for s in range(4)]; t_Fm = [T() for _ in range(4)]
            FL = [A(f"FL{s}", [128, 128]) for s in range(4)]; t_FL = [T() for _ in range(4)]
            FU = [A(f"FU{s}", [128, 128]) for s in range(4)]; t_FU = [T() for _ in range(4)]
            ad = [A(f"ad{s}", [128, 128]) for s in range(4)]; t_ad = [T() for _ in range(4)]
            EG = [A(f"EG{s}", [128, 128]) for s in range(4)]; t_EG = [T() for _ in range(4)]
            dgm = [A(f"dgm{s}", [128, 128]) for s in range(4)]; t_dgm = [T() for _ in range(4)]
            S32 = [A(f"S32{c}", [128, 128]) for c in range(2)]; t_S32 = [T(), T()]
            Sbf = [A(f"Sbf{c}", [128, 128], BF16) for c in range(2)]; t_Sbf = [T(), T()]
            vnb = [A(f"vnb{c}", [128, 128], BF16) for c in range(2)]; t_vnb = [T(), T()]
            oacc = A("oacc", [128, 16, 128]); t_oacc = [T() for _ in range(16)]
            oacc_set = [False] * 16
            for c in range(2):
                k.memset("dve", S32[c][:], 0.0, [t_S32[c]])
                k.cp("act", Sbf[c][:], S32[c][:], [t_S32[c]], [t_Sbf[c]])
            f_list = [(0, 32), (0, 33)] + [(0, i) for i in range(16)]
            b_own = [(1, i) for i in range(15, -1, -1)]
            units = [(1, 33), (1, 32)] + [(1, i) for i in range(31, 15, -1)]
            for i in range(18):
                units.append(f_list[i])
                if i < 16:
                    units.append(b_own[i])
            groups = [units[g0:g0 + 4] for g0 in range(0, len(units), 4)]

            def pre_pieces(gi):
                grp = groups[gi]
                so = 4 * (gi % 2)
                pcs = []

                def stA():
                    for s, (c, ti) in enumerate(grp):
                        j = c * 4 + h
                        col = slice(ti * 128, (ti + 1) * 128)
                        k.ts("dve", dgm[s][:], ident, g_g[:, ti, j:j + 1], None, ALU.mult, ALU.bypass,
                             [t_cst, t_gs], [t_dgm[s]])
                        k.mm(PS[s][:, 0:128], ones, dgm[s][:], True, True, [t_cst, t_dgm[s]], [PT[s]])
                        k.mm(PS[s][:, 128:256], gkT[:, col], gkT[:, col], True, True, [t_gkT], [PT[s]])
                        k.tr(PSB[s][:, 512:640], gkT[:, col], identb, [t_gkT, t_cstb], [PT[s]])
                        k.tr(PSB[s][:, 640:768], gvT[:, col], identb, [t_gvT, t_cstb], [PT[s]])

                def stB():
                    for s, (c, ti) in enumerate(grp):
                        j = c * 4 + h
                        u = so + s
                        hasq = ti < 16
                        Mst = L_st if c == 0 else U_st
                        MinT = U_in if c == 0 else L_in
                        k.ts("dve", ad[s][:], PS[s][:, 0:128], g_g[:, ti, j:j + 1], None, ALU.subtract, ALU.bypass,
                             [PT[s], t_gs], [t_ad[s]])
                        k.ts("dve", Fm[s][:], PS[s][:, 0:128], -1.0, g_g[:, ti, j:j + 1], ALU.mult, ALU.add,
                             [PT[s], t_gs], [t_Fm[s]])
                        k.tt("dve", Fm[s][:], Fm[s][:], ad[s][:], ALU.min, [t_Fm[s], t_ad[s]], [t_Fm[s]])
                        k.act(Fm[s][:], Fm[s][:], AF.Exp, [t_Fm[s]], [t_Fm[s]])
                        if hasq:
                            k.act(EG[s][:], PS[s][:, 0:128], AF.Exp, [PT[s]], [t_EG[s]])
                        k.ts("dve", XAB[u][:, 0:128], PSB[s][:, 640:768], g_beta[:, ti, j:j + 1], None, ALU.mult,
                             ALU.bypass, [PT[s], t_gs], [t_XAB[u]])
                        k.ts("dve", XAB[u][:, 128:256], PSB[s][:, 512:640], g_beg[:, ti, j:j + 1], None, ALU.mult,
                             ALU.bypass, [PT[s], t_gs], [t_XAB[u]])
                        k.ts("dve", ktl[u][:], PSB[s][:, 512:640], g_kts[:, ti, j:j + 1], None, ALU.mult, ALU.bypass,
                             [PT[s], t_gs], [t_ktl[u]])
                        k.tt("dve", FL[s][:], Fm[s][:], Mst, ALU.mult, [t_Fm[s], t_cst], [t_FL[s]])
                        if hasq:
                            k.tt("dve", FU[s][:], Fm[s][:], MinT, ALU.mult, [t_Fm[s], t_cst], [t_FU[s]])
                        k.stt(XAB[u][:, 256:384], PS[s][:, 128:256], g_nbeta[:, ti, j:j + 1], FL[s][:], ALU.mult,
                              ALU.mult, [PT[s], t_gs, t_FL[s]], [t_XAB[u]])

                def stC():
                    for s, (c, ti) in enumerate(grp):
                        u = so + s
                        hasq = ti < 16
                        col = slice(ti * 128, (ti + 1) * 128)
                        k.tr(PS[s][:, 384:512], XAB[u][:, 256:384], ident, [t_XAB[u], t_cst], [PT[s]])
                        k.cp("act", XAB[u][:, 384:512], PS[s][:, 384:512], [PT[s]], [t_XAB[u]])
                        if hasq:
                            k.mm(PS[s][:, 256:384], gkT[:, col], gqT[:, col], True, True, [t_gkT, t_gqT], [PT[s]])
                            k.tt("dve", QKm[u][:], PS[s][:, 256:384], FU[s][:], ALU.mult, [PT[s], t_FU[s]], [t_QKm[u]])
                            k.tt("pool", QgT[u][:], gqT[:, col], EG[s][:], ALU.mult, [t_gqT, t_EG[s]], [t_QgT[u]])

                def level(lv):
                    def f():
                        last = lv == 6
                        w_ = 256 if last else 384
                        for s in range(len(grp)):
                            u = so + s
                            k.mm(PS[s][:, 0:w_], XAB[u][:, 384:512], XAB[u][:, 0:w_], True, True, [t_XAB[u]], [PT[s]])
                            if not last:
                                k.mm(PS[s][:, 384:512], XAB[u][:, 256:384], XAB[u][:, 384:512], True, True,
                                     [t_XAB[u]], [PT[s]])
                        for s in range(len(grp)):
                            u = so + s
                            k.tt("dve", XAB[u][:, 0:256], XAB[u][:, 0:256], PS[s][:, 0:256], ALU.add,
                                 [t_XAB[u], PT[s]], [t_XAB[u]])
                            if not last:
                                k.cp("act", XAB[u][:, 256:512], PS[s][:, 256:512], [PT[s]], [t_XAB[u]])
                    return f

                def fin():
                    for s in range(len(grp)):
                        u = so + s
                        k.tr(PS[s][:, 0:128], XAB[u][:, 128:256], ident, [t_XAB[u], t_cst], [PT[s]])
                        k.cp("act", wkT[u][:], PS[s][:, 0:128], [PT[s]], [t_wkT[u]])

                return [stA, stB, stC] + [level(lv) for lv in range(7)] + [fin]

            def scan_pieces(gi):
                grp = groups[gi]
                so = 4 * (gi % 2)
                pcs = []
                for s, (c, ti) in enumerate(grp):
                    u = so + s
                    j = c * 4 + h
                    hasq = ti < 16
                    pb = 4 + c

                    def pa(u=u, c=c, pb=pb):
                        k.mm(PS[pb][:, 0:128], wkT[u][:], Sbf[c][:], True, True, [t_wkT[u], t_Sbf[c]], [PT[pb]])
                        k.tt("dve", vnb[c][:], XAB[u][:, 0:128], PS[pb][:, 0:128], ALU.subtract, [t_XAB[u], PT[pb]],
                             [t_vnb[c]])

                    def pbf(u=u, c=c, pb=pb, ti=ti, j=j, hasq=hasq):
                        if hasq:
                            k.mm(PS[6 + c][:, 0:128], QKm[u][:], vnb[c][:], True, False, [t_QKm[u], t_vnb[c]], [PT[6 + c]])
                            k.mm(PS[6 + c][:, 0:128], QgT[u][:], Sbf[c][:], False, True, [t_QgT[u], t_Sbf[c]], [PT[6 + c]])
                        k.mm(PS[pb][:, 128:256], ktl[u][:], vnb[c][:], True, True, [t_ktl[u], t_vnb[c]], [PT[pb]])
                        k.stt(S32[c][:], S32[c][:], g_egl[:, ti, j:j + 1], PS[pb][:, 128:256], ALU.mult, ALU.add,
                              [t_S32[c], t_gs, PT[pb]], [t_S32[c]])
                        k.cp("act", Sbf[c][:], S32[c][:], [t_S32[c]], [t_Sbf[c]])
                        if hasq:
                            if not oacc_set[ti]:
                                k.cp("act", oacc[:, ti, :], PS[6 + c][:, 0:128], [PT[6 + c]], [t_oacc[ti]])
                                oacc_set[ti] = True
                            else:
                                k.tt("dve", oacc[:, ti, :], oacc[:, ti, :], PS[6 + c][:, 0:128], ALU.add,
                                     [t_oacc[ti], PT[6 + c]], [t_oacc[ti]])
                    pcs += [pa, pbf]
                return pcs

            NG = len(groups)
            for gi in range(NG + 1):
                pre = pre_pieces(gi) if gi < NG else []
                scn = scan_pieces(gi - 1) if gi >= 1 else []
                if noq == "seq":
                    for f_ in scn:
                        f_()
                    for f_ in pre:
                        f_()
                    continue
                n = max(len(pre), len(scn))
                for i in range(n):
                    if i < len(pre):
                        pre[i]()
                    if i < len(scn):
                        scn[i]()
            dump(f"oacc{h}", oacc[:], t_oacc, [128, 16, 128])
            for n_ in range(16):
                k.act(junk[:, 0:128], oacc[:, n_, :], AF.Square, [t_oacc[n_]], [t_junk, t_ss],
                      accum_out=ss[:, 40 + n_:41 + n_])
            rstd_to(rs[:, 40:56], ss[:, 40:56], 1.0 / 128, [t_ss], t_rs)
            for n_ in range(16):
                i = n_ % 2
                cs = slice(n_ * 128, (n_ + 1) * 128)
                k.ts("dve", onb[i][:], oacc[:, n_, :], rs[:, 40 + n_:41 + n_], None, ALU.mult, ALU.bypass,
                     [t_oacc[n_], t_rs], [t_onb[i]])
                k.tr(PSB[6 + i][:, 0:128], onb[i][:], identb, [t_onb[i], t_cstb], [PT[6 + i]])
                k.stt(ybuf[:, cs], PSB[6 + i][:, 0:128], nw[:, 1, h:h + 1], sgzT[:, cs], ALU.mult, ALU.mult,
                      [PT[6 + i], t_nw, t_sgz[n_ // 4]], [t_ybuf])
            dump(f"ygdn{h}", ybuf[:], [t_ybuf], [128, NTOK], BF16)
            k.dma("sp", y_d[4 + h], ybuf[:], reads=[t_ybuf])
            k.barrier()
        if stop_after == f"H{h}":
            return finish(nc, k, out_toks, out_d)

    es_heads.close()
    h2T = P("h2T", [128, 8, NTOK], BF16); t_h2T = [T() for _ in range(16)]
    wts = P("wts", [128, 16, NE]); t_wts = [T() for _ in range(16)]
    with ExitStack() as es:
        A = lambda name, shape, dt=F32: es.enter_context(nc.sbuf_tensor("s_" + name, list(shape), dt))
        wg = A("wg", [128, 8, 2048], BF16); t_wg = T()
        wgv = w_gates_d.rearrange("(k p) n -> p k n", p=128)
        for q4 in range(4):
            k.dma("pool", wg[:, :, q4 * 512:(q4 + 1) * 512], wgv[:, :, q4 * 512:(q4 + 1) * 512], writes=[t_wg])
        wro = A("wro", [128, 4, D], BF16); t_wro = T()
        k.dma("pool", wro[:], w_ro_d.rearrange("(k p) n -> p k n", p=128), writes=[t_wro])
        wgo = A("wgo", [128, 4, D], BF16); t_wgo = T()
        k.dma("pool", wgo[:], w_go_d.rearrange("(k p) n -> p k n", p=128), writes=[t_wgo])
        wo = A("wo", [128, 8, D], BF16); t_wo = T()
        k.dma("pool", wo[:], w_o_d.rearrange("(k p) n -> p k n", p=128), writes=[t_wo])
        wr = A("wr", [128, 8, 64]); t_wr = T()
        k.dma("sp", wr[:], w_r_d.rearrange("(k p) n -> p k n", p=128), writes=[t_wr])
        rb = A("rb", [128, 64]); t_rb = T()
        k.dma("sp", rb[:], rb_d, writes=[t_rb])
        hxb31 = A("hxb3", [128, 8, 512], BF16); hxb = [hxb31, hxb31]; t_hxb31 = T(); t_hxb = [t_hxb31, t_hxb31]
        yb1 = A("yb", [128, 8, 512], BF16); yb = [yb1, yb1]; t_yb1 = T(); t_yb = [t_yb1, t_yb1]
        mT = A("mT", [128, 8, 512], BF16); t_mT = T()
        sg1 = A("sg1", [128, 512]); t_sg1 = T()
        sg2 = A("sg2", [128, 512]); t_sg2 = T()
        u1 = A("u1", [128, 512]); t_u1 = T()
        u2 = A("u2", [128, 512]); t_u2 = T()
        xt = [A(f"xt3{i}", [128, D]) for i in range(2)]; t_xt = [T(), T()]
        x1 = [A(f"x1{i}", [128, D]) for i in range(2)]; t_x1 = [T(), T()]
        xn = A("xn3", [128, D]); t_xn = T()
        h2f = A("h2f", [128, 8, 128]); t_h2f = T()
        sc = A("sc", [128, 64]); t_sc = T()
        bs = A("bs", [128, 64]); t_bs = T()
        m8 = A("m8", [128, 8, 8]); t_m8 = T()
        grp_ = A("grp", [128, 8]); t_grp = T()
        g8 = A("g8", [128, 8]); t_g8 = T()
        pen = A("pen", [128, 8]); t_pen = T()
        cand = A("cand", [128, 64]); t_cand = T()
        c8 = A("c8", [128, 8]); t_c8 = T()
        den = A("den", [128, 2]); t_den = T()
        for bi in range(4):
            tok0 = bi * 512
            hb = bi % 2
            k.dma("sp", hxb[hb][:], hx_d[:, :, tok0:tok0 + 512], writes=[t_hxb[hb]])
            k.dma("sp", yb[hb][:], y_d[:, :, tok0:tok0 + 512].rearrange("f p n -> p f n"), writes=[t_yb[hb]])
            for dc in range(8):
                for kc in range(8):
                    k.mm(PS[0][:], wg[:, kc, dc * 128:(dc + 1) * 128], hxb[hb][:, kc, :], kc == 0, kc == 7,
                         [t_wg, t_hxb[hb]], [PT[0]])
                for kc in range(8):
                    k.mm(PS[1][:], wg[:, kc, 1024 + dc * 128:1024 + (dc + 1) * 128], hxb[hb][:, kc, :], kc == 0, kc == 7,
                         [t_wg, t_hxb[hb]], [PT[1]])
                for fc in range(4):
                    k.mm(PS[2][:], wro[:, fc, dc * 128:(dc + 1) * 128], yb[hb][:, fc, :], fc == 0, fc == 3,
                         [t_wro, t_yb[hb]], [PT[2]])
                for fc in range(4):
                    k.mm(PS[3][:], wgo[:, fc, dc * 128:(dc + 1) * 128], yb[hb][:, 4 + fc, :], fc == 0, fc == 3,
                         [t_wgo, t_yb[hb]], [PT[3]])
                k.act(sg1[:], PS[0][:], AF.Sigmoid, [PT[0]], [t_sg1])
                k.act(sg2[:], PS[1][:], AF.Sigmoid, [PT[1]], [t_sg2])
                k.tt("dve", u1[:], PS[2][:], sg1[:], ALU.mult, [PT[2], t_sg1], [t_u1])
                k.tt("dve", u2[:], PS[3][:], sg2[:], ALU.mult, [PT[3], t_sg2], [t_u2])
                k.tt("dve", mT[:, dc, :], u1[:], u2[:], ALU.add, [t_u1, t_u2], [t_mT])
            for j in range(4):
                it = bi * 4 + j
                i2 = it % 2
                for nh in range(2):
                    for dc in range(8):
                        k.mm(PS[4 + nh][:], mT[:, dc, j * 128:(j + 1) * 128], wo[:, dc, nh * 512:(nh + 1) * 512],
                             dc == 0, dc == 7, [t_mT, t_wo], [PT[4 + nh]])
                k.dma("sp", xt[i2][:], xl[it * 128:(it + 1) * 128, :], writes=[t_xt[i2]])
                for nh in range(2):
                    k.act(junk[:, 0:512], PS[4 + nh][:], AF.Square, [PT[4 + nh]], [t_junk, t_den],
                          accum_out=den[:, nh:nh + 1])
                k.tt("dve", ss[:, it:it + 1], den[:, 0:1], den[:, 1:2], ALU.add, [t_den], [t_ss])
                rstd_to(rs[:, it:it + 1], ss[:, it:it + 1], 1.0 / D, [t_ss], t_rs)
                for nh in range(2):
                    hs = slice(nh * 512, (nh + 1) * 512)
                    k.stt(x1[i2][:, hs], PS[4 + nh][:], rs[:, it:it + 1], gbc[0][:, hs], ALU.mult, ALU.mult,
                          [PT[4 + nh], t_rs, t_gbc[0]], [t_x1[i2]])
                k.tt("dve", x1[i2][:], x1[i2][:], xt[i2][:], ALU.add, [t_x1[i2], t_xt[i2]], [t_x1[i2]])
                k.dma("sp", x1_d[it * 128:(it + 1) * 128, :], x1[i2][:], reads=[t_x1[i2]])
                if it == 0:
                    dump("x1_0", x1[i2][:], [t_x1[i2]], [128, D])
                norm_transpose((xn, t_xn), x1[i2][:], t_x1[i2], 16 + it, 4, 5,
                               h2T[:, :, it * 128:(it + 1) * 128], t_h2T[it], 6, f32out=(h2f, t_h2f))
                for kc in range(8):
                    k.mm(PS[0][:, 0:64], h2f[:, kc, :], wr[:, kc, :], kc == 0, kc == 7, [t_h2f, t_wr], [PT[0]])
                k.act(sc[:], PS[0][:, 0:64], AF.Exp, [PT[0]], [t_sc], scale=-1.0)
                k.ts("dve", sc[:], sc[:], 1.0, None, ALU.add, ALU.bypass, [t_sc], [t_sc])
                k.op("dve", lambda g: g.reciprocal(out=sc[:], in_=sc[:]), [t_sc], [t_sc])
                k.tt("dve", bs[:], sc[:], rb[:], ALU.add, [t_sc, t_rb], [t_bs])
                for gidx in range(8):
                    k.op("dve", lambda g: g.max(out=m8[:, gidx, :], in_=bs[:, gidx * 8:(gidx + 1) * 8]), [t_bs], [t_m8])
                k.tt("dve", grp_[:], m8[:, :, 0], m8[:, :, 1], ALU.add, [t_m8], [t_grp])
                k.op("dve", lambda g: g.max(out=g8[:], in_=grp_[:]), [t_grp], [t_g8])
                k.ts("dve", pen[:], grp_[:], g8[:, 3:4], 1.0e9, ALU.is_ge, ALU.mult, [t_grp, t_g8], [t_pen])
                k.ts("dve", pen[:], pen[:], -1.0e9, None, ALU.add, ALU.bypass, [t_pen], [t_pen])
                k.tt("dve", cand[:].rearrange("p (g m) -> p g m", m=8), bs[:].rearrange("p (g m) -> p g m", m=8),
                     pen[:].unsqueeze(2).broadcast_to([128, 8, 8]), ALU.add, [t_bs, t_pen], [t_cand])
                k.op("dve", lambda g: g.max(out=c8[:], in_=cand[:]), [t_cand], [t_c8])
                k.ts("dve", cand[:], cand[:], c8[:, 7:8], None, ALU.is_ge, ALU.bypass, [t_cand, t_c8], [t_cand])
                k.tt("dve", cand[:], cand[:], sc[:], ALU.mult, [t_cand, t_sc], [t_cand])
                k.op("dve", lambda g: g.reduce_sum(out=den[:, 0:1], in_=cand[:], axis=mybir.AxisListType.X),
                     [t_cand], [t_den])
                k.op("dve", lambda g: g.reciprocal(out=den[:, 1:2], in_=den[:, 0:1]), [t_den], [t_den])
                k.ts("dve", wts[:, it, 0:64], cand[:], den[:, 1:2], 2.5, ALU.mult, ALU.mult, [t_cand, t_den], [t_wts[it]])
                k.memset("dve", wts[:, it, 64:65], 1.0, [t_wts[it]])
        dump("wts", wts[:], t_wts, [128, 16, NE])
        dump("h2T", h2T[:], t_h2T, [128, 8, NTOK], BF16)
        k.barrier()
    if stop_after == "P3":
        return finish(nc, k, out_toks, out_d)

    with ExitStack() as es:
        A = lambda name, shape, dt=F32: es.enter_context(nc.sbuf_tensor("s_" + name, list(shape), dt))
        oacc = A("moe_acc", [128, 16, D]); t_oacc = [T() for _ in range(16)]
        wgu = [A(f"wgu{i}", [128, 8, 512], BF16) for i in range(2)]; t_wgu = [T(), T()]
        wdn = [A(f"wdn{i}", [128, 2, D], BF16) for i in range(2)]; t_wdn = [T(), T()]
        sgm = [A(f"sgm{i}", [128, 256]) for i in range(2)]; t_sgm = [T(), T()]
        ab = [A(f"ab{i}", [128, 256], BF16) for i in range(2)]; t_ab = [T(), T()]
        aT = [A(f"aT{i}", [128, 256], BF16) for i in range(2)]; t_aT = [T(), T()]
        xt = [A(f"xt5{i}", [128, D]) for i in range(2)]; t_xt = [T(), T()]
        fo = [A(f"fo{i}", [128, D]) for i in range(2)]; t_fo = [T(), T()]
        steps = [(e, it) for e in range(NE) for it in range(16)]
        NSTEP = len(steps)
        loaded = set()

        def load_w(e):
            if e >= NE or e in loaded:
                return
            loaded.add(e)
            eb = e % 2
            k.dma("pool", wgu[eb][:], w_gu_d[e].rearrange("(k p) n -> p k n", p=128), writes=[t_wgu[eb]])
            k.dma("pool", wdn[eb][:], w_dn_d[e].rearrange("(k p) n -> p k n", p=128), writes=[t_wdn[eb]])

        def gu_act(si):
            e, it = steps[si]
            eb, i2 = e % 2, si % 2
            for kc in range(8):
                k.mm(PS[i2][:], h2T[:, kc, it * 128:(it + 1) * 128], wgu[eb][:, kc, :], kc == 0, kc == 7,
                     [t_h2T[it], t_wgu[eb]], [PT[i2]])
            k.act(sgm[i2][:], PS[i2][:, 0:256], AF.Silu, [PT[i2]], [t_sgm[i2]])
            k.stt(ab[i2][:], PS[i2][:, 256:512], wts[:, it, e:e + 1], sgm[i2][:], ALU.mult, ALU.mult,
                  [PT[i2], t_wts[it], t_sgm[i2]], [t_ab[i2]])

        def tr_act(si):
            i2 = si % 2
            ptr = 2 + i2
            for fc in range(2):
                k.tr(PSB[ptr][:, fc * 128:(fc + 1) * 128], ab[i2][:, fc * 128:(fc + 1) * 128], identb,
                     [t_ab[i2], t_cstb], [PT[ptr]])
            k.cp("act", aT[i2][:], PSB[ptr][:, 0:256], [PT[ptr]], [t_aT[i2]])

        def down_acc(si):
            e, it = steps[si]
            eb, i2 = e % 2, si % 2
            pd0 = 4 + 2 * i2
            for nh in range(2):
                for fc in range(2):
                    k.mm(PS[pd0 + nh][:], aT[i2][:, fc * 128:(fc + 1) * 128], wdn[eb][:, fc, nh * 512:(nh + 1) * 512],
                         fc == 0, fc == 1, [t_aT[i2], t_wdn[eb]], [PT[pd0 + nh]])
            for nh in range(2):
                hs = slice(nh * 512, (nh + 1) * 512)
                if e == 0:
                    k.cp("act", oacc[:, it, hs], PS[pd0 + nh][:], [PT[pd0 + nh]], [t_oacc[it]])
                else:
                    k.tt("dve", oacc[:, it, hs], oacc[:, it, hs], PS[pd0 + nh][:], ALU.add,
                         [t_oacc[it], PT[pd0 + nh]], [t_oacc[it]])

        load_w(0)
        load_w(1)
        gu_act(0)
        for si in range(NSTEP + 1):
            if si + 1 < NSTEP:
                gu_act(si + 1)
            if si < NSTEP:
                tr_act(si)
            if si >= 1:
                down_acc(si - 1)
                if steps[si - 1][1] == 15:
                    load_w(steps[si - 1][0] + 2)
        dump("moe0", oacc[:, 0, :], [t_oacc[0]], [128, D])
        for it in range(16):
            i2 = it % 2
            k.dma("sp", xt[i2][:], x1_d[it * 128:(it + 1) * 128, :], writes=[t_xt[i2]])
            k.act(junk[:], oacc[:, it, :], AF.Square, [t_oacc[it]], [t_junk, t_ss], accum_out=ss[:, 32 + it % 8:33 + it % 8])
            rstd_to(rs[:, 32 + it % 8:33 + it % 8], ss[:, 32 + it % 8:33 + it % 8], 1.0 / D, [t_ss], t_rs)
            k.stt(fo[i2][:], oacc[:, it, :], rs[:, 32 + it % 8:33 + it % 8], gbc[1][:], ALU.mult, ALU.mult,
                  [t_oacc[it], t_rs, t_gbc[1]], [t_fo[i2]])
            k.tt("dve", fo[i2][:], fo[i2][:], xt[i2][:], ALU.add, [t_fo[i2], t_xt[i2]], [t_fo[i2]])
            out_toks.append(k.dma("sp", out_d[it * 128:(it + 1) * 128, :], fo[i2][:], reads=[t_fo[i2]]))
    return finish(nc, k, out_toks, out_d)


def finish(nc, k, out_toks, out_d):
    for tok in k.all_tokens():
        if tok[3] == "dma":
            semname, sem, val, owner = tok
            if k.waited["sp"].get(semname, 0) < val:
                nc.sync.wait_ge(sem, val)
                k.waited["sp"][semname] = val
    return nc


def _consts():
    p = np.arange(128, dtype=np.float32)[:, None]
    f = np.arange(128, dtype=np.float32)[None, :]
    c = np.zeros((128, 11, 128), np.float32)
    c[:, 0] = (p == f)
    c[:, 1] = 1.0
    c[:, 2] = (p > f)
    c[:, 3] = (p >= f)
    c[:, 4] = (p < f)
    c[:, 5] = (p <= f)
    c[:, 6] = np.maximum(f - p, 0)
    c[:, 7] = np.maximum(p - f, 0)
    c[:, 8] = f + 1 + 0 * p
    c[:, 9] = 128 - f + 0 * p
    c[:, 10, 0] = p[:, 0]
    c[:, 10, 1] = 127 - p[:, 0]
    c[:, 10, 2] = 128.0
    cb = np.zeros((128, 2, 128), np.float32)
    cb[:, 0] = (p == f)
    cb[:, 1] = 1.0
    return c, cb


def _rope_tables(flip):
    t = np.arange(NLAT)
    tg = (NLAT - 1 - t) if flip else t
    rows = (tg // 64).astype(np.float32)
    cols = (tg % 64).astype(np.float32)
    inv = (np.float32(10000.0) ** (-np.arange(32, dtype=np.float32) / np.float32(32))).astype(np.float32)
    ang = np.concatenate([rows[:, None] * inv, cols[:, None] * inv], axis=-1).astype(np.float32)
    cos = np.cos(ang).astype(np.float32)
    sin = np.sin(ang).astype(np.float32)
    C2 = np.concatenate([cos, cos], axis=1).T
    S2 = np.concatenate([-sin, sin], axis=1).T
    tab = np.zeros((128, 4, NS), np.float32)
    tab[:, 0, :NLAT] = C2 * QSC
    tab[:, 1, :NLAT] = S2 * QSC
    tab[:, 0, NLAT:] = QSC
    tab[:, 2, :NLAT] = C2
    tab[:, 3, :NLAT] = S2
    return tab


def fm(v):
    return np.ascontiguousarray(v.reshape(-1, 128).T)


def prep_inputs(inp, c):
    b, hf = c // 2, c % 2
    flip = hf == 1
    f32 = np.float32
    x = np.asarray(inp["x"][b], f32)
    ctx = np.asarray(inp["ctx"][b], f32)
    if flip:
        x = x[::-1]
        ctx = ctx[::-1]
    d = {}
    d["xl"] = np.ascontiguousarray(x)
    d["ctxl"] = np.ascontiguousarray(ctx)
    cv = np.stack([fm(np.asarray(inp["c"][b], f32)), fm(np.asarray(inp["c_ctx"], f32))], axis=-1)
    d["cvec"] = np.ascontiguousarray(cv)
    d["w_mod"] = np.ascontiguousarray(np.asarray(inp["w_mod"][0], f32))
    d["bmod"] = fm(np.asarray(inp["b_mod"][0], f32))
    d["gains"] = np.ascontiguousarray(np.stack([fm(np.asarray(inp[n][0], f32)) for n in
                                                ("norm_mix_pre", "norm_mix_post", "norm_ffn_pre", "norm_ffn_post")], axis=1))
    cst, cstb = _consts()
    d["cst"] = cst
    d["cstb"] = cstb
    d["rope"] = _rope_tables(flip)
    w_in = np.asarray(inp["w_in"][0], f32)
    Q0 = 2064
    sw = (np.arange(128) + 64) % 128
    heads = []
    for h in range(4):
        hs = slice(h * 128, (h + 1) * 128)
        rk = w_in[:, 0:512][:, hs]
        rv = w_in[:, 512:1024][:, hs]
        gk = w_in[:, 1024:1536][:, hs]
        gv = w_in[:, 1536:2048][:, hs]
        rq = w_in[:, Q0:Q0 + 512][:, hs]
        rg = w_in[:, Q0 + 512:Q0 + 1024][:, hs]
        gq = w_in[:, Q0 + 1024:Q0 + 1536][:, hs]
        gz = w_in[:, Q0 + 1536:Q0 + 2048][:, hs]
        heads.append(np.concatenate([rk, rk[:, sw], rv, gk, gv, rq, rq[:, sw], rg, gq, gz], axis=1))
    d["w_in_h"] = np.ascontiguousarray(np.stack(heads, 0))
    gabc = w_in[:, 2048:2064].reshape(D, 2, 2, 4)
    dirs = [1, 0] if flip else [0, 1]
    d["w_gab"] = np.ascontiguousarray(gabc[:, :, dirs, :].reshape(D, 16))
    d["w_gates"] = np.ascontiguousarray(w_in[:, Q0 + 2048:Q0 + 4096])
    conv = np.asarray(inp["gdn_conv"][0], f32)
    if flip:
        conv = conv[::-1]
    cw = np.zeros((128, 4, 3, 3), f32)
    for h in range(4):
        for fam in range(3):
            cw[:, h, fam, :] = conv[:, fam * 512 + h * 128: fam * 512 + (h + 1) * 128].T
    d["convw"] = cw
    hpv = np.concatenate([np.asarray(inp["ret_log_decay"][0], f32)[dirs].reshape(-1),
                          np.asarray(inp["gdn_a_log"][0], f32)[dirs].reshape(-1),
                          np.asarray(inp["gdn_dt_bias"][0], f32)[dirs].reshape(-1)])
    d["hp"] = np.ascontiguousarray(np.broadcast_to(hpv[None, :], (128, 24)).astype(f32))
    d["nw"] = np.ascontiguousarray(np.stack([fm(np.asarray(inp["ret_gn_w"][0], f32)),
                                             fm(np.asarray(inp["gdn_norm_w"][0], f32))], axis=1))
    d["w_ro"] = np.ascontiguousarray(np.asarray(inp["w_ret_out"][0], f32))
    d["w_go"] = np.ascontiguousarray(np.asarray(inp["w_gdn_out"][0], f32))
    d["w_o"] = np.ascontiguousarray(np.asarray(inp["w_o"][0], f32))
    d["w_r"] = np.ascontiguousarray(np.asarray(inp["w_router"][0], f32))
    d["rb"] = np.ascontiguousarray(np.broadcast_to(np.asarray(inp["router_bias"][0], f32)[None, :], (128, 64)).astype(f32))
    return d


_SHARED = {}


def shared_inputs(inp):
    f32 = np.float32
    wg = np.asarray(inp["w_gate"][0], f32)
    wu = np.asarray(inp["w_up"][0], f32)
    gu = np.empty((NE, D, 512), f32)
    gu[:64, :, :256] = wg
    gu[:64, :, 256:] = wu
    gu[64, :, :256] = np.asarray(inp["w_sh_gate"][0], f32)
    gu[64, :, 256:] = np.asarray(inp["w_sh_up"][0], f32)
    dn = np.empty((NE, 256, D), f32)
    dn[:64] = np.asarray(inp["w_down"][0], f32)
    dn[64] = np.asarray(inp["w_sh_down"][0], f32)
    return {"w_gu": gu, "w_dn": dn}


def kernel(**inputs):
    nc = build()
    sh = shared_inputs(inputs)
    in_maps = []
    for c in range(8):
        d = prep_inputs(inputs, c)
        d.update(sh)
        in_maps.append(d)
    res = run_bass_kernel_spmd(nc, in_maps, core_ids=list(range(8)))
    out = np.empty((4, NLAT, D), np.float32)
    for c in range(8):
        o = np.asarray(res.results[c]["out"], np.float32)
        b, hf = c // 2, c % 2
        if hf == 0:
            out[b, :NTOK] = o
        else:
            out[b, NTOK:] = o[::-1]
    return out
```

```python
from contextlib import ExitStack
import numpy as np
import concourse.bass as bass
import concourse.mybir as mybir
from concourse.bass_utils import run_bass_kernel_spmd

F32 = mybir.dt.float32
BF16 = mybir.dt.bfloat16
AF = mybir.ActivationFunctionType
ALU = mybir.AluOpType

D = 1024
NTOK = 2048
NLAT = 4096
NCTX = 256
NS = NLAT + NCTX
NT = NS // 128
EPS = 1e-6
NE = 65
QSC = float(128 ** -0.5)


class T:
    __slots__ = ("name", "w", "r")

    def __init__(self, name=""):
        self.name = name
        self.w = None
        self.r = {}


class KB:
    def __init__(self, nc):
        self.nc = nc
        self.eng = {"pe": nc.tensor, "act": nc.scalar, "dve": nc.vector, "pool": nc.gpsimd, "sp": nc.sync}
        self.sem = {k: nc.alloc_semaphore("sem_" + k) for k in ["pe", "act", "dve", "pool"]}
        self.cnt = {k: 0 for k in self.sem}
        self.waited = {k: {} for k in self.eng}
        self.ring = {}
        for q in ("sp", "pool"):
            self.ring[q] = dict(sems=[nc.alloc_semaphore(f"dq_{q}_{i}") for i in range(12)], cnt=[0] * 12, idx=0)

    def _wait(self, e, tok):
        semname, sem, val, owner = tok
        if owner == e and e == "pe":
            return
        w = self.waited[e]
        if w.get(semname, 0) >= val:
            return
        self.eng[e].wait_ge(sem, val)
        w[semname] = val

    def _deps(self, e, reads, writes):
        for t in reads:
            if t.w is not None:
                self._wait(e, t.w)
        for t in writes:
            if t.w is not None:
                self._wait(e, t.w)
            for tok in t.r.values():
                self._wait(e, tok)

    def _record(self, tok, reads, writes):
        for t in reads:
            t.r[tok[0]] = tok
        for t in writes:
            t.w = tok
            t.r = {}

    def op(self, e, fn, reads=(), writes=()):
        self._deps(e, reads, writes)
        ins = fn(self.eng[e])
        self.cnt[e] += 1
        ins.then_inc(self.sem[e], 1)
        tok = ("sem_" + e, self.sem[e], self.cnt[e], e)
        self._record(tok, reads, writes)
        return tok

    def dma(self, q, out, in_, reads=(), writes=(), **kw):
        rg = self.ring[q]
        i = rg["idx"] % len(rg["sems"])
        rg["idx"] += 1
        sem = rg["sems"][i]
        name = f"dq_{q}_{i}"
        if rg["cnt"][i] > 0:
            self._wait(q, (name, sem, rg["cnt"][i] * 16, "dma"))
        self._deps(q, reads, writes)
        self.eng[q].dma_start(out=out, in_=in_, **kw).then_inc(sem, 16)
        rg["cnt"][i] += 1
        tok = (name, sem, rg["cnt"][i] * 16, "dma")
        self._record(tok, reads, writes)
        return tok

    def all_tokens(self):
        toks = [("sem_" + e, self.sem[e], self.cnt[e], e) for e in self.sem if self.cnt[e] > 0]
        for q, rg in self.ring.items():
            for i, c in enumerate(rg["cnt"]):
                if c > 0:
                    toks.append((f"dq_{q}_{i}", rg["sems"][i], c * 16, "dma"))
        return toks

    def barrier(self):
        toks = self.all_tokens()
        for e in self.eng:
            for tok in toks:
                semname, sem, val, owner = tok
                w = self.waited[e]
                if w.get(semname, 0) >= val:
                    continue
                self.eng[e].wait_ge(sem, val)
                w[semname] = val

    def mm(self, out, lhsT, rhs, start, stop, reads, writes):
        return self.op("pe", lambda e: e.matmul(out, lhsT=lhsT, rhs=rhs, start=start, stop=stop), reads, writes)

    def tr(self, out, in_, ident, reads, writes):
        return self.op("pe", lambda e: e.transpose(out, in_, ident), reads, writes)

    def act(self, out, in_, func, reads, writes, **kw):
        return self.op("act", lambda e: e.activation(out=out, in_=in_, func=func, **kw), reads, writes)

    def ts(self, e, out, in0, s1, s2, op0, op1, reads, writes):
        return self.op(e, lambda g: g.tensor_scalar(out=out, in0=in0, scalar1=s1, scalar2=s2, op0=op0, op1=op1),
                       reads, writes)

    def tt(self, e, out, in0, in1, op, reads, writes):
        return self.op(e, lambda g: g.tensor_tensor(out=out, in0=in0, in1=in1, op=op), reads, writes)

    def stt(self, out, in0, scalar, in1, op0, op1, reads, writes):
        return self.op("dve", lambda g: g.scalar_tensor_tensor(out=out, in0=in0, scalar=scalar, in1=in1,
                                                                op0=op0, op1=op1), reads, writes)

    def cp(self, e, out, in_, reads, writes):
        if e == "act":
            return self.act(out, in_, AF.Copy, reads, writes)
        return self.op(e, lambda g: g.tensor_copy(out=out, in_=in_), reads, writes)

    def memset(self, e, ap, val, writes):
        return self.op(e, lambda g: g.memset(ap, val), (), writes)


def build(dbg=(), stop_after=None, noq=False):
    nc = bass.Bass("TRN2", target_bir_lowering=False)
    k = KB(nc)

    def din(name, shape, dt=F32):
        return nc.dram_tensor(name, list(shape), dt, kind="ExternalInput").ap()

    xl = din("xl", [NLAT, D])
    ctxl = din("ctxl", [NCTX, D])
    cvec = din("cvec", [128, 8, 2])
    w_mod = din("w_mod", [D, 6 * D])
    bmod_d = din("bmod", [128, 48])
    gains_d = din("gains", [128, 4, 8])
    cst_d = din("cst", [128, 11, 128])
    cstb_d = din("cstb", [128, 2, 128])
    rope_d = din("rope", [128, 4, NS])
    w_in_h = din("w_in_h", [4, D, 1280])
    w_gab_d = din("w_gab", [D, 16])
    w_gates_d = din("w_gates", [D, 2048])
    convw_d = din("convw", [128, 4, 3, 3])
    hp_d = din("hp", [128, 24])
    nw_d = din("nw", [128, 2, 4])
    w_ro_d = din("w_ro", [512, D])
    w_go_d = din("w_go", [512, D])
    w_o_d = din("w_o", [D, D])
    w_r_d = din("w_r", [D, 64])
    rb_d = din("rb", [128, 64])
    ne_decl = NE if stop_after is None else 1
    w_gu_d = din("w_gu", [ne_decl, D, 512])
    w_dn_d = din("w_dn", [ne_decl, 256, D])
    out_d = nc.dram_tensor("out", [NTOK, D], F32, kind="ExternalOutput").ap()
    hx_d = nc.dram_tensor("hx_d", [128, 8, NS], BF16, kind="Internal").ap()
    y_d = nc.dram_tensor("y_d", [8, 128, NTOK], BF16, kind="Internal").ap()
    x1_d = nc.dram_tensor("x1_d", [NTOK, D], F32, kind="Internal").ap()
    out_toks = []

    def dump(name, ap_sb, tls, shape, dt=F32):
        if name not in dbg:
            return
        d = nc.dram_tensor("dbg_" + name, list(shape), dt, kind="ExternalOutput").ap()
        out_toks.append(k.dma("sp", d, ap_sb, reads=tls))

    PS = [nc.alloc_psum_tensor(f"ps{i}", [128, 512], F32) for i in range(8)]
    PSB = [p[:].bitcast(BF16) for p in PS]
    PT = [T(f"ps{i}") for i in range(8)]

    def P(name, shape, dt=F32):
        return nc.alloc_sbuf_tensor("s_" + name, list(shape), dt)

    cst = P("cst", [128, 11, 128]); t_cst = T()
    k.dma("sp", cst[:], cst_d, writes=[t_cst])
    cstb = P("cstb", [128, 2, 128], BF16); t_cstb = T()
    k.dma("pool", cstb[:], cstb_d, writes=[t_cstb])
    ident = cst[:, 0, :]
    ones = cst[:, 1, :]
    L_st, L_in, U_st, U_in = cst[:, 2, :], cst[:, 3, :], cst[:, 4, :], cst[:, 5, :]
    identb = cstb[:, 0, :]
    onesb = cstb[:, 1, :]
    hp = P("hp", [128, 24]); t_hp = T()
    k.dma("sp", hp[:], hp_d, writes=[t_hp])
    nw = P("nw", [128, 2, 4]); t_nw = T()
    k.dma("sp", nw[:], nw_d, writes=[t_nw])
    convw = P("convw", [128, 4, 3, 3]); t_convw = T()
    k.dma("sp", convw[:], convw_d, writes=[t_convw])
    mv = P("mv", [128, 8, 8]); t_mv = T()
    gbc = [P(f"gbc{i}", [128, D]) for i in range(2)]
    t_gbc = [T() for _ in range(2)]
    ss = P("ss", [128, 64]); t_ss = T()
    rs = P("rs", [128, 64]); t_rs = T()
    junk = P("junk", [128, D], BF16); t_junk = T()
    g_la = P("g_la", [128, NT, 8]); g_beta = P("g_beta", [128, NT, 8]); g_nbeta = P("g_nbeta", [128, NT, 8])
    g_g = P("g_g", [128, NT, 8]); g_beg = P("g_beg", [128, NT, 8]); g_egl = P("g_egl", [128, NT, 8])
    g_kts = P("g_kts", [128, NT, 8])
    g_ng = P("g_ng", [128, NT, 8])
    t_gs = T()

    def sigmoid_to(out_ap, in_ap, reads, t_out):
        k.act(out_ap, in_ap, AF.Exp, reads, [t_out], scale=-1.0)
        k.act(out_ap, out_ap, AF.Ln, [t_out], [t_out], bias=1.0)
        k.act(out_ap, out_ap, AF.Exp, [t_out], [t_out], scale=-1.0)

    def rstd_to(out_ap, in_ap, scale, reads, t_out, post_scale=None):
        k.act(out_ap, in_ap, AF.Ln, reads, [t_out], scale=scale, bias=EPS)
        if post_scale is None:
            k.act(out_ap, out_ap, AF.Exp, [t_out], [t_out], scale=-0.5)
        else:
            k.act(out_ap, out_ap, AF.Exp, [t_out], [t_out], scale=-0.5, bias=float(np.log(post_scale)))

    def norm_transpose(es_xn, src_ap, tl_src, it, Acol, Bcol, dst_ap, t_dst, pbank, f32out=None, stage=0):
        xn_, t_xn_ = es_xn
        if stage != 2:
            k.act(junk[:], src_ap, AF.Square, [tl_src], [t_junk, t_ss], accum_out=ss[:, it:it + 1])
            rstd_to(rs[:, it:it + 1], ss[:, it:it + 1], 1.0 / D, [t_ss], t_rs)
            k.ts("dve", xn_[:], src_ap, rs[:, it:it + 1], None, ALU.mult, ALU.bypass, [tl_src, t_rs], [t_xn_])
            for kc in range(8):
                pb = pbank + kc // 4
                k.tr(PS[pb][:, (kc % 4) * 128:(kc % 4 + 1) * 128], xn_[:, kc * 128:(kc + 1) * 128], ident,
                     [t_xn_, t_cst], [PT[pb]])
        if stage != 1:
            for kc in range(8):
                pb = pbank + kc // 4
                o = dst_ap[:, kc, :] if f32out is None else f32out[0][:, kc, :]
                if kc % 2 == 0:
                    k.act(o, PS[pb][:, (kc % 4) * 128:(kc % 4 + 1) * 128], AF.Identity,
                          [PT[pb], t_mv], [t_dst if f32out is None else f32out[1]],
                          scale=mv[:, Acol, kc:kc + 1], bias=mv[:, Bcol, kc:kc + 1])
                else:
                    k.ts("dve", o, PS[pb][:, (kc % 4) * 128:(kc % 4 + 1) * 128], mv[:, Acol, kc:kc + 1],
                         mv[:, Bcol, kc:kc + 1], ALU.mult, ALU.add, [PT[pb], t_mv],
                         [t_dst if f32out is None else f32out[1]])
            if f32out is not None:
                k.cp("act", dst_ap, f32out[0][:], [f32out[1]], [t_dst])
        return
        k.act(junk[:], src_ap, AF.Square, [tl_src], [t_junk, t_ss], accum_out=ss[:, it:it + 1])
        rstd_to(rs[:, it:it + 1], ss[:, it:it + 1], 1.0 / D, [t_ss], t_rs)
        k.ts("dve", xn_[:], src_ap, rs[:, it:it + 1], None, ALU.mult, ALU.bypass, [tl_src, t_rs], [t_xn_])
        for kc in range(8):
            pb = pbank + kc // 4
            k.tr(PS[pb][:, (kc % 4) * 128:(kc % 4 + 1) * 128], xn_[:, kc * 128:(kc + 1) * 128], ident,
                 [t_xn_, t_cst], [PT[pb]])
        for kc in range(8):
            pb = pbank + kc // 4
            o = dst_ap[:, kc, :] if f32out is None else f32out[0][:, kc, :]
            k.act(o, PS[pb][:, (kc % 4) * 128:(kc % 4 + 1) * 128], AF.Identity,
                  [PT[pb], t_mv], [t_dst if f32out is None else f32out[1]],
                  scale=mv[:, Acol, kc:kc + 1], bias=mv[:, Bcol, kc:kc + 1])
        if f32out is not None:
            k.cp("act", dst_ap, f32out[0][:], [f32out[1]], [t_dst])

    with ExitStack() as es:
        A = lambda name, shape, dt=F32: es.enter_context(nc.sbuf_tensor("s_" + name, list(shape), dt))
        cv = A("cv", [128, 8, 2]); t_cv = T()
        k.dma("sp", cv[:], cvec, writes=[t_cv])
        bmod = A("bmod", [128, 48]); t_bmod = T()
        k.dma("sp", bmod[:], bmod_d, writes=[t_bmod])
        gains = A("gains", [128, 4, 8]); t_gains = T()
        k.dma("sp", gains[:], gains_d, writes=[t_gains])
        sg = A("sg", [128, 8, 2]); t_sg = T()
        scb = A("scb", [128, 8, 2], BF16); t_scb = T()
        sigmoid_to(sg[:], cv[:], [t_cv], t_sg)
        k.tt("dve", scb[:], sg[:], cv[:], ALU.mult, [t_sg, t_cv], [t_scb])
        wmf = [A(f"wmf{i}", [128, 8, 512]) for i in range(2)]
        t_wmf = [T() for _ in range(2)]
        wmb = [A(f"wmb{i}", [128, 8, 512], BF16) for i in range(2)]
        t_wmb = [T() for _ in range(2)]
        wm_v = w_mod.rearrange("(k p) n -> p k n", p=128)
        for j in range(12):
            b = j % 2
            k.dma("sp", wmf[b][:], wm_v[:, :, j * 512:(j + 1) * 512], writes=[t_wmf[b]])
            k.cp("dve" if j % 2 == 0 else "act", wmb[b][:], wmf[b][:], [t_wmf[b]], [t_wmb[b]])
            for m in range(4):
                jc = j * 4 + m
                for kc in range(8):
                    k.mm(PS[0][:, jc * 2:jc * 2 + 2], wmb[b][:, kc, m * 128:(m + 1) * 128], scb[:, kc, :],
                         kc == 0, kc == 7, [t_wmb[b], t_scb], [PT[0]])
        mod = A("mod", [128, 48, 2]); t_mod = T()
        k.tt("dve", mod[:], PS[0][:, 0:96].rearrange("p (j t) -> p j t", t=2),
             bmod[:].unsqueeze(2).broadcast_to([128, 48, 2]), ALU.add, [PT[0], t_bmod], [t_mod])
        k.stt(mv[:, 0, :], mod[:, 8:16, 0], 1.0, gains[:, 0, :], ALU.add, ALU.mult, [t_mod, t_gains], [t_mv])
        k.cp("dve", mv[:, 1, :], mod[:, 0:8, 0], [t_mod], [t_mv])
        k.stt(mv[:, 2, :], mod[:, 8:16, 1], 1.0, gains[:, 0, :], ALU.add, ALU.mult, [t_mod, t_gains], [t_mv])
        k.cp("dve", mv[:, 3, :], mod[:, 0:8, 1], [t_mod], [t_mv])
        k.stt(mv[:, 4, :], mod[:, 32:40, 0], 1.0, gains[:, 2, :], ALU.add, ALU.mult, [t_mod, t_gains], [t_mv])
        k.cp("dve", mv[:, 5, :], mod[:, 24:32, 0], [t_mod], [t_mv])
        k.tt("dve", mv[:, 6, :], mod[:, 16:24, 0], gains[:, 1, :], ALU.mult, [t_mod, t_gains], [t_mv])
        k.tt("dve", mv[:, 7, :], mod[:, 40:48, 0], gains[:, 3, :], ALU.mult, [t_mod, t_gains], [t_mv])
        dump("mv", mv[:], [t_mv], [128, 8, 8])
        dg = A("dg", [128, 8, 128]); t_dg = T()
        for gi in range(2):
            for kc in range(8):
                k.ts("dve", dg[:, kc, :], ident, mv[:, 6 + gi, kc:kc + 1], None, ALU.mult, ALU.bypass,
                     [t_cst, t_mv], [t_dg])
            for kc in range(8):
                pb = 1 + kc // 4
                k.mm(PS[pb][:, (kc % 4) * 128:(kc % 4 + 1) * 128], ones, dg[:, kc, :], True, True,
                     [t_cst, t_dg], [PT[pb]])
            k.cp("act", gbc[gi][:, 0:512], PS[1][:], [PT[1]], [t_gbc[gi]])
            k.cp("act", gbc[gi][:, 512:1024], PS[2][:], [PT[2]], [t_gbc[gi]])
        dump("gbc0", gbc[0][:], [t_gbc[0]], [128, D])

        xt = [A(f"xt{i}", [128, D]) for i in range(3)]; t_xt = [T() for _ in range(3)]
        xn = [A(f"xn{i}", [128, D]) for i in range(2)]; t_xn = [T() for _ in range(2)]
        hxo = [A(f"hxo{i}", [128, 8, 128], BF16) for i in range(2)]; t_hxo = [T() for _ in range(2)]
        def p1_load(it):
            if it >= NT:
                return
            src = xl[it * 128:(it + 1) * 128, :] if it < 32 else ctxl[(it - 32) * 128:(it - 31) * 128, :]
            k.dma("sp", xt[it % 3][:], src, writes=[t_xt[it % 3]])

        def p1_stage(it, stage):
            b3, b2 = it % 3, it % 2
            lat = it < 32
            if stage == 1:
                p1_load(it + 1)
            norm_transpose((xn[b2], t_xn[b2]), xt[b3][:], t_xt[b3], it, 0 if lat else 2, 1 if lat else 3,
                           hxo[b2][:], t_hxo[b2], 3 + 2 * b2, stage=stage)
            if stage == 2:
                k.dma("pool", hx_d[:, :, it * 128:(it + 1) * 128], hxo[b2][:], reads=[t_hxo[b2]])

        p1_load(0)
        p1_stage(0, 1)
        for it in range(NT):
            if it + 1 < NT:
                p1_stage(it + 1, 1)
            p1_stage(it, 2)
        k.barrier()
    if stop_after == "A":
        return finish(nc, k, out_toks, out_d)

    with ExitStack() as es:
        A = lambda name, shape, dt=F32: es.enter_context(nc.sbuf_tensor("s_" + name, list(shape), dt))
        wgab = A("wgab", [128, 8, 16], BF16); t_wgab = T()
        k.dma("pool", wgab[:], w_gab_d.rearrange("(k p) n -> p k n", p=128), writes=[t_wgab])
        hxb = [A(f"hxbg{i}", [128, 8, 512], BF16) for i in range(2)]; t_hxb = [T(), T()]
        gab = A("gab", [128, NT, 16]); t_gab = T()
        for bi in range(9):
            tok0 = bi * 512
            n = 512 if bi < 8 else 256
            hb = bi % 2
            k.dma("sp", hxb[hb][:, :, :n], hx_d[:, :, tok0:tok0 + n], writes=[t_hxb[hb]])
            pb = bi % 2
            for j in range(n // 128):
                for kc in range(8):
                    k.mm(PS[pb][:, j * 16:(j + 1) * 16], hxb[hb][:, kc, j * 128:(j + 1) * 128], wgab[:, kc, :],
                         kc == 0, kc == 7, [t_hxb[hb], t_wgab], [PT[pb]])
            k.cp("act", gab[:, bi * 4:bi * 4 + n // 128, :],
                 PS[pb][:, 0:(n // 128) * 16].rearrange("p (j c) -> p j c", c=16), [PT[pb]], [t_gab])
        dump("gab", gab[:], [t_gab], [128, NT, 16])
        negA = A("negA", [128, 8]); t_negA = T()
        k.act(negA[:], hp[:, 8:16], AF.Exp, [t_hp], [t_negA])
        k.ts("dve", negA[:], negA[:], -1.0, None, ALU.mult, ALU.bypass, [t_negA], [t_negA])
        z = A("z", [128, NT, 8]); t_z = T()
        k.tt("dve", z[:], gab[:, :, 0:8], hp[:, 16:24].unsqueeze(1).broadcast_to([128, NT, 8]), ALU.add,
             [t_gab, t_hp], [t_z])
        k.act(z[:], z[:], AF.Exp, [t_z], [t_z])
        k.act(z[:], z[:], AF.Ln, [t_z], [t_z], bias=1.0)
        k.tt("dve", g_la[:], z[:], negA[:].unsqueeze(1).broadcast_to([128, NT, 8]), ALU.mult, [t_z, t_negA], [t_gs])
        sigmoid_to(g_beta[:], gab[:, :, 8:16], [t_gab], t_gs)
        k.ts("dve", g_nbeta[:], g_beta[:], -1.0, None, ALU.mult, ALU.bypass, [t_gs], [t_gs])
        for ti in range(NT):
            pb = ti % 2
            k.mm(PS[pb][:, 0:4], U_in, g_la[:, ti, 0:4], True, True, [t_cst, t_gs], [PT[pb]])
            k.mm(PS[pb][:, 4:8], L_in, g_la[:, ti, 4:8], True, True, [t_cst, t_gs], [PT[pb]])
            k.mm(PS[pb][:, 8:16], ones, g_la[:, ti, :], True, True, [t_cst, t_gs], [PT[pb]])
            k.cp("act", g_g[:, ti, :], PS[pb][:, 0:8], [PT[pb]], [t_gs])
            k.act(g_egl[:, ti, :], PS[pb][:, 8:16], AF.Exp, [PT[pb]], [t_gs])
            k.tt("dve", g_kts[:, ti, :], PS[pb][:, 8:16], g_g[:, ti, :], ALU.subtract, [PT[pb], t_gs], [t_gs])
        k.act(g_kts[:], g_kts[:], AF.Exp, [t_gs], [t_gs])
        k.ts("dve", g_ng[:], g_g[:], -1.0, None, ALU.mult, ALU.bypass, [t_gs], [t_gs])
        k.act(g_beg[:], g_g[:], AF.Exp, [t_gs], [t_gs])
        k.tt("dve", g_beg[:], g_beg[:], g_beta[:], ALU.mult, [t_gs], [t_gs])
        dump("g_g", g_g[:], [t_gs], [128, NT, 8])
        dump("g_beta", g_beta[:], [t_gs], [128, NT, 8])
        k.barrier()
    if stop_after == "G":
        return finish(nc, k, out_toks, out_d)

    es_heads = ExitStack()
    whd2 = [es_heads.enter_context(nc.sbuf_tensor(f"s_whd{i}", [128, 8, 1280], BF16)) for i in range(2)]
    t_whd2 = [T(), T()]

    def load_whd(hh):
        wv_ = w_in_h[hh].rearrange("(k p) n -> p k n", p=128)
        for half in range(2):
            k.dma("pool", whd2[hh % 2][:, :, half * 640:(half + 1) * 640], wv_[:, :, half * 640:(half + 1) * 640],
                  writes=[t_whd2[hh % 2]])

    load_whd(0)
    for h in range(4):
        with ExitStack() as es:
            A = lambda name, shape, dt=F32: es.enter_context(nc.sbuf_tensor(f"s_{name}_{h}", list(shape), dt))
            esr = ExitStack()
            AR = lambda name, shape, dt=F32: esr.enter_context(nc.sbuf_tensor(f"s_{name}_{h}", list(shape), dt))
            whd = whd2[h % 2]; t_whd = t_whd2[h % 2]
            hxb = [A(f"hxb{i}", [128, 8, 512], BF16) for i in range(2)]; t_hxb = [T(), T()]
            rope1 = A("rope", [128, 4, 512]); rope = [rope1, rope1]; t_rope1 = T(); t_rope = [t_rope1, t_rope1]
            sgzT = A("sgzT", [128, NTOK], BF16); t_sgz = [T() for _ in range(4)]
            pck = A("pck", [128, NS + 6], BF16); t_pck = T()
            pcv = A("pcv", [128, NS + 6], BF16); t_pcv = T()
            pcq = A("pcq", [128, NTOK + 130], BF16); t_pcq = T()
            tmp1 = A("tmp1", [128, 512]); t_tmp1 = T()
            tmp2 = A("tmp2", [128, 512]); t_tmp2 = T()
            tmp3 = A("tmp3", [128, 512]); t_tmp3 = T()
            onb = [A(f"onb{i}", [128, 128], BF16) for i in range(2)]; t_onb = [T(), T()]
            ybuf = A("ybuf", [128, NTOK], BF16); t_ybuf = T()
            rkT = AR("rkT", [128, NTOK], BF16); t_rkT = [T() for _ in range(4)]
            rkblk = AR("rkblk", [128, 512], BF16); t_rkblk = T()
            rk_tm = AR("rk_tm", [128, NT, 128], BF16); t_rk_tm = [T() for _ in range(9)]
            rv_tm = AR("rv_tm", [128, NT, 128], BF16); t_rv_tm = [T() for _ in range(9)]
            rqT = AR("rqT", [128, NTOK], BF16); t_rqT = [T() for _ in range(4)]
            QF = AR("QF", [128, NTOK], BF16); QB = AR("QB", [128, NTOK], BF16); t_QFB = [T() for _ in range(4)]
            srgT = AR("srgT", [128, NTOK], BF16); t_srg = [T() for _ in range(4)]
            lgf = hp[:, h:h + 1]
            lgb = hp[:, 4 + h:5 + h]
            GF = AR("GF", [128, 128]); GB = AR("GB", [128, 128]); DmT = AR("DmT", [128, 128])
            E1 = AR("E1", [128, 128]); E2 = AR("E2", [128, 128]); wcol = AR("wcol", [128, 4])
            t_rc = T()
            k.act(GF[:], cst[:, 8, :], AF.Exp, [t_cst, t_hp], [t_rc], scale=lgf)
            k.act(GB[:], cst[:, 9, :], AF.Exp, [t_cst, t_hp], [t_rc], scale=lgb)
            k.act(wcol[:, 0:1], cst[:, 10, 1:2], AF.Exp, [t_cst, t_hp], [t_rc], scale=lgf)
            k.act(wcol[:, 1:2], cst[:, 10, 0:1], AF.Exp, [t_cst, t_hp], [t_rc], scale=lgb)
            k.act(wcol[:, 2:3], cst[:, 10, 2:3], AF.Exp, [t_cst, t_hp], [t_rc], scale=lgf)
            k.act(wcol[:, 3:4], cst[:, 10, 2:3], AF.Exp, [t_cst, t_hp], [t_rc], scale=lgb)
            k.act(E1[:], cst[:, 6, :], AF.Exp, [t_cst, t_hp], [t_rc], scale=lgf)
            k.act(E2[:], cst[:, 7, :], AF.Exp, [t_cst, t_hp], [t_rc], scale=lgb)
            k.tt("dve", E1[:], E1[:], U_st, ALU.mult, [t_rc, t_cst], [t_rc])
            k.tt("dve", E2[:], E2[:], L_st, ALU.mult, [t_rc, t_cst], [t_rc])
            k.tt("dve", E1[:], E1[:], E2[:], ALU.add, [t_rc], [t_rc])
            k.stt(DmT[:], ident, 2.0, E1[:], ALU.mult, ALU.add, [t_cst, t_rc], [t_rc])
            for (buf, tl, cols) in ((pck, t_pck, (0, 4097, 4098, 4355)), (pcv, t_pcv, (0, 4097, 4098, 4355)),
                                    (pcq, t_pcq, (0,))):
                for c in cols:
                    k.memset("pool", buf[:, c:c + 1], 0.0, [tl])

            def silu_psum_to(dst_ap, t_dst, pb, n):
                k.act(dst_ap, PS[pb][:, :n], AF.Silu, [PT[pb]], [t_dst])

            for bi in range(9):
                tok0 = bi * 512
                n = 512 if bi < 8 else 256
                own = bi < 4
                hb = bi % 2
                nt = n // 128
                k.dma("sp", hxb[hb][:, :, :n], hx_d[:, :, tok0:tok0 + n], writes=[t_hxb[hb]])
                k.dma("sp", rope[hb][:, :, :n], rope_d[:, :, tok0:tok0 + n], writes=[t_rope[hb]])

                def proj(cb, pb, ncols=n, c0=0):
                    for kc in range(8):
                        k.mm(PS[pb][:, :ncols], whd[:, kc, cb * 128:(cb + 1) * 128], hxb[hb][:, kc, c0:c0 + ncols],
                             kc == 0, kc == 7, [t_whd, t_hxb[hb]], [PT[pb]])

                def rope_to(dst_ap, t_dst, tc, ts_):
                    proj(tc[0], 0)
                    proj(tc[1], 1)
                    k.tt("dve", tmp1[:, :n], PS[0][:, :n], rope[hb][:, ts_, :n], ALU.mult, [PT[0], t_rope[hb]], [t_tmp1])
                    k.tt("dve", tmp2[:, :n], PS[1][:, :n], rope[hb][:, ts_ + 1, :n], ALU.mult, [PT[1], t_rope[hb]],
                         [t_tmp2])
                    k.tt("dve", dst_ap, tmp1[:, :n], tmp2[:, :n], ALU.add, [t_tmp1, t_tmp2], [t_dst])

                if own:
                    dst, t_dst = rkT[:, tok0:tok0 + n], t_rkT[bi]
                else:
                    dst, t_dst = rkblk[:, :n], t_rkblk
                rope_to(dst, t_dst, (0, 1), 0)
                for j in range(nt):
                    k.tr(PSB[2][:, j * 128:(j + 1) * 128], dst[:, j * 128:(j + 1) * 128], identb, [t_dst, t_cstb], [PT[2]])
                k.cp("act", rk_tm[:, bi * 4:bi * 4 + nt, :], PSB[2][:, :n].rearrange("p (j c) -> p j c", c=128),
                     [PT[2]], [t_rk_tm[bi]])
                for j in range(nt):
                    for kc in range(8):
                        k.mm(PS[3][:, j * 128:(j + 1) * 128], hxb[hb][:, kc, j * 128:(j + 1) * 128],
                             whd[:, kc, 256:384], kc == 0, kc == 7, [t_whd, t_hxb[hb]], [PT[3]])
                k.cp("act", rv_tm[:, bi * 4:bi * 4 + nt, :], PS[3][:, :n].rearrange("p (j c) -> p j c", c=128),
                     [PT[3]], [t_rv_tm[bi]])
                off = tok0 + 1 if bi < 8 else tok0 + 3
                proj(3, 4)
                k.cp("act", pck[:, off:off + n], PS[4][:, :n], [PT[4]], [t_pck])
                proj(4, 5)
                k.cp("act", pcv[:, off:off + n], PS[5][:, :n], [PT[5]], [t_pcv])
                if own:
                    rope_to(rqT[:, tok0:tok0 + n], t_rqT[bi], (5, 6), 2)
                    k.tt("pool", QF[:, tok0:tok0 + n].rearrange("p (c i) -> p c i", i=128),
                         rqT[:, tok0:tok0 + n].rearrange("p (c i) -> p c i", i=128),
                         GF[:].unsqueeze(1).broadcast_to([128, 4, 128]), ALU.mult, [t_rqT[bi], t_rc], [t_QFB[bi]])
                    k.tt("pool", QB[:, tok0:tok0 + n].rearrange("p (c i) -> p c i", i=128),
                         rqT[:, tok0:tok0 + n].rearrange("p (c i) -> p c i", i=128),
                         GB[:].unsqueeze(1).broadcast_to([128, 4, 128]), ALU.mult, [t_rqT[bi], t_rc], [t_QFB[bi]])
                    proj(7, 6)
                    silu_psum_to(srgT[:, tok0:tok0 + n], t_srg[bi], 6, n)
                    proj(9, 6)
                    silu_psum_to(sgzT[:, tok0:tok0 + n], t_sgz[bi], 6, n)
                    proj(8, 7)
                    k.cp("act", pcq[:, 1 + tok0:1 + tok0 + n], PS[7][:, :n], [PT[7]], [t_pcq])
                if bi == 4:
                    proj(8, 7, ncols=128)
                    k.cp("act", pcq[:, 2049:2049 + 128], PS[7][:, :128], [PT[7]], [t_pcq])
            if h + 1 < 4:
                load_whd(h + 1)
            if stop_after == f"H{h}a":
                k.barrier(); esr.close()
                return finish(nc, k, out_toks, out_d)
            dump(f"rkT{h}", rkT[:], t_rkT, [128, NTOK], BF16)
            dump(f"rv_tm{h}", rv_tm[:], t_rv_tm, [128, NT, 128], BF16)
            dump(f"rk_tm{h}", rk_tm[:], t_rk_tm, [128, NT, 128], BF16)
            dump(f"QF{h}", QF[:], t_QFB, [128, NTOK], BF16)
            dump(f"srgT{h}", srgT[:], t_srg, [128, NTOK], BF16)

            Sb32 = AR("Sb32", [128, 128]); t_Sb32 = T()
            Sf32 = AR("Sf32", [128, 128]); t_Sf32 = T()
            Sbst = AR("Sbst", [128, 16, 128], BF16); t_Sbst = [T() for _ in range(16)]
            Sfb = [AR(f"Sfb{i}", [128, 128], BF16) for i in range(2)]; t_Sfb = [T(), T()]
            wvb = [AR(f"wvb{i}", [128, 128], BF16) for i in range(2)]; t_wvb = [T(), T()]
            scm = [AR(f"scm{i}", [128, 128], BF16) for i in range(2)]; t_scm = [T(), T()]
            osb = AR("osb", [128, 16, 128]); t_osb = [T() for _ in range(16)]
            bst = AR("bst", [128, 16, 6]); t_bst = T()
            mvar = AR("mvar", [128, 16, 2]); t_mvar = T()
            rstd16 = AR("rstd16", [128, 16]); t_rstd16 = T()
            k.memset("dve", Sb32[:], 0.0, [t_Sb32])
            k.memset("dve", Sf32[:], 0.0, [t_Sf32])
            cnt = [0]

            def ret_update(S32, t_S32, ti, wc, dc):
                i = cnt[0] % 2
                cnt[0] += 1
                bi_ = ti // 4
                k.ts("dve", wvb[i][:], rv_tm[:, ti, :], wcol[:, wc:wc + 1], None, ALU.mult, ALU.bypass,
                     [t_rv_tm[bi_], t_rc], [t_wvb[i]])
                k.mm(PS[i][:, 0:128], rk_tm[:, ti, :], wvb[i][:], True, True, [t_rk_tm[bi_], t_wvb[i]], [PT[i]])
                k.stt(S32[:], S32[:], wcol[:, dc:dc + 1], PS[i][:, 0:128], ALU.mult, ALU.add,
                      [t_S32, t_rc, PT[i]], [t_S32])

            for ti in [33, 32] + list(range(31, 15, -1)) + list(range(15, -1, -1)):
                if ti < 16:
                    k.cp("act", Sbst[:, ti, :], Sb32[:], [t_Sb32], [t_Sbst[ti]])
                ret_update(Sb32, t_Sb32, ti, 1, 3)
            if stop_after == f"H{h}r2":
                k.barrier(); esr.close()
                return finish(nc, k, out_toks, out_d)
            for ti in (32, 33):
                ret_update(Sf32, t_Sf32, ti, 0, 2)
            for n_ in range(16):
                i = n_ % 2
                bi_ = n_ // 4
                cs = slice(n_ * 128, (n_ + 1) * 128)
                k.cp("act", Sfb[i][:], Sf32[:], [t_Sf32], [t_Sfb[i]])
                k.mm(PS[2 + i][:, 0:128], rkT[:, cs], rqT[:, cs], True, True, [t_rkT[bi_], t_rqT[bi_]], [PT[2 + i]])
                k.tt("dve", scm[i][:], PS[2 + i][:, 0:128], DmT[:], ALU.mult, [PT[2 + i], t_rc], [t_scm[i]])
                k.mm(PS[4 + i][:, 0:128], scm[i][:], rv_tm[:, n_, :], True, False, [t_scm[i], t_rv_tm[bi_]], [PT[4 + i]])
                k.mm(PS[4 + i][:, 0:128], QF[:, cs], Sfb[i][:], False, False, [t_QFB[bi_], t_Sfb[i]], [PT[4 + i]])
                k.mm(PS[4 + i][:, 0:128], QB[:, cs], Sbst[:, n_, :], False, True, [t_QFB[bi_], t_Sbst[n_]], [PT[4 + i]])
                k.act(osb[:, n_, :], PS[4 + i][:, 0:128], AF.Identity, [PT[4 + i]], [t_osb[n_], t_bst],
                      accum_out=bst[:, n_, 0:1])
                k.act(junk[:, 0:128], PS[4 + i][:, 0:128], AF.Square, [PT[4 + i]], [t_junk, t_bst],
                      accum_out=bst[:, n_, 1:2])
                ret_update(Sf32, t_Sf32, n_, 0, 2)
            if stop_after == f"H{h}r3":
                k.barrier(); esr.close()
                return finish(nc, k, out_toks, out_d)
            k.ts("dve", mvar[:, :, 0], bst[:, :, 0], 1.0 / 128, None, ALU.mult, ALU.bypass, [t_bst], [t_mvar])
            k.tt("dve", mvar[:, :, 1], mvar[:, :, 0], mvar[:, :, 0], ALU.mult, [t_mvar], [t_mvar])
            k.stt(mvar[:, :, 1], bst[:, :, 1], 1.0 / 128, mvar[:, :, 1], ALU.mult, ALU.subtract, [t_bst, t_mvar], [t_mvar])
            rstd_to(rstd16[:], mvar[:, :, 1], 1.0, [t_mvar], t_rstd16)
            dump(f"osb{h}", osb[:], t_osb, [128, 16, 128])
            for n_ in range(16):
                i = n_ % 2
                cs = slice(n_ * 128, (n_ + 1) * 128)
                k.ts("dve", onb[i][:], osb[:, n_, :], mvar[:, n_, 0:1], rstd16[:, n_:n_ + 1], ALU.subtract, ALU.mult,
                     [t_osb[n_], t_mvar, t_rstd16], [t_onb[i]])
                k.tr(PSB[6 + i][:, 0:128], onb[i][:], identb, [t_onb[i], t_cstb], [PT[6 + i]])
                k.stt(ybuf[:, cs], PSB[6 + i][:, 0:128], nw[:, 0, h:h + 1], srgT[:, cs], ALU.mult, ALU.mult,
                      [PT[6 + i], t_nw, t_srg[n_ // 4]], [t_ybuf])
            dump(f"yret{h}", ybuf[:], [t_ybuf], [128, NTOK], BF16)
            k.dma("sp", y_d[h], ybuf[:], reads=[t_ybuf])
            k.barrier()
            esr.close()
            if stop_after == f"H{h}b":
                return finish(nc, k, out_toks, out_d)

            gkT = A("gkT", [128, NS], BF16); t_gkT = T()
            gvT = A("gvT", [128, NS], BF16); t_gvT = T()
            gqT = A("gqT", [128, NTOK], BF16); t_gqT = T()
            sqb_ = [A(f"sqb{i}", [128, 512], BF16) for i in range(2)]; t_sqb_ = [T(), T()]
            ct1 = [tmp1, A("tmp1b", [128, 512])]; t_ct1 = [t_tmp1, T()]
            ct2 = [tmp2, A("tmp2b", [128, 512])]; t_ct2 = [t_tmp2, T()]
            ct3 = [tmp3, A("tmp3b", [128, 512])]; t_ct3 = [t_tmp3, T()]
            cpc = [0]

            def conv_piece(pc, t_pc, o, n, fam, dstT, t_dstT, d0, l2, qscale):
                w = convw[:, h, fam, :]
                pp = cpc[0] % 2
                cpc[0] += 1
                tmp1, t_tmp1, tmp2, t_tmp2, tmp3, t_tmp3 = ct1[pp], t_ct1[pp], ct2[pp], t_ct2[pp], ct3[pp], t_ct3[pp]
                sqb, t_sqb = sqb_[pp], t_sqb_[pp]
                pbank = 6 + pp
                k.ts("dve", tmp1[:, :n], pc[:, o - 1:o - 1 + n], w[:, 0:1], None, ALU.mult, ALU.bypass,
                     [t_pc, t_convw], [t_tmp1])
                k.stt(tmp1[:, :n], pc[:, o:o + n], w[:, 1:2], tmp1[:, :n], ALU.mult, ALU.add, [t_pc, t_convw, t_tmp1],
                      [t_tmp1])
                k.stt(tmp1[:, :n], pc[:, o + 1:o + 1 + n], w[:, 2:3], tmp1[:, :n], ALU.mult, ALU.add,
                      [t_pc, t_convw, t_tmp1], [t_tmp1])
                sigmoid_to(tmp3[:, :n], tmp1[:, :n], [t_tmp1], t_tmp3)
                if not l2:
                    k.tt("dve", dstT[:, d0:d0 + n], tmp1[:, :n], tmp3[:, :n], ALU.mult, [t_tmp1, t_tmp3], [t_dstT])
                    return
                k.tt("dve", tmp2[:, :n], tmp1[:, :n], tmp3[:, :n], ALU.mult, [t_tmp1, t_tmp3], [t_tmp2])
                k.act(sqb[:, :n], tmp2[:, :n], AF.Square, [t_tmp2], [t_sqb])
                k.mm(PS[pbank][:, :n], onesb, sqb[:, :n], True, True, [t_cstb, t_sqb], [PT[pbank]])
                rstd_to(tmp3[:, :n], PS[pbank][:, :n], 1.0, [PT[pbank]], t_tmp3, post_scale=qscale)
                k.tt("dve", dstT[:, d0:d0 + n], tmp2[:, :n], tmp3[:, :n], ALU.mult, [t_tmp2, t_tmp3], [t_dstT])

            for bi in range(9):
                tok0 = bi * 512
                n = 512 if bi < 8 else 256
                off = tok0 + 1 if bi < 8 else tok0 + 3
                conv_piece(pck, t_pck, off, n, 1, gkT, t_gkT, tok0, True, None)
                conv_piece(pcv, t_pcv, off, n, 2, gvT, t_gvT, tok0, False, None)
                if bi < 4:
                    conv_piece(pcq, t_pcq, tok0 + 1, n, 0, gqT, t_gqT, tok0, True, QSC)
            dump(f"gkT{h}", gkT[:], [t_gkT], [128, NS], BF16)
            dump(f"gvT{h}", gvT[:], [t_gvT], [128, NS], BF16)
            dump(f"gqT{h}", gqT[:], [t_gqT], [128, NTOK], BF16)

            if stop_after == f"H{h}g1":
                k.barrier()
                return finish(nc, k, out_toks, out_d)
            XAB = [A(f"XAB{s}", [128, 512]) for s in range(8)]; t_XAB = [T() for _ in range(8)]
            wkT = [A(f"wkT{s}", [128, 128], BF16) for s in range(8)]; t_wkT = [T() for _ in range(8)]
            ktl = [A(f"ktl{s}", [128, 128], BF16) for s in range(8)]; t_ktl = [T() for _ in range(8)]
            QKm = [A(f"QKm{s}", [128, 128], BF16) for s in range(8)]; t_QKm = [T() for _ in range(8)]
            QgT = [A(f"QgT{s}", [128, 128], BF16) for s in range(8)]; t_QgT = [T() for _ in range(8)]
            Fm = [A(f"Fm{s}", [128, 128]) for s in range(4)]; t_Fm = [T() for _ in range(4)]
            FL = [A(f"FL{s}", [128, 128]) for s in range(4)]; t_FL = [T() for _ in range(4)]
            FU = [A(f"FU{s}", [128, 128]) for s in range(4)]; t_FU = [T() for _ in range(4)]
            ad = [A(f"ad{s}", [128, 128]) for s in range(4)]; t_ad = [T() for _ in range(4)]
            EG = [A(f"EG{s}", [128, 128]) for s in range(4)]; t_EG = [T() for _ in range(4)]
            dgm = [A(f"dgm{s}", [128, 128]) for s in range(4)]; t_dgm = [T() for _ in range(4)]
            S32 = [A(f"S32{c}", [128, 128]) for c in range(2)]; t_S32 = [T(), T()]
            Sbf = [A(f"Sbf{c}", [128, 128], BF16) for c in range(2)]; t_Sbf = [T(), T()]
            vnb = [A(f"vnb{c}", [128, 128], BF16) for c in range(2)]; t_vnb = [T(), T()]
            oacc = A("oacc", [128, 16, 128]); t_oacc = [T() for _ in range(16)]
            oacc_set = [False] * 16
            for c in range(2):
                k.memset("dve", S32[c][:], 0.0, [t_S32[c]])
                k.cp("act", Sbf[c][:], S32[c][:], [t_S32[c]], [t_Sbf[c]])
            f_list = [(0, 32), (0, 33)] + [(0, i) for i in range(16)]
            b_own = [(1, i) for i in range(15, -1, -1)]
            units = [(1, 33), (1, 32)] + [(1, i) for i in range(31, 15, -1)]
            for i in range(18):
                units.append(f_list[i])
                if i < 16:
                    units.append(b_own[i])
            groups = [units[g0:g0 + 4] for g0 in range(0, len(units), 4)]

            def pre_pieces(gi):
                grp = groups[gi]
                so = 4 * (gi % 2)
                pcs = []

                def stA():
                    for s, (c, ti) in enumerate(grp):
                        j = c * 4 + h
                        col = slice(ti * 128, (ti + 1) * 128)
                        k.ts("dve", dgm[s][:], ident, g_g[:, ti, j:j + 1], None, ALU.mult, ALU.bypass,
                             [t_cst, t_gs], [t_dgm[s]])
                        k.mm(PS[s][:, 0:128], ones, dgm[s][:], True, True, [t_cst, t_dgm[s]], [PT[s]])
                        k.mm(PS[s][:, 128:256], gkT[:, col], gkT[:, col], True, True, [t_gkT], [PT[s]])
                        k.tr(PSB[s][:, 512:640], gkT[:, col], identb, [t_gkT, t_cstb], [PT[s]])
                        k.tr(PSB[s][:, 640:768], gvT[:, col], identb, [t_gvT, t_cstb], [PT[s]])

                def stB():
                    for s, (c, ti) in enumerate(grp):
                        j = c * 4 + h
                        u = so + s
                        hasq = ti < 16
                        Mst = L_st if c == 0 else U_st
                        MinT = U_in if c == 0 else L_in
                        k.ts("dve", ad[s][:], PS[s][:, 0:128], g_g[:, ti, j:j + 1], None, ALU.subtract, ALU.bypass,
                             [PT[s], t_gs], [t_ad[s]])
                        k.ts("dve", Fm[s][:], PS[s][:, 0:128], -1.0, g_g[:, ti, j:j + 1], ALU.mult, ALU.add,
                             [PT[s], t_gs], [t_Fm[s]])
                        k.tt("dve", Fm[s][:], Fm[s][:], ad[s][:], ALU.min, [t_Fm[s], t_ad[s]], [t_Fm[s]])
                        k.act(Fm[s][:], Fm[s][:], AF.Exp, [t_Fm[s]], [t_Fm[s]])
                        if hasq:
                            k.act(EG[s][:], PS[s][:, 0:128], AF.Exp, [PT[s]], [t_EG[s]])
                        k.ts("dve", XAB[u][:, 0:128], PSB[s][:, 640:768], g_beta[:, ti, j:j + 1], None, ALU.mult,
                             ALU.bypass, [PT[s], t_gs], [t_XAB[u]])
                        k.ts("dve", XAB[u][:, 128:256], PSB[s][:, 512:640], g_beg[:, ti, j:j + 1], None, ALU.mult,
                             ALU.bypass, [PT[s], t_gs], [t_XAB[u]])
                        k.ts("dve", ktl[u][:], PSB[s][:, 512:640], g_kts[:, ti, j:j + 1], None, ALU.mult, ALU.bypass,
                             [PT[s], t_gs], [t_ktl[u]])
                        k.tt("dve", FL[s][:], Fm[s][:], Mst, ALU.mult, [t_Fm[s], t_cst], [t_FL[s]])
                        if hasq:
                            k.tt("dve", FU[s][:], Fm[s][:], MinT, ALU.mult, [t_Fm[s], t_cst], [t_FU[s]])
                        k.stt(XAB[u][:, 256:384], PS[s][:, 128:256], g_nbeta[:, ti, j:j + 1], FL[s][:], ALU.mult,
                              ALU.mult, [PT[s], t_gs, t_FL[s]], [t_XAB[u]])

                def stC():
                    for s, (c, ti) in enumerate(grp):
                        u = so + s
                        hasq = ti < 16
                        col = slice(ti * 128, (ti + 1) * 128)
                        k.tr(PS[s][:, 384:512], XAB[u][:, 256:384], ident, [t_XAB[u], t_cst], [PT[s]])
                        k.cp("act", XAB[u][:, 384:512], PS[s][:, 384:512], [PT[s]], [t_XAB[u]])
                        if hasq:
                            k.mm(PS[s][:, 256:384], gkT[:, col], gqT[:, col], True, True, [t_gkT, t_gqT], [PT[s]])
                            k.tt("dve", QKm[u][:], PS[s][:, 256:384], FU[s][:], ALU.mult, [PT[s], t_FU[s]], [t_QKm[u]])
                            k.tt("pool", QgT[u][:], gqT[:, col], EG[s][:], ALU.mult, [t_gqT, t_EG[s]], [t_QgT[u]])

                def level(lv):
                    def f():
                        last = lv == 6
                        w_ = 256 if last else 384
                        for s in range(len(grp)):
                            u = so + s
                            k.mm(PS[s][:, 0:w_], XAB[u][:, 384:512], XAB[u][:, 0:w_], True, True, [t_XAB[u]], [PT[s]])
                            if not last:
                                k.mm(PS[s][:, 384:512], XAB[u][:, 256:384], XAB[u][:, 384:512], True, True,
                                     [t_XAB[u]], [PT[s]])
                        for s in range(len(grp)):
                            u = so + s
                            k.tt("dve", XAB[u][:, 0:256], XAB[u][:, 0:256], PS[s][:, 0:256], ALU.add,
                                 [t_XAB[u], PT[s]], [t_XAB[u]])
                            if not last:
                                k.cp("act", XAB[u][:, 256:512], PS[s][:, 256:512], [PT[s]], [t_XAB[u]])
                    return f

                def fin():
                    for s in range(len(grp)):
                        u = so + s
                        k.tr(PS[s][:, 0:128], XAB[u][:, 128:256], ident, [t_XAB[u], t_cst], [PT[s]])
                        k.cp("act", wkT[u][:], PS[s][:, 0:128], [PT[s]], [t_wkT[u]])

                return [stA, stB, stC] + [level(lv) for lv in range(7)] + [fin]

            def scan_pieces(gi):
                grp = groups[gi]
                so = 4 * (gi % 2)
                pcs = []
                for s, (c, ti) in enumerate(grp):
                    u = so + s
                    j = c * 4 + h
                    hasq = ti < 16
                    pb = 4 + c

                    def pa(u=u, c=c, pb=pb):
                        k.mm(PS[pb][:, 0:128], wkT[u][:], Sbf[c][:], True, True, [t_wkT[u], t_Sbf[c]], [PT[pb]])
                        k.tt("dve", vnb[c][:], XAB[u][:, 0:128], PS[pb][:, 0:128], ALU.subtract, [t_XAB[u], PT[pb]],
                             [t_vnb[c]])

                    def pbf(u=u, c=c, pb=pb, ti=ti, j=j, hasq=hasq):
                        if hasq:
                            k.mm(PS[6 + c][:, 0:128], QKm[u][:], vnb[c][:], True, False, [t_QKm[u], t_vnb[c]], [PT[6 + c]])
                            k.mm(PS[6 + c][:, 0:128], QgT[u][:], Sbf[c][:], False, True, [t_QgT[u], t_Sbf[c]], [PT[6 + c]])
                        k.mm(PS[pb][:, 128:256], ktl[u][:], vnb[c][:], True, True, [t_ktl[u], t_vnb[c]], [PT[pb]])
                        k.stt(S32[c][:], S32[c][:], g_egl[:, ti, j:j + 1], PS[pb][:, 128:256], ALU.mult, ALU.add,
                              [t_S32[c], t_gs, PT[pb]], [t_S32[c]])
                        k.cp("act", Sbf[c][:], S32[c][:], [t_S32[c]], [t_Sbf[c]])
                        if hasq:
                            if not oacc_set[ti]:
                                k.cp("act", oacc[:, ti, :], PS[6 + c][:, 0:128], [PT[6 + c]], [t_oacc[ti]])
                                oacc_set[ti] = True
                            else:
                                k.tt("dve", oacc[:, ti, :], oacc[:, ti, :], PS[6 + c][:, 0:128], ALU.add,
                                     [t_oacc[ti], PT[6 + c]], [t_oacc[ti]])
                    pcs += [pa, pbf]
                return pcs

            NG = len(groups)
            for gi in range(NG + 1):
                pre = pre_pieces(gi) if gi < NG else []
                scn = scan_pieces(gi - 1) if gi >= 1 else []
                if noq == "seq":
                    for f_ in scn:
                        f_()
                    for f_ in pre:
                        f_()
                    continue
                n = max(len(pre), len(scn))
                for i in range(n):
                    if i < len(pre):
                        pre[i]()
                    if i < len(scn):
                        scn[i]()
            dump(f"oacc{h}", oacc[:], t_oacc, [128, 16, 128])
            for n_ in range(16):
                k.act(junk[:, 0:128], oacc[:, n_, :], AF.Square, [t_oacc[n_]], [t_junk, t_ss],
                      accum_out=ss[:, 40 + n_:41 + n_])
            rstd_to(rs[:, 40:56], ss[:, 40:56], 1.0 / 128, [t_ss], t_rs)
            for n_ in range(16):
                i = n_ % 2
                cs = slice(n_ * 128, (n_ + 1) * 128)
                k.ts("dve", onb[i][:], oacc[:, n_, :], rs[:, 40 + n_:41 + n_], None, ALU.mult, ALU.bypass,
                     [t_oacc[n_], t_rs], [t_onb[i]])
                k.tr(PSB[6 + i][:, 0:128], onb[i][:], identb, [t_onb[i], t_cstb], [PT[6 + i]])
                k.stt(ybuf[:, cs], PSB[6 + i][:, 0:128], nw[:, 1, h:h + 1], sgzT[:, cs], ALU.mult, ALU.mult,
                      [PT[6 + i], t_nw, t_sgz[n_ // 4]], [t_ybuf])
            dump(f"ygdn{h}", ybuf[:], [t_ybuf], [128, NTOK], BF16)
            k.dma("sp", y_d[4 + h], ybuf[:], reads=[t_ybuf])
            k.barrier()
        if stop_after == f"H{h}":
            return finish(nc, k, out_toks, out_d)

    es_heads.close()
    h2T = P("h2T", [128, 8, NTOK], BF16); t_h2T = [T() for _ in range(16)]
    wts = P("wts", [128, 16, NE]); t_wts = [T() for _ in range(16)]
    with ExitStack() as es:
        A = lambda name, shape, dt=F32: es.enter_context(nc.sbuf_tensor("s_" + name, list(shape), dt))
        wg = A("wg", [128, 8, 2048], BF16); t_wg = T()
        wgv = w_gates_d.rearrange("(k p) n -> p k n", p=128)
        for q4 in range(4):
            k.dma("pool", wg[:, :, q4 * 512:(q4 + 1) * 512], wgv[:, :, q4 * 512:(q4 + 1) * 512], writes=[t_wg])
        wro = A("wro", [128, 4, D], BF16); t_wro = T()
        k.dma("pool", wro[:], w_ro_d.rearrange("(k p) n -> p k n", p=128), writes=[t_wro])
        wgo = A("wgo", [128, 4, D], BF16); t_wgo = T()
        k.dma("pool", wgo[:], w_go_d.rearrange("(k p) n -> p k n", p=128), writes=[t_wgo])
        wo = A("wo", [128, 8, D], BF16); t_wo = T()
        k.dma("pool", wo[:], w_o_d.rearrange("(k p) n -> p k n", p=128), writes=[t_wo])
        wr = A("wr", [128, 8, 64]); t_wr = T()
        k.dma("sp", wr[:], w_r_d.rearrange("(k p) n -> p k n", p=128), writes=[t_wr])
        rb = A("rb", [128, 64]); t_rb = T()
        k.dma("sp", rb[:], rb_d, writes=[t_rb])
        hxb31 = A("hxb3", [128, 8, 512], BF16); hxb = [hxb31, hxb31]; t_hxb31 = T(); t_hxb = [t_hxb31, t_hxb31]
        yb1 = A("yb", [128, 8, 512], BF16); yb = [yb1, yb1]; t_yb1 = T(); t_yb = [t_yb1, t_yb1]
        mT = A("mT", [128, 8, 512], BF16); t_mT = T()
        sg1 = A("sg1", [128, 512]); t_sg1 = T()
        sg2 = A("sg2", [128, 512]); t_sg2 = T()
        u1 = A("u1", [128, 512]); t_u1 = T()
        u2 = A("u2", [128, 512]); t_u2 = T()
        xt = [A(f"xt3{i}", [128, D]) for i in range(2)]; t_xt = [T(), T()]
        x1 = [A(f"x1{i}", [128, D]) for i in range(2)]; t_x1 = [T(), T()]
        xn = A("xn3", [128, D]); t_xn = T()
        h2f = A("h2f", [128, 8, 128]); t_h2f = T()
        sc = A("sc", [128, 64]); t_sc = T()
        bs = A("bs", [128, 64]); t_bs = T()
        m8 = A("m8", [128, 8, 8]); t_m8 = T()
        grp_ = A("grp", [128, 8]); t_grp = T()
        g8 = A("g8", [128, 8]); t_g8 = T()
        pen = A("pen", [128, 8]); t_pen = T()
        cand = A("cand", [128, 64]); t_cand = T()
        c8 = A("c8", [128, 8]); t_c8 = T()
        den = A("den", [128, 2]); t_den = T()
        for bi in range(4):
            tok0 = bi * 512
            hb = bi % 2
            k.dma("sp", hxb[hb][:], hx_d[:, :, tok0:tok0 + 512], writes=[t_hxb[hb]])
            k.dma("sp", yb[hb][:], y_d[:, :, tok0:tok0 + 512].rearrange("f p n -> p f n"), writes=[t_yb[hb]])
            for dc in range(8):
                for kc in range(8):
                    k.mm(PS[0][:], wg[:, kc, dc * 128:(dc + 1) * 128], hxb[hb][:, kc, :], kc == 0, kc == 7,
                         [t_wg, t_hxb[hb]], [PT[0]])
                for kc in range(8):
                    k.mm(PS[1][:], wg[:, kc, 1024 + dc * 128:1024 + (dc + 1) * 128], hxb[hb][:, kc, :], kc == 0, kc == 7,
                         [t_wg, t_hxb[hb]], [PT[1]])
                for fc in range(4):
                    k.mm(PS[2][:], wro[:, fc, dc * 128:(dc + 1) * 128], yb[hb][:, fc, :], fc == 0, fc == 3,
                         [t_wro, t_yb[hb]], [PT[2]])
                for fc in range(4):
                    k.mm(PS[3][:], wgo[:, fc, dc * 128:(dc + 1) * 128], yb[hb][:, 4 + fc, :], fc == 0, fc == 3,
                         [t_wgo, t_yb[hb]], [PT[3]])
                k.act(sg1[:], PS[0][:], AF.Sigmoid, [PT[0]], [t_sg1])
                k.act(sg2[:], PS[1][:], AF.Sigmoid, [PT[1]], [t_sg2])
                k.tt("dve", u1[:], PS[2][:], sg1[:], ALU.mult, [PT[2], t_sg1], [t_u1])
                k.tt("dve", u2[:], PS[3][:], sg2[:], ALU.mult, [PT[3], t_sg2], [t_u2])
                k.tt("dve", mT[:, dc, :], u1[:], u2[:], ALU.add, [t_u1, t_u2], [t_mT])
            for j in range(4):
                it = bi * 4 + j
                i2 = it % 2
                for nh in range(2):
                    for dc in range(8):
                        k.mm(PS[4 + nh][:], mT[:, dc, j * 128:(j + 1) * 128], wo[:, dc, nh * 512:(nh + 1) * 512],
                             dc == 0, dc == 7, [t_mT, t_wo], [PT[4 + nh]])
                k.dma("sp", xt[i2][:], xl[it * 128:(it + 1) * 128, :], writes=[t_xt[i2]])
                for nh in range(2):
                    k.act(junk[:, 0:512], PS[4 + nh][:], AF.Square, [PT[4 + nh]], [t_junk, t_den],
                          accum_out=den[:, nh:nh + 1])
                k.tt("dve", ss[:, it:it + 1], den[:, 0:1], den[:, 1:2], ALU.add, [t_den], [t_ss])
                rstd_to(rs[:, it:it + 1], ss[:, it:it + 1], 1.0 / D, [t_ss], t_rs)
                for nh in range(2):
                    hs = slice(nh * 512, (nh + 1) * 512)
                    k.stt(x1[i2][:, hs], PS[4 + nh][:], rs[:, it:it + 1], gbc[0][:, hs], ALU.mult, ALU.mult,
                          [PT[4 + nh], t_rs, t_gbc[0]], [t_x1[i2]])
                k.tt("dve", x1[i2][:], x1[i2][:], xt[i2][:], ALU.add, [t_x1[i2], t_xt[i2]], [t_x1[i2]])
                k.dma("pool", x1_d[it * 128:(it + 1) * 128, :], x1[i2][:], reads=[t_x1[i2]])
                if it == 0:
                    dump("x1_0", x1[i2][:], [t_x1[i2]], [128, D])
                norm_transpose((xn, t_xn), x1[i2][:], t_x1[i2], 16 + it, 4, 5,
                               h2T[:, :, it * 128:(it + 1) * 128], t_h2T[it], 6, f32out=(h2f, t_h2f))
                for kc in range(8):
                    k.mm(PS[0][:, 0:64], h2f[:, kc, :], wr[:, kc, :], kc == 0, kc == 7, [t_h2f, t_wr], [PT[0]])
                k.act(sc[:], PS[0][:, 0:64], AF.Exp, [PT[0]], [t_sc], scale=-1.0)
                k.ts("dve", sc[:], sc[:], 1.0, None, ALU.add, ALU.bypass, [t_sc], [t_sc])
                k.op("dve", lambda g: g.reciprocal(out=sc[:], in_=sc[:]), [t_sc], [t_sc])
                k.tt("dve", bs[:], sc[:], rb[:], ALU.add, [t_sc, t_rb], [t_bs])
                for gidx in range(8):
                    k.op("dve", lambda g: g.max(out=m8[:, gidx, :], in_=bs[:, gidx * 8:(gidx + 1) * 8]), [t_bs], [t_m8])
                k.tt("dve", grp_[:], m8[:, :, 0], m8[:, :, 1], ALU.add, [t_m8], [t_grp])
                k.op("dve", lambda g: g.max(out=g8[:], in_=grp_[:]), [t_grp], [t_g8])
                k.ts("dve", pen[:], grp_[:], g8[:, 3:4], 1.0e9, ALU.is_ge, ALU.mult, [t_grp, t_g8], [t_pen])
                k.ts("dve", pen[:], pen[:], -1.0e9, None, ALU.add, ALU.bypass, [t_pen], [t_pen])
                k.tt("dve", cand[:].rearrange("p (g m) -> p g m", m=8), bs[:].rearrange("p (g m) -> p g m", m=8),
                     pen[:].unsqueeze(2).broadcast_to([128, 8, 8]), ALU.add, [t_bs, t_pen], [t_cand])
                k.op("dve", lambda g: g.max(out=c8[:], in_=cand[:]), [t_cand], [t_c8])
                k.ts("dve", cand[:], cand[:], c8[:, 7:8], None, ALU.is_ge, ALU.bypass, [t_cand, t_c8], [t_cand])
                k.tt("dve", cand[:], cand[:], sc[:], ALU.mult, [t_cand, t_sc], [t_cand])
                k.op("dve", lambda g: g.reduce_sum(out=den[:, 0:1], in_=cand[:], axis=mybir.AxisListType.X),
                     [t_cand], [t_den])
                k.op("dve", lambda g: g.reciprocal(out=den[:, 1:2], in_=den[:, 0:1]), [t_den], [t_den])
                k.ts("dve", wts[:, it, 0:64], cand[:], den[:, 1:2], 2.5, ALU.mult, ALU.mult, [t_cand, t_den], [t_wts[it]])
                k.memset("dve", wts[:, it, 64:65], 1.0, [t_wts[it]])
        dump("wts", wts[:], t_wts, [128, 16, NE])
        dump("h2T", h2T[:], t_h2T, [128, 8, NTOK], BF16)
        k.barrier()
    if stop_after == "P3":
        return finish(nc, k, out_toks, out_d)

    with ExitStack() as es:
        A = lambda name, shape, dt=F32: es.enter_context(nc.sbuf_tensor("s_" + name, list(shape), dt))
        oacc = A("moe_acc", [128, 16, D]); t_oacc = [T() for _ in range(16)]
        wgu = [A(f"wgu{i}", [128, 8, 512], BF16) for i in range(2)]; t_wgu = [T(), T()]
        wdn = [A(f"wdn{i}", [128, 2, D], BF16) for i in range(2)]; t_wdn = [T(), T()]
        sgm = [A(f"sgm{i}", [128, 256]) for i in range(2)]; t_sgm = [T(), T()]
        ab = [A(f"ab{i}", [128, 256], BF16) for i in range(2)]; t_ab = [T(), T()]
        aT = [A(f"aT{i}", [128, 256], BF16) for i in range(2)]; t_aT = [T(), T()]
        xt = [A(f"xt5{i}", [128, D]) for i in range(2)]; t_xt = [T(), T()]
        fo = [A(f"fo{i}", [128, D]) for i in range(2)]; t_fo = [T(), T()]
        steps = [(e, it) for e in range(NE) for it in range(16)]
        NSTEP = len(steps)
        loaded = set()

        def load_w(e):
            if e >= NE or e in loaded:
                return
            loaded.add(e)
            eb = e % 2
            k.dma("pool", wgu[eb][:], w_gu_d[e].rearrange("(k p) n -> p k n", p=128), writes=[t_wgu[eb]])
            k.dma("pool", wdn[eb][:], w_dn_d[e].rearrange("(k p) n -> p k n", p=128), writes=[t_wdn[eb]])

        def gu_act(si):
            e, it = steps[si]
            eb, i2 = e % 2, si % 2
            for kc in range(8):
                k.mm(PS[i2][:], h2T[:, kc, it * 128:(it + 1) * 128], wgu[eb][:, kc, :], kc == 0, kc == 7,
                     [t_h2T[it], t_wgu[eb]], [PT[i2]])
            k.act(sgm[i2][:], PS[i2][:, 0:256], AF.Silu, [PT[i2]], [t_sgm[i2]])
            k.stt(ab[i2][:], PS[i2][:, 256:512], wts[:, it, e:e + 1], sgm[i2][:], ALU.mult, ALU.mult,
                  [PT[i2], t_wts[it], t_sgm[i2]], [t_ab[i2]])

        def tr_act(si):
            i2 = si % 2
            ptr = 2 + i2
            for fc in range(2):
                k.tr(PSB[ptr][:, fc * 128:(fc + 1) * 128], ab[i2][:, fc * 128:(fc + 1) * 128], identb,
                     [t_ab[i2], t_cstb], [PT[ptr]])
            k.cp("act", aT[i2][:], PSB[ptr][:, 0:256], [PT[ptr]], [t_aT[i2]])

        def down_acc(si):
            e, it = steps[si]
            eb, i2 = e % 2, si % 2
            pd0 = 4 + 2 * i2
            for nh in range(2):
                for fc in range(2):
                    k.mm(PS[pd0 + nh][:], aT[i2][:, fc * 128:(fc + 1) * 128], wdn[eb][:, fc, nh * 512:(nh + 1) * 512],
                         fc == 0, fc == 1, [t_aT[i2], t_wdn[eb]], [PT[pd0 + nh]])
            for nh in range(2):
                hs = slice(nh * 512, (nh + 1) * 512)
                if e == 0:
                    k.cp("act", oacc[:, it, hs], PS[pd0 + nh][:], [PT[pd0 + nh]], [t_oacc[it]])
                else:
                    k.tt("dve", oacc[:, it, hs], oacc[:, it, hs], PS[pd0 + nh][:], ALU.add,
                         [t_oacc[it], PT[pd0 + nh]], [t_oacc[it]])

        load_w(0)
        load_w(1)
        gu_act(0)
        for si in range(NSTEP + 1):
            if si + 1 < NSTEP:
                gu_act(si + 1)
            if si < NSTEP:
                tr_act(si)
            if si >= 1:
                down_acc(si - 1)
                if steps[si - 1][1] == 15:
                    load_w(steps[si - 1][0] + 2)
        dump("moe0", oacc[:, 0, :], [t_oacc[0]], [128, D])
        for it in range(16):
            i2 = it % 2
            k.dma("sp", xt[i2][:], x1_d[it * 128:(it + 1) * 128, :], writes=[t_xt[i2]])
            k.act(junk[:], oacc[:, it, :], AF.Square, [t_oacc[it]], [t_junk, t_ss], accum_out=ss[:, 32 + it % 8:33 + it % 8])
            rstd_to(rs[:, 32 + it % 8:33 + it % 8], ss[:, 32 + it % 8:33 + it % 8], 1.0 / D, [t_ss], t_rs)
            k.stt(fo[i2][:], oacc[:, it, :], rs[:, 32 + it % 8:33 + it % 8], gbc[1][:], ALU.mult, ALU.mult,
                  [t_oacc[it], t_rs, t_gbc[1]], [t_fo[i2]])
            k.tt("dve", fo[i2][:], fo[i2][:], xt[i2][:], ALU.add, [t_fo[i2], t_xt[i2]], [t_fo[i2]])
            out_toks.append(k.dma("sp", out_d[it * 128:(it + 1) * 128, :], fo[i2][:], reads=[t_fo[i2]]))
    return finish(nc, k, out_toks, out_d)


def finish(nc, k, out_toks, out_d):
    for tok in k.all_tokens():
        if tok[3] == "dma":
            semname, sem, val, owner = tok
            if k.waited["sp"].get(semname, 0) < val:
                nc.sync.wait_ge(sem, val)
                k.waited["sp"][semname] = val
    return nc


def _consts():
    p = np.arange(128, dtype=np.float32)[:, None]
    f = np.arange(128, dtype=np.float32)[None, :]
    c = np.zeros((128, 11, 128), np.float32)
    c[:, 0] = (p == f)
    c[:, 1] = 1.0
    c[:, 2] = (p > f)
    c[:, 3] = (p >= f)
    c[:, 4] = (p < f)
    c[:, 5] = (p <= f)
    c[:, 6] = np.maximum(f - p, 0)
    c[:, 7] = np.maximum(p - f, 0)
    c[:, 8] = f + 1 + 0 * p
    c[:, 9] = 128 - f + 0 * p
    c[:, 10, 0] = p[:, 0]
    c[:, 10, 1] = 127 - p[:, 0]
    c[:, 10, 2] = 128.0
    cb = np.zeros((128, 2, 128), np.float32)
    cb[:, 0] = (p == f)
    cb[:, 1] = 1.0
    return c, cb


def _rope_tables(flip):
    t = np.arange(NLAT)
    tg = (NLAT - 1 - t) if flip else t
    rows = (tg // 64).astype(np.float32)
    cols = (tg % 64).astype(np.float32)
    inv = (np.float32(10000.0) ** (-np.arange(32, dtype=np.float32) / np.float32(32))).astype(np.float32)
    ang = np.concatenate([rows[:, None] * inv, cols[:, None] * inv], axis=-1).astype(np.float32)
    cos = np.cos(ang).astype(np.float32)
    sin = np.sin(ang).astype(np.float32)
    C2 = np.concatenate([cos, cos], axis=1).T
    S2 = np.concatenate([-sin, sin], axis=1).T
    tab = np.zeros((128, 4, NS), np.float32)
    tab[:, 0, :NLAT] = C2 * QSC
    tab[:, 1, :NLAT] = S2 * QSC
    tab[:, 0, NLAT:] = QSC
    tab[:, 2, :NLAT] = C2
    tab[:, 3, :NLAT] = S2
    return tab


def fm(v):
    return np.ascontiguousarray(v.reshape(-1, 128).T)


def prep_inputs(inp, c):
    b, hf = c // 2, c % 2
    flip = hf == 1
    f32 = np.float32
    x = np.asarray(inp["x"][b], f32)
    ctx = np.asarray(inp["ctx"][b], f32)
    if flip:
        x = x[::-1]
        ctx = ctx[::-1]
    d = {}
    d["xl"] = np.ascontiguousarray(x)
    d["ctxl"] = np.ascontiguousarray(ctx)
    cv = np.stack([fm(np.asarray(inp["c"][b], f32)), fm(np.asarray(inp["c_ctx"], f32))], axis=-1)
    d["cvec"] = np.ascontiguousarray(cv)
    d["w_mod"] = np.ascontiguousarray(np.asarray(inp["w_mod"][0], f32))
    d["bmod"] = fm(np.asarray(inp["b_mod"][0], f32))
    d["gains"] = np.ascontiguousarray(np.stack([fm(np.asarray(inp[n][0], f32)) for n in
                                                ("norm_mix_pre", "norm_mix_post", "norm_ffn_pre", "norm_ffn_post")], axis=1))
    cst, cstb = _consts()
    d["cst"] = cst
    d["cstb"] = cstb
    d["rope"] = _rope_tables(flip)
    w_in = np.asarray(inp["w_in"][0], f32)
    Q0 = 2064
    sw = (np.arange(128) + 64) % 128
    heads = []
    for h in range(4):
        hs = slice(h * 128, (h + 1) * 128)
        rk = w_in[:, 0:512][:, hs]
        rv = w_in[:, 512:1024][:, hs]
        gk = w_in[:, 1024:1536][:, hs]
        gv = w_in[:, 1536:2048][:, hs]
        rq = w_in[:, Q0:Q0 + 512][:, hs]
        rg = w_in[:, Q0 + 512:Q0 + 1024][:, hs]
        gq = w_in[:, Q0 + 1024:Q0 + 1536][:, hs]
        gz = w_in[:, Q0 + 1536:Q0 + 2048][:, hs]
        heads.append(np.concatenate([rk, rk[:, sw], rv, gk, gv, rq, rq[:, sw], rg, gq, gz], axis=1))
    d["w_in_h"] = np.ascontiguousarray(np.stack(heads, 0))
    gabc = w_in[:, 2048:2064].reshape(D, 2, 2, 4)
    dirs = [1, 0] if flip else [0, 1]
    d["w_gab"] = np.ascontiguousarray(gabc[:, :, dirs, :].reshape(D, 16))
    d["w_gates"] = np.ascontiguousarray(w_in[:, Q0 + 2048:Q0 + 4096])
    conv = np.asarray(inp["gdn_conv"][0], f32)
    if flip:
        conv = conv[::-1]
    cw = np.zeros((128, 4, 3, 3), f32)
    for h in range(4):
        for fam in range(3):
            cw[:, h, fam, :] = conv[:, fam * 512 + h * 128: fam * 512 + (h + 1) * 128].T
    d["convw"] = cw
    hpv = np.concatenate([np.asarray(inp["ret_log_decay"][0], f32)[dirs].reshape(-1),
                          np.asarray(inp["gdn_a_log"][0], f32)[dirs].reshape(-1),
                          np.asarray(inp["gdn_dt_bias"][0], f32)[dirs].reshape(-1)])
    d["hp"] = np.ascontiguousarray(np.broadcast_to(hpv[None, :], (128, 24)).astype(f32))
    d["nw"] = np.ascontiguousarray(np.stack([fm(np.asarray(inp["ret_gn_w"][0], f32)),
                                             fm(np.asarray(inp["gdn_norm_w"][0], f32))], axis=1))
    d["w_ro"] = np.ascontiguousarray(np.asarray(inp["w_ret_out"][0], f32))
    d["w_go"] = np.ascontiguousarray(np.asarray(inp["w_gdn_out"][0], f32))
    d["w_o"] = np.ascontiguousarray(np.asarray(inp["w_o"][0], f32))
    d["w_r"] = np.ascontiguousarray(np.asarray(inp["w_router"][0], f32))
    d["rb"] = np.ascontiguousarray(np.broadcast_to(np.asarray(inp["router_bias"][0], f32)[None, :], (128, 64)).astype(f32))
    return d


_SHARED = {}


def shared_inputs(inp):
    f32 = np.float32
    wg = np.asarray(inp["w_gate"][0], f32)
    wu = np.asarray(inp["w_up"][0], f32)
    gu = np.empty((NE, D, 512), f32)
    gu[:64, :, :256] = wg
    gu[:64, :, 256:] = wu
    gu[64, :, :256] = np.asarray(inp["w_sh_gate"][0], f32)
    gu[64, :, 256:] = np.asarray(inp["w_sh_up"][0], f32)
    dn = np.empty((NE, 256, D), f32)
    dn[:64] = np.asarray(inp["w_down"][0], f32)
    dn[64] = np.asarray(inp["w_sh_down"][0], f32)
    return {"w_gu": gu, "w_dn": dn}


def kernel(**inputs):
    nc = build()
    sh = shared_inputs(inputs)
    in_maps = []
    for c in range(8):
        d = prep_inputs(inputs, c)
        d.update(sh)
        in_maps.append(d)
    res = run_bass_kernel_spmd(nc, in_maps, core_ids=list(range(8)))
    out = np.empty((4, NLAT, D), np.float32)
    for c in range(8):
        o = np.asarray(res.results[c]["out"], np.float32)
        b, hf = c // 2, c % 2
        if hf == 0:
            out[b, :NTOK] = o
        else:
            out[b, NTOK:] = o[::-1]
    return out
```

```python
from contextlib import ExitStack
import numpy as np
import concourse.bass as bass
import concourse.mybir as mybir
from concourse.bass_utils import run_bass_kernel_spmd

F32 = mybir.dt.float32
BF16 = mybir.dt.bfloat16
AF = mybir.ActivationFunctionType
ALU = mybir.AluOpType

D = 1024
NTOK = 2048
NLAT = 4096
NCTX = 256
NS = NLAT + NCTX
NT = NS // 128
EPS = 1e-6
NE = 65
QSC = float(128 ** -0.5)


class T:
    __slots__ = ("name", "w", "r")

    def __init__(self, name=""):
        self.name = name
        self.w = None
        self.r = {}


class KB:
    def __init__(self, nc):
        self.nc = nc
        self.eng = {"pe": nc.tensor, "act": nc.scalar, "dve": nc.vector, "pool": nc.gpsimd, "sp": nc.sync}
        self.sem = {k: nc.alloc_semaphore("sem_" + k) for k in ["pe", "act", "dve", "pool"]}
        self.cnt = {k: 0 for k in self.sem}
        self.waited = {k: {} for k in self.eng}
        self.ring = {}
        for q in ("sp", "pool"):
            self.ring[q] = dict(sems=[nc.alloc_semaphore(f"dq_{q}_{i}") for i in range(12)], cnt=[0] * 12, idx=0)

    def _wait(self, e, tok):
        semname, sem, val, owner = tok
        if owner == e and e == "pe":
            return
        w = self.waited[e]
        if w.get(semname, 0) >= val:
            return
        self.eng[e].wait_ge(sem, val)
        w[semname] = val

    def _deps(self, e, reads, writes):
        for t in reads:
            if t.w is not None:
                self._wait(e, t.w)
        for t in writes:
            if t.w is not None:
                self._wait(e, t.w)
            for tok in t.r.values():
                self._wait(e, tok)

    def _record(self, tok, reads, writes):
        for t in reads:
            t.r[tok[0]] = tok
        for t in writes:
            t.w = tok
            t.r = {}

    def op(self, e, fn, reads=(), writes=()):
        self._deps(e, reads, writes)
        ins = fn(self.eng[e])
        self.cnt[e] += 1
        ins.then_inc(self.sem[e], 1)
        tok = ("sem_" + e, self.sem[e], self.cnt[e], e)
        self._record(tok, reads, writes)
        return tok

    def dma(self, q, out, in_, reads=(), writes=(), **kw):
        rg = self.ring[q]
        i = rg["idx"] % len(rg["sems"])
        rg["idx"] += 1
        sem = rg["sems"][i]
        name = f"dq_{q}_{i}"
        if rg["cnt"][i] > 0:
            self._wait(q, (name, sem, rg["cnt"][i] * 16, "dma"))
        self._deps(q, reads, writes)
        self.eng[q].dma_start(out=out, in_=in_, **kw).then_inc(sem, 16)
        rg["cnt"][i] += 1
        tok = (name, sem, rg["cnt"][i] * 16, "dma")
        self._record(tok, reads, writes)
        return tok

    def all_tokens(self):
        toks = [("sem_" + e, self.sem[e], self.cnt[e], e) for e in self.sem if self.cnt[e] > 0]
        for q, rg in self.ring.items():
            for i, c in enumerate(rg["cnt"]):
                if c > 0:
                    toks.append((f"dq_{q}_{i}", rg["sems"][i], c * 16, "dma"))
        return toks

    def barrier(self):
        toks = self.all_tokens()
        for e in self.eng:
            for tok in toks:
                semname, sem, val, owner = tok
                w = self.waited[e]
                if w.get(semname, 0) >= val:
                    continue
                self.eng[e].wait_ge(sem, val)
                w[semname] = val

    def mm(self, out, lhsT, rhs, start, stop, reads, writes):
        return self.op("pe", lambda e: e.matmul(out, lhsT=lhsT, rhs=rhs, start=start, stop=stop), reads, writes)

    def tr(self, out, in_, ident, reads, writes):
        return self.op("pe", lambda e: e.transpose(out, in_, ident), reads, writes)

    def act(self, out, in_, func, reads, writes, **kw):
        return self.op("act", lambda e: e.activation(out=out, in_=in_, func=func, **kw), reads, writes)

    def ts(self, e, out, in0, s1, s2, op0, op1, reads, writes):
        return self.op(e, lambda g: g.tensor_scalar(out=out, in0=in0, scalar1=s1, scalar2=s2, op0=op0, op1=op1),
                       reads, writes)

    def tt(self, e, out, in0, in1, op, reads, writes):
        return self.op(e, lambda g: g.tensor_tensor(out=out, in0=in0, in1=in1, op=op), reads, writes)

    def stt(self, out, in0, scalar, in1, op0, op1, reads, writes):
        return self.op("dve", lambda g: g.scalar_tensor_tensor(out=out, in0=in0, scalar=scalar, in1=in1,
                                                                op0=op0, op1=op1), reads, writes)

    def cp(self, e, out, in_, reads, writes):
        if e == "act":
            return self.act(out, in_, AF.Copy, reads, writes)
        return self.op(e, lambda g: g.tensor_copy(out=out, in_=in_), reads, writes)

    def memset(self, e, ap, val, writes):
        return self.op(e, lambda g: g.memset(ap, val), (), writes)


def build(dbg=(), stop_after=None, noq=False):
    nc = bass.Bass("TRN2", target_bir_lowering=False)
    k = KB(nc)

    def din(name, shape, dt=F32):
        return nc.dram_tensor(name, list(shape), dt, kind="ExternalInput").ap()

    xl = din("xl", [NLAT, D])
    ctxl = din("ctxl", [NCTX, D])
    cvec = din("cvec", [128, 8, 2])
    w_mod = din("w_mod", [D, 6 * D])
    bmod_d = din("bmod", [128, 48])
    gains_d = din("gains", [128, 4, 8])
    cst_d = din("cst", [128, 11, 128])
    cstb_d = din("cstb", [128, 2, 128])
    rope_d = din("rope", [128, 4, NS])
    w_in_h = din("w_in_h", [4, D, 1280])
    w_gab_d = din("w_gab", [D, 16])
    w_gates_d = din("w_gates", [D, 2048])
    convw_d = din("convw", [128, 4, 3, 3])
    hp_d = din("hp", [128, 24])
    nw_d = din("nw", [128, 2, 4])
    w_ro_d = din("w_ro", [512, D])
    w_go_d = din("w_go", [512, D])
    w_o_d = din("w_o", [D, D])
    w_r_d = din("w_r", [D, 64])
    rb_d = din("rb", [128, 64])
    ne_decl = NE if stop_after is None else 1
    w_gu_d = din("w_gu", [ne_decl, D, 512])
    w_dn_d = din("w_dn", [ne_decl, 256, D])
    out_d = nc.dram_tensor("out", [NTOK, D], F32, kind="ExternalOutput").ap()
    hx_d = nc.dram_tensor("hx_d", [128, 8, NS], BF16, kind="Internal").ap()
    y_d = nc.dram_tensor("y_d", [8, 128, NTOK], BF16, kind="Internal").ap()
    x1_d = nc.dram_tensor("x1_d", [NTOK, D], F32, kind="Internal").ap()
    out_toks = []

    def dump(name, ap_sb, tls, shape, dt=F32):
        if name not in dbg:
            return
        d = nc.dram_tensor("dbg_" + name, list(shape), dt, kind="ExternalOutput").ap()
        out_toks.append(k.dma("sp", d, ap_sb, reads=tls))

    PS = [nc.alloc_psum_tensor(f"ps{i}", [128, 512], F32) for i in range(8)]
    PSB = [p[:].bitcast(BF16) for p in PS]
    PT = [T(f"ps{i}") for i in range(8)]

    def P(name, shape, dt=F32):
        return nc.alloc_sbuf_tensor("s_" + name, list(shape), dt)

    cst = P("cst", [128, 11, 128]); t_cst = T()
    k.dma("sp", cst[:], cst_d, writes=[t_cst])
    cstb = P("cstb", [128, 2, 128], BF16); t_cstb = T()
    k.dma("pool", cstb[:], cstb_d, writes=[t_cstb])
    ident = cst[:, 0, :]
    ones = cst[:, 1, :]
    L_st, L_in, U_st, U_in = cst[:, 2, :], cst[:, 3, :], cst[:, 4, :], cst[:, 5, :]
    identb = cstb[:, 0, :]
    onesb = cstb[:, 1, :]
    hp = P("hp", [128, 24]); t_hp = T()
    k.dma("sp", hp[:], hp_d, writes=[t_hp])
    nw = P("nw", [128, 2, 4]); t_nw = T()
    k.dma("sp", nw[:], nw_d, writes=[t_nw])
    convw = P("convw", [128, 4, 3, 3]); t_convw = T()
    k.dma("sp", convw[:], convw_d, writes=[t_convw])
    mv = P("mv", [128, 8, 8]); t_mv = T()
    gbc = [P(f"gbc{i}", [128, D]) for i in range(2)]
    t_gbc = [T() for _ in range(2)]
    ss = P("ss", [128, 64]); t_ss = T()
    rs = P("rs", [128, 64]); t_rs = T()
    junk = P("junk", [128, D], BF16); t_junk = T()
    g_la = P("g_la", [128, NT, 8]); g_beta = P("g_beta", [128, NT, 8]); g_nbeta = P("g_nbeta", [128, NT, 8])
    g_g = P("g_g", [128, NT, 8]); g_beg = P("g_beg", [128, NT, 8]); g_egl = P("g_egl", [128, NT, 8])
    g_kts = P("g_kts", [128, NT, 8])
    g_ng = P("g_ng", [128, NT, 8])
    t_gs = T()

    def sigmoid_to(out_ap, in_ap, reads, t_out):
        k.act(out_ap, in_ap, AF.Exp, reads, [t_out], scale=-1.0)
        k.act(out_ap, out_ap, AF.Ln, [t_out], [t_out], bias=1.0)
        k.act(out_ap, out_ap, AF.Exp, [t_out], [t_out], scale=-1.0)

    def rstd_to(out_ap, in_ap, scale, reads, t_out, post_scale=None):
        k.act(out_ap, in_ap, AF.Ln, reads, [t_out], scale=scale, bias=EPS)
        if post_scale is None:
            k.act(out_ap, out_ap, AF.Exp, [t_out], [t_out], scale=-0.5)
        else:
            k.act(out_ap, out_ap, AF.Exp, [t_out], [t_out], scale=-0.5, bias=float(np.log(post_scale)))

    def norm_transpose(es_xn, src_ap, tl_src, it, Acol, Bcol, dst_ap, t_dst, pbank, f32out=None, stage=0):
        xn_, t_xn_ = es_xn
        if stage != 2:
            k.act(junk[:], src_ap, AF.Square, [tl_src], [t_junk, t_ss], accum_out=ss[:, it:it + 1])
            rstd_to(rs[:, it:it + 1], ss[:, it:it + 1], 1.0 / D, [t_ss], t_rs)
            k.ts("dve", xn_[:], src_ap, rs[:, it:it + 1], None, ALU.mult, ALU.bypass, [tl_src, t_rs], [t_xn_])
            for kc in range(8):
                pb = pbank + kc // 4
                k.tr(PS[pb][:, (kc % 4) * 128:(kc % 4 + 1) * 128], xn_[:, kc * 128:(kc + 1) * 128], ident,
                     [t_xn_, t_cst], [PT[pb]])
        if stage != 1:
            for kc in range(8):
                pb = pbank + kc // 4
                o = dst_ap[:, kc, :] if f32out is None else f32out[0][:, kc, :]
                if kc % 2 == 0:
                    k.act(o, PS[pb][:, (kc % 4) * 128:(kc % 4 + 1) * 128], AF.Identity,
                          [PT[pb], t_mv], [t_dst if f32out is None else f32out[1]],
                          scale=mv[:, Acol, kc:kc + 1], bias=mv[:, Bcol, kc:kc + 1])
                else:
                    k.ts("dve", o, PS[pb][:, (kc % 4) * 128:(kc % 4 + 1) * 128], mv[:, Acol, kc:kc + 1],
                         mv[:, Bcol, kc:kc + 1], ALU.mult, ALU.add, [PT[pb], t_mv],
                         [t_dst if f32out is None else f32out[1]])
            if f32out is not None:
                k.cp("act", dst_ap, f32out[0][:], [f32out[1]], [t_dst])
        return
        k.act(junk[:], src_ap, AF.Square, [tl_src], [t_junk, t_ss], accum_out=ss[:, it:it + 1])
        rstd_to(rs[:, it:it + 1], ss[:, it:it + 1], 1.0 / D, [t_ss], t_rs)
        k.ts("dve", xn_[:], src_ap, rs[:, it:it + 1], None, ALU.mult, ALU.bypass, [tl_src, t_rs], [t_xn_])
        for kc in range(8):
            pb = pbank + kc // 4
            k.tr(PS[pb][:, (kc % 4) * 128:(kc % 4 + 1) * 128], xn_[:, kc * 128:(kc + 1) * 128], ident,
                 [t_xn_, t_cst], [PT[pb]])
        for kc in range(8):
            pb = pbank + kc // 4
            o = dst_ap[:, kc, :] if f32out is None else f32out[0][:, kc, :]
            k.act(o, PS[pb][:, (kc % 4) * 128:(kc % 4 + 1) * 128], AF.Identity,
                  [PT[pb], t_mv], [t_dst if f32out is None else f32out[1]],
                  scale=mv[:, Acol, kc:kc + 1], bias=mv[:, Bcol, kc:kc + 1])
        if f32out is not None:
            k.cp("act", dst_ap, f32out[0][:], [f32out[1]], [t_dst])

    with ExitStack() as es:
        A = lambda name, shape, dt=F32: es.enter_context(nc.sbuf_tensor("s_" + name, list(shape), dt))
        cv = A("cv", [128, 8, 2]); t_cv = T()
        k.dma("sp", cv[:], cvec, writes=[t_cv])
        bmod = A("bmod", [128, 48]); t_bmod = T()
        k.dma("sp", bmod[:], bmod_d, writes=[t_bmod])
        gains = A("gains", [128, 4, 8]); t_gains = T()
        k.dma("sp", gains[:], gains_d, writes=[t_gains])
        sg = A("sg", [128, 8, 2]); t_sg = T()
        scb = A("scb", [128, 8, 2], BF16); t_scb = T()
        sigmoid_to(sg[:], cv[:], [t_cv], t_sg)
        k.tt("dve", scb[:], sg[:], cv[:], ALU.mult, [t_sg, t_cv], [t_scb])
        wmf = [A(f"wmf{i}", [128, 8, 512]) for i in range(2)]
        t_wmf = [T() for _ in range(2)]
        wmb = [A(f"wmb{i}", [128, 8, 512], BF16) for i in range(2)]
        t_wmb = [T() for _ in range(2)]
        wm_v = w_mod.rearrange("(k p) n -> p k n", p=128)
        for j in range(12):
            b = j % 2
            k.dma("sp", wmf[b][:], wm_v[:, :, j * 512:(j + 1) * 512], writes=[t_wmf[b]])
            k.cp("dve" if j % 2 == 0 else "act", wmb[b][:], wmf[b][:], [t_wmf[b]], [t_wmb[b]])
            for m in range(4):
                jc = j * 4 + m
                for kc in range(8):
                    k.mm(PS[0][:, jc * 2:jc * 2 + 2], wmb[b][:, kc, m * 128:(m + 1) * 128], scb[:, kc, :],
                         kc == 0, kc == 7, [t_wmb[b], t_scb], [PT[0]])
        mod = A("mod", [128, 48, 2]); t_mod = T()
        k.tt("dve", mod[:], PS[0][:, 0:96].rearrange("p (j t) -> p j t", t=2),
             bmod[:].unsqueeze(2).broadcast_to([128, 48, 2]), ALU.add, [PT[0], t_bmod], [t_mod])
        k.stt(mv[:, 0, :], mod[:, 8:16, 0], 1.0, gains[:, 0, :], ALU.add, ALU.mult, [t_mod, t_gains], [t_mv])
        k.cp("dve", mv[:, 1, :], mod[:, 0:8, 0], [t_mod], [t_mv])
        k.stt(mv[:, 2, :], mod[:, 8:16, 1], 1.0, gains[:, 0, :], ALU.add, ALU.mult, [t_mod, t_gains], [t_mv])
        k.cp("dve", mv[:, 3, :], mod[:, 0:8, 1], [t_mod], [t_mv])
        k.stt(mv[:, 4, :], mod[:, 32:40, 0], 1.0, gains[:, 2, :], ALU.add, ALU.mult, [t_mod, t_gains], [t_mv])
        k.cp("dve", mv[:, 5, :], mod[:, 24:32, 0], [t_mod], [t_mv])
        k.tt("dve", mv[:, 6, :], mod[:, 16:24, 0], gains[:, 1, :], ALU.mult, [t_mod, t_gains], [t_mv])
        k.tt("dve", mv[:, 7, :], mod[:, 40:48, 0], gains[:, 3, :], ALU.mult, [t_mod, t_gains], [t_mv])
        dump("mv", mv[:], [t_mv], [128, 8, 8])
        dg = A("dg", [128, 8, 128]); t_dg = T()
        for gi in range(2):
            for kc in range(8):
                k.ts("dve", dg[:, kc, :], ident, mv[:, 6 + gi, kc:kc + 1], None, ALU.mult, ALU.bypass,
                     [t_cst, t_mv], [t_dg])
            for kc in range(8):
                pb = 1 + kc // 4
                k.mm(PS[pb][:, (kc % 4) * 128:(kc % 4 + 1) * 128], ones, dg[:, kc, :], True, True,
                     [t_cst, t_dg], [PT[pb]])
            k.cp("act", gbc[gi][:, 0:512], PS[1][:], [PT[1]], [t_gbc[gi]])
            k.cp("act", gbc[gi][:, 512:1024], PS[2][:], [PT[2]], [t_gbc[gi]])
        dump("gbc0", gbc[0][:], [t_gbc[0]], [128, D])

        xt = [A(f"xt{i}", [128, D]) for i in range(3)]; t_xt = [T() for _ in range(3)]
        xn = [A(f"xn{i}", [128, D]) for i in range(2)]; t_xn = [T() for _ in range(2)]
        hxo = [A(f"hxo{i}", [128, 8, 128], BF16) for i in range(2)]; t_hxo = [T() for _ in range(2)]
        def p1_load(it):
            if it >= NT:
                return
            src = xl[it * 128:(it + 1) * 128, :] if it < 32 else ctxl[(it - 32) * 128:(it - 31) * 128, :]
            k.dma("sp", xt[it % 3][:], src, writes=[t_xt[it % 3]])

        def p1_stage(it, stage):
            b3, b2 = it % 3, it % 2
            lat = it < 32
            if stage == 1:
                p1_load(it + 1)
            norm_transpose((xn[b2], t_xn[b2]), xt[b3][:], t_xt[b3], it, 0 if lat else 2, 1 if lat else 3,
                           hxo[b2][:], t_hxo[b2], 3 + 2 * b2, stage=stage)
            if stage == 2:
                k.dma("pool", hx_d[:, :, it * 128:(it + 1) * 128], hxo[b2][:], reads=[t_hxo[b2]])

        p1_load(0)
        p1_stage(0, 1)
        for it in range(NT):
            if it + 1 < NT:
                p1_stage(it + 1, 1)
            p1_stage(it, 2)
        k.barrier()
    if stop_after == "A":
        return finish(nc, k, out_toks, out_d)

    with ExitStack() as es:
        A = lambda name, shape, dt=F32: es.enter_context(nc.sbuf_tensor("s_" + name, list(shape), dt))
        wgab = A("wgab", [128, 8, 16], BF16); t_wgab = T()
        k.dma("pool", wgab[:], w_gab_d.rearrange("(k p) n -> p k n", p=128), writes=[t_wgab])
        hxb = [A(f"hxbg{i}", [128, 8, 512], BF16) for i in range(2)]; t_hxb = [T(), T()]
        gab = A("gab", [128, NT, 16]); t_gab = T()
        for bi in range(9):
            tok0 = bi * 512
            n = 512 if bi < 8 else 256
            hb = bi % 2
            k.dma("sp", hxb[hb][:, :, :n], hx_d[:, :, tok0:tok0 + n], writes=[t_hxb[hb]])
            pb = bi % 2
            for j in range(n // 128):
                for kc in range(8):
                    k.mm(PS[pb][:, j * 16:(j + 1) * 16], hxb[hb][:, kc, j * 128:(j + 1) * 128], wgab[:, kc, :],
                         kc == 0, kc == 7, [t_hxb[hb], t_wgab], [PT[pb]])
            k.cp("act", gab[:, bi * 4:bi * 4 + n // 128, :],
                 PS[pb][:, 0:(n // 128) * 16].rearrange("p (j c) -> p j c", c=16), [PT[pb]], [t_gab])
        dump("gab", gab[:], [t_gab], [128, NT, 16])
        negA = A("negA", [128, 8]); t_negA = T()
        k.act(negA[:], hp[:, 8:16], AF.Exp, [t_hp], [t_negA])
        k.ts("dve", negA[:], negA[:], -1.0, None, ALU.mult, ALU.bypass, [t_negA], [t_negA])
        z = A("z", [128, NT, 8]); t_z = T()
        k.tt("dve", z[:], gab[:, :, 0:8], hp[:, 16:24].unsqueeze(1).broadcast_to([128, NT, 8]), ALU.add,
             [t_gab, t_hp], [t_z])
        k.act(z[:], z[:], AF.Exp, [t_z], [t_z])
        k.act(z[:], z[:], AF.Ln, [t_z], [t_z], bias=1.0)
        k.tt("dve", g_la[:], z[:], negA[:].unsqueeze(1).broadcast_to([128, NT, 8]), ALU.mult, [t_z, t_negA], [t_gs])
        sigmoid_to(g_beta[:], gab[:, :, 8:16], [t_gab], t_gs)
        k.ts("dve", g_nbeta[:], g_beta[:], -1.0, None, ALU.mult, ALU.bypass, [t_gs], [t_gs])
        for ti in range(NT):
            pb = ti % 2
            k.mm(PS[pb][:, 0:4], U_in, g_la[:, ti, 0:4], True, True, [t_cst, t_gs], [PT[pb]])
            k.mm(PS[pb][:, 4:8], L_in, g_la[:, ti, 4:8], True, True, [t_cst, t_gs], [PT[pb]])
            k.mm(PS[pb][:, 8:16], ones, g_la[:, ti, :], True, True, [t_cst, t_gs], [PT[pb]])
            k.cp("act", g_g[:, ti, :], PS[pb][:, 0:8], [PT[pb]], [t_gs])
            k.act(g_egl[:, ti, :], PS[pb][:, 8:16], AF.Exp, [PT[pb]], [t_gs])
            k.tt("dve", g_kts[:, ti, :], PS[pb][:, 8:16], g_g[:, ti, :], ALU.subtract, [PT[pb], t_gs], [t_gs])
        k.act(g_kts[:], g_kts[:], AF.Exp, [t_gs], [t_gs])
        k.ts("dve", g_ng[:], g_g[:], -1.0, None, ALU.mult, ALU.bypass, [t_gs], [t_gs])
        k.act(g_beg[:], g_g[:], AF.Exp, [t_gs], [t_gs])
        k.tt("dve", g_beg[:], g_beg[:], g_beta[:], ALU.mult, [t_gs], [t_gs])
        dump("g_g", g_g[:], [t_gs], [128, NT, 8])
        dump("g_beta", g_beta[:], [t_gs], [128, NT, 8])
        k.barrier()
    if stop_after == "G":
        return finish(nc, k, out_toks, out_d)

    es_heads = ExitStack()
    whd2 = [es_heads.enter_context(nc.sbuf_tensor(f"s_whd{i}", [128, 8, 1280], BF16)) for i in range(2)]
    t_whd2 = [T(), T()]

    def load_whd(hh):
        wv_ = w_in_h[hh].rearrange("(k p) n -> p k n", p=128)
        for half in range(2):
            k.dma("pool", whd2[hh % 2][:, :, half * 640:(half + 1) * 640], wv_[:, :, half * 640:(half + 1) * 640],
                  writes=[t_whd2[hh % 2]])

    load_whd(0)
    for h in range(4):
        with ExitStack() as es:
            A = lambda name, shape, dt=F32: es.enter_context(nc.sbuf_tensor(f"s_{name}_{h}", list(shape), dt))
            esr = ExitStack()
            AR = lambda name, shape, dt=F32: esr.enter_context(nc.sbuf_tensor(f"s_{name}_{h}", list(shape), dt))
            whd = whd2[h % 2]; t_whd = t_whd2[h % 2]
            hxb = [A(f"hxb{i}", [128, 8, 512], BF16) for i in range(2)]; t_hxb = [T(), T()]
            rope1 = A("rope", [128, 4, 512]); rope = [rope1, rope1]; t_rope1 = T(); t_rope = [t_rope1, t_rope1]
            sgzT = A("sgzT", [128, NTOK], BF16); t_sgz = [T() for _ in range(4)]
            pck = A("pck", [128, NS + 6], BF16); t_pck = T()
            pcv = A("pcv", [128, NS + 6], BF16); t_pcv = T()
            pcq = A("pcq", [128, NTOK + 130], BF16); t_pcq = T()
            tmp1 = A("tmp1", [128, 512]); t_tmp1 = T()
            tmp2 = A("tmp2", [128, 512]); t_tmp2 = T()
            tmp3 = A("tmp3", [128, 512]); t_tmp3 = T()
            onb = [A(f"onb{i}", [128, 128], BF16) for i in range(2)]; t_onb = [T(), T()]
            ybuf = A("ybuf", [128, NTOK], BF16); t_ybuf = T()
            rkT = AR("rkT", [128, NTOK], BF16); t_rkT = [T() for _ in range(4)]
            rkblk = AR("rkblk", [128, 512], BF16); t_rkblk = T()
            rk_tm = AR("rk_tm", [128, NT, 128], BF16); t_rk_tm = [T() for _ in range(9)]
            rv_tm = AR("rv_tm", [128, NT, 128], BF16); t_rv_tm = [T() for _ in range(9)]
            rqT = AR("rqT", [128, NTOK], BF16); t_rqT = [T() for _ in range(4)]
            QF = AR("QF", [128, NTOK], BF16); QB = AR("QB", [128, NTOK], BF16); t_QFB = [T() for _ in range(4)]
            srgT = AR("srgT", [128, NTOK], BF16); t_srg = [T() for _ in range(4)]
            lgf = hp[:, h:h + 1]
            lgb = hp[:, 4 + h:5 + h]
            GF = AR("GF", [128, 128]); GB = AR("GB", [128, 128]); DmT = AR("DmT", [128, 128])
            E1 = AR("E1", [128, 128]); E2 = AR("E2", [128, 128]); wcol = AR("wcol", [128, 4])
            t_rc = T()
            k.act(GF[:], cst[:, 8, :], AF.Exp, [t_cst, t_hp], [t_rc], scale=lgf)
            k.act(GB[:], cst[:, 9, :], AF.Exp, [t_cst, t_hp], [t_rc], scale=lgb)
            k.act(wcol[:, 0:1], cst[:, 10, 1:2], AF.Exp, [t_cst, t_hp], [t_rc], scale=lgf)
            k.act(wcol[:, 1:2], cst[:, 10, 0:1], AF.Exp, [t_cst, t_hp], [t_rc], scale=lgb)
            k.act(wcol[:, 2:3], cst[:, 10, 2:3], AF.Exp, [t_cst, t_hp], [t_rc], scale=lgf)
            k.act(wcol[:, 3:4], cst[:, 10, 2:3], AF.Exp, [t_cst, t_hp], [t_rc], scale=lgb)
            k.act(E1[:], cst[:, 6, :], AF.Exp, [t_cst, t_hp], [t_rc], scale=lgf)
            k.act(E2[:], cst[:, 7, :], AF.Exp, [t_cst, t_hp], [t_rc], scale=lgb)
            k.tt("dve", E1[:], E1[:], U_st, ALU.mult, [t_rc, t_cst], [t_rc])
            k.tt("dve", E2[:], E2[:], L_st, ALU.mult, [t_rc, t_cst], [t_rc])
            k.tt("dve", E1[:], E1[:], E2[:], ALU.add, [t_rc], [t_rc])
            k.stt(DmT[:], ident, 2.0, E1[:], ALU.mult, ALU.add, [t_cst, t_rc], [t_rc])
            for (buf, tl, cols) in ((pck, t_pck, (0, 4097, 4098, 4355)), (pcv, t_pcv, (0, 4097, 4098, 4355)),
                                    (pcq, t_pcq, (0,))):
                for c in cols:
                    k.memset("pool", buf[:, c:c + 1], 0.0, [tl])

            def silu_psum_to(dst_ap, t_dst, pb, n):
                k.act(dst_ap, PS[pb][:, :n], AF.Silu, [PT[pb]], [t_dst])

            for bi in range(9):
                tok0 = bi * 512
                n = 512 if bi < 8 else 256
                own = bi < 4
                hb = bi % 2
                nt = n // 128
                k.dma("sp", hxb[hb][:, :, :n], hx_d[:, :, tok0:tok0 + n], writes=[t_hxb[hb]])
                k.dma("sp", rope[hb][:, :, :n], rope_d[:, :, tok0:tok0 + n], writes=[t_rope[hb]])

                def proj(cb, pb, ncols=n, c0=0):
                    for kc in range(8):
                        k.mm(PS[pb][:, :ncols], whd[:, kc, cb * 128:(cb + 1) * 128], hxb[hb][:, kc, c0:c0 + ncols],
                             kc == 0, kc == 7, [t_whd, t_hxb[hb]], [PT[pb]])

                def rope_to(dst_ap, t_dst, tc, ts_):
                    proj(tc[0], 0)
                    proj(tc[1], 1)
                    k.tt("dve", tmp1[:, :n], PS[0][:, :n], rope[hb][:, ts_, :n], ALU.mult, [PT[0], t_rope[hb]], [t_tmp1])
                    k.tt("dve", tmp2[:, :n], PS[1][:, :n], rope[hb][:, ts_ + 1, :n], ALU.mult, [PT[1], t_rope[hb]],
                         [t_tmp2])
                    k.tt("dve", dst_ap, tmp1[:, :n], tmp2[:, :n], ALU.add, [t_tmp1, t_tmp2], [t_dst])

                if own:
                    dst, t_dst = rkT[:, tok0:tok0 + n], t_rkT[bi]
                else:
                    dst, t_dst = rkblk[:, :n], t_rkblk
                rope_to(dst, t_dst, (0, 1), 0)
                for j in range(nt):
                    k.tr(PSB[2][:, j * 128:(j + 1) * 128], dst[:, j * 128:(j + 1) * 128], identb, [t_dst, t_cstb], [PT[2]])
                k.cp("act", rk_tm[:, bi * 4:bi * 4 + nt, :], PSB[2][:, :n].rearrange("p (j c) -> p j c", c=128),
                     [PT[2]], [t_rk_tm[bi]])
                for j in range(nt):
                    for kc in range(8):
                        k.mm(PS[3][:, j * 128:(j + 1) * 128], hxb[hb][:, kc, j * 128:(j + 1) * 128],
                             whd[:, kc, 256:384], kc == 0, kc == 7, [t_whd, t_hxb[hb]], [PT[3]])
                k.cp("act", rv_tm[:, bi * 4:bi * 4 + nt, :], PS[3][:, :n].rearrange("p (j c) -> p j c", c=128),
                     [PT[3]], [t_rv_tm[bi]])
                off = tok0 + 1 if bi < 8 else tok0 + 3
                proj(3, 4)
                k.cp("act", pck[:, off:off + n], PS[4][:, :n], [PT[4]], [t_pck])
                proj(4, 5)
                k.cp("act", pcv[:, off:off + n], PS[5][:, :n], [PT[5]], [t_pcv])
                if own:
                    rope_to(rqT[:, tok0:tok0 + n], t_rqT[bi], (5, 6), 2)
                    k.tt("pool", QF[:, tok0:tok0 + n].rearrange("p (c i) -> p c i", i=128),
                         rqT[:, tok0:tok0 + n].rearrange("p (c i) -> p c i", i=128),
                         GF[:].unsqueeze(1).broadcast_to([128, 4, 128]), ALU.mult, [t_rqT[bi], t_rc], [t_QFB[bi]])
                    k.tt("pool", QB[:, tok0:tok0 + n].rearrange("p (c i) -> p c i", i=128),
                         rqT[:, tok0:tok0 + n].rearrange("p (c i) -> p c i", i=128),
                         GB[:].unsqueeze(1).broadcast_to([128, 4, 128]), ALU.mult, [t_rqT[bi], t_rc], [t_QFB[bi]])
                    proj(7, 6)
                    silu_psum_to(srgT[:, tok0:tok0 + n], t_srg[bi], 6, n)
                    proj(9, 6)
                    silu_psum_to(sgzT[:, tok0:tok0 + n], t_sgz[bi], 6, n)
                    proj(8, 7)
                    k.cp("act", pcq[:, 1 + tok0:1 + tok0 + n], PS[7][:, :n], [PT[7]], [t_pcq])
                if bi == 4:
                    proj(8, 7, ncols=128)
                    k.cp("act", pcq[:, 2049:2049 + 128], PS[7][:, :128], [PT[7]], [t_pcq])
            if h + 1 < 4:
                load_whd(h + 1)
            if stop_after == f"H{h}a":
                k.barrier(); esr.close()
                return finish(nc, k, out_toks, out_d)
            dump(f"rkT{h}", rkT[:], t_rkT, [128, NTOK], BF16)
            dump(f"rv_tm{h}", rv_tm[:], t_rv_tm, [128, NT, 128], BF16)
            dump(f"rk_tm{h}", rk_tm[:], t_rk_tm, [128, NT, 128], BF16)
            dump(f"QF{h}", QF[:], t_QFB, [128, NTOK], BF16)
            dump(f"srgT{h}", srgT[:], t_srg, [128, NTOK], BF16)

            Sb32 = AR("Sb32", [128, 128]); t_Sb32 = T()
            Sf32 = AR("Sf32", [128, 128]); t_Sf32 = T()
            Sbst = AR("Sbst", [128, 16, 128], BF16); t_Sbst = [T() for _ in range(16)]
            Sfb = [AR(f"Sfb{i}", [128, 128], BF16) for i in range(2)]; t_Sfb = [T(), T()]
            wvb = [AR(f"wvb{i}", [128, 128], BF16) for i in range(2)]; t_wvb = [T(), T()]
            scm = [AR(f"scm{i}", [128, 128], BF16) for i in range(2)]; t_scm = [T(), T()]
            osb = AR("osb", [128, 16, 128]); t_osb = [T() for _ in range(16)]
            bst = AR("bst", [128, 16, 6]); t_bst = T()
            mvar = AR("mvar", [128, 16, 2]); t_mvar = T()
            rstd16 = AR("rstd16", [128, 16]); t_rstd16 = T()
            k.memset("dve", Sb32[:], 0.0, [t_Sb32])
            k.memset("dve", Sf32[:], 0.0, [t_Sf32])
            cnt = [0]

            def ret_update(S32, t_S32, ti, wc, dc):
                i = cnt[0] % 2
                cnt[0] += 1
                bi_ = ti // 4
                k.ts("dve", wvb[i][:], rv_tm[:, ti, :], wcol[:, wc:wc + 1], None, ALU.mult, ALU.bypass,
                     [t_rv_tm[bi_], t_rc], [t_wvb[i]])
                k.mm(PS[i][:, 0:128], rk_tm[:, ti, :], wvb[i][:], True, True, [t_rk_tm[bi_], t_wvb[i]], [PT[i]])
                k.stt(S32[:], S32[:], wcol[:, dc:dc + 1], PS[i][:, 0:128], ALU.mult, ALU.add,
                      [t_S32, t_rc, PT[i]], [t_S32])

            for ti in [33, 32] + list(range(31, 15, -1)) + list(range(15, -1, -1)):
                if ti < 16:
                    k.cp("act", Sbst[:, ti, :], Sb32[:], [t_Sb32], [t_Sbst[ti]])
                ret_update(Sb32, t_Sb32, ti, 1, 3)
            if stop_after == f"H{h}r2":
                k.barrier(); esr.close()
                return finish(nc, k, out_toks, out_d)
            for ti in (32, 33):
                ret_update(Sf32, t_Sf32, ti, 0, 2)
            for n_ in range(16):
                i = n_ % 2
                bi_ = n_ // 4
                cs = slice(n_ * 128, (n_ + 1) * 128)
                k.cp("act", Sfb[i][:], Sf32[:], [t_Sf32], [t_Sfb[i]])
                k.mm(PS[2 + i][:, 0:128], rkT[:, cs], rqT[:, cs], True, True, [t_rkT[bi_], t_rqT[bi_]], [PT[2 + i]])
                k.tt("dve", scm[i][:], PS[2 + i][:, 0:128], DmT[:], ALU.mult, [PT[2 + i], t_rc], [t_scm[i]])
                k.mm(PS[4 + i][:, 0:128], scm[i][:], rv_tm[:, n_, :], True, False, [t_scm[i], t_rv_tm[bi_]], [PT[4 + i]])
                k.mm(PS[4 + i][:, 0:128], QF[:, cs], Sfb[i][:], False, False, [t_QFB[bi_], t_Sfb[i]], [PT[4 + i]])
                k.mm(PS[4 + i][:, 0:128], QB[:, cs], Sbst[:, n_, :], False, True, [t_QFB[bi_], t_Sbst[n_]], [PT[4 + i]])
                k.act(osb[:, n_, :], PS[4 + i][:, 0:128], AF.Identity, [PT[4 + i]], [t_osb[n_], t_bst],
                      accum_out=bst[:, n_, 0:1])
                k.act(junk[:, 0:128], PS[4 + i][:, 0:128], AF.Square, [PT[4 + i]], [t_junk, t_bst],
                      accum_out=bst[:, n_, 1:2])
                ret_update(Sf32, t_Sf32, n_, 0, 2)
            if stop_after == f"H{h}r3":
                k.barrier(); esr.close()
                return finish(nc, k, out_toks, out_d)
            k.ts("dve", mvar[:, :, 0], bst[:, :, 0], 1.0 / 128, None, ALU.mult, ALU.bypass, [t_bst], [t_mvar])
            k.tt("dve", mvar[:, :, 1], mvar[:, :, 0], mvar[:, :, 0], ALU.mult, [t_mvar], [t_mvar])
            k.stt(mvar[:, :, 1], bst[:, :, 1], 1.0 / 128, mvar[:, :, 1], ALU.mult, ALU.subtract, [t_bst, t_mvar], [t_mvar])
            rstd_to(rstd16[:], mvar[:, :, 1], 1.0, [t_mvar], t_rstd16)
            dump(f"osb{h}", osb[:], t_osb, [128, 16, 128])
            for n_ in range(16):
                i = n_ % 2
                cs = slice(n_ * 128, (n_ + 1) * 128)
                k.ts("dve", onb[i][:], osb[:, n_, :], mvar[:, n_, 0:1], rstd16[:, n_:n_ + 1], ALU.subtract, ALU.mult,
                     [t_osb[n_], t_mvar, t_rstd16], [t_onb[i]])
                k.tr(PSB[6 + i][:, 0:128], onb[i][:], identb, [t_onb[i], t_cstb], [PT[6 + i]])
                k.stt(ybuf[:, cs], PSB[6 + i][:, 0:128], nw[:, 0, h:h + 1], srgT[:, cs], ALU.mult, ALU.mult,
                      [PT[6 + i], t_nw, t_srg[n_ // 4]], [t_ybuf])
            dump(f"yret{h}", ybuf[:], [t_ybuf], [128, NTOK], BF16)
            k.dma("sp", y_d[h], ybuf[:], reads=[t_ybuf])
            k.barrier()
            esr.close()
            if stop_after == f"H{h}b":
                return finish(nc, k, out_toks, out_d)

            gkT = A("gkT", [128, NS], BF16); t_gkT = T()
            gvT = A("gvT", [128, NS], BF16); t_gvT = T()
            gqT = A("gqT", [128, NTOK], BF16); t_gqT = T()
            sqb_ = [A(f"sqb{i}", [128, 512], BF16) for i in range(2)]; t_sqb_ = [T(), T()]
            ct1 = [tmp1, A("tmp1b", [128, 512])]; t_ct1 = [t_tmp1, T()]
            ct2 = [tmp2, A("tmp2b", [128, 512])]; t_ct2 = [t_tmp2, T()]
            ct3 = [tmp3, A("tmp3b", [128, 512])]; t_ct3 = [t_tmp3, T()]
            cpc = [0]

            def conv_piece(pc, t_pc, o, n, fam, dstT, t_dstT, d0, l2, qscale):
                w = convw[:, h, fam, :]
                pp = cpc[0] % 2
                cpc[0] += 1
                tmp1, t_tmp1, tmp2, t_tmp2, tmp3, t_tmp3 = ct1[pp], t_ct1[pp], ct2[pp], t_ct2[pp], ct3[pp], t_ct3[pp]
                sqb, t_sqb = sqb_[pp], t_sqb_[pp]
                pbank = 6 + pp
                k.ts("dve", tmp1[:, :n], pc[:, o - 1:o - 1 + n], w[:, 0:1], None, ALU.mult, ALU.bypass,
                     [t_pc, t_convw], [t_tmp1])
                k.stt(tmp1[:, :n], pc[:, o:o + n], w[:, 1:2], tmp1[:, :n], ALU.mult, ALU.add, [t_pc, t_convw, t_tmp1],
                      [t_tmp1])
                k.stt(tmp1[:, :n], pc[:, o + 1:o + 1 + n], w[:, 2:3], tmp1[:, :n], ALU.mult, ALU.add,
                      [t_pc, t_convw, t_tmp1], [t_tmp1])
                sigmoid_to(tmp3[:, :n], tmp1[:, :n], [t_tmp1], t_tmp3)
                if not l2:
                    k.tt("dve", dstT[:, d0:d0 + n], tmp1[:, :n], tmp3[:, :n], ALU.mult, [t_tmp1, t_tmp3], [t_dstT])
                    return
                k.tt("dve", tmp2[:, :n], tmp1[:, :n], tmp3[:, :n], ALU.mult, [t_tmp1, t_tmp3], [t_tmp2])
                k.act(sqb[:, :n], tmp2[:, :n], AF.Square, [t_tmp2], [t_sqb])
                k.mm(PS[pbank][:, :n], onesb, sqb[:, :n], True, True, [t_cstb, t_sqb], [PT[pbank]])
                rstd_to(tmp3[:, :n], PS[pbank][:, :n], 1.0, [PT[pbank]], t_tmp3, post_scale=qscale)
                k.tt("dve", dstT[:, d0:d0 + n], tmp2[:, :n], tmp3[:, :n], ALU.mult, [t_tmp2, t_tmp3], [t_dstT])

            for bi in range(9):
                tok0 = bi * 512
                n = 512 if bi < 8 else 256
                off = tok0 + 1 if bi < 8 else tok0 + 3
                conv_piece(pck, t_pck, off, n, 1, gkT, t_gkT, tok0, True, None)
                conv_piece(pcv, t_pcv, off, n, 2, gvT, t_gvT, tok0, False, None)
                if bi < 4:
                    conv_piece(pcq, t_pcq, tok0 + 1, n, 0, gqT, t_gqT, tok0, True, QSC)
            dump(f"gkT{h}", gkT[:], [t_gkT], [128, NS], BF16)
            dump(f"gvT{h}", gvT[:], [t_gvT], [128, NS], BF16)
            dump(f"gqT{h}", gqT[:], [t_gqT], [128, NTOK], BF16)

            if stop_after == f"H{h}g1":
                k.barrier()
                return finish(nc, k, out_toks, out_d)
            XAB = [A(f"XAB{s}", [128, 512]) for s in range(8)]; t_XAB = [T() for _ in range(8)]
            wkT = [A(f"wkT{s}", [128, 128], BF16) for s in range(8)]; t_wkT = [T() for _ in range(8)]
            ktl = [A(f"ktl{s}", [128, 128], BF16) for s in range(8)]; t_ktl = [T() for _ in range(8)]
            QKm = [A(f"QKm{s}", [128, 128], BF16) for s in range(8)]; t_QKm = [T() for _ in range(8)]
            QgT = [A(f"QgT{s}", [128, 128], BF16) for s in range(8)]; t_QgT = [T() for _ in range(8)]
            Fm = [A(f"Fm{s}", [128, 128]) for s in range(4)]; t_Fm = [T() for _ in range(4)]
            FL = [A(f"FL{s}", [128, 128]) for s in range(4)]; t_FL = [T() for _ in range(4)]
            FU = [A(f"FU{s}", [128, 128]) for s in range(4)]; t_FU = [T() for _ in range(4)]
            ad = [A(f"ad{s}", [128, 128]) for s in range(4)]; t_ad = [T() for _ in range(4)]
            EG = [A(f"EG{s}", [128, 128]) for s in range(4)]; t_EG = [T() for _ in range(4)]
            dgm = [A(f"dgm{s}", [128, 128]) for s in range(4)]; t_dgm = [T() for _ in range(4)]
            S32 = [A(f"S32{c}", [128, 128]) for c in range(2)]; t_S32 = [T(), T()]
            Sbf = [A(f"Sbf{c}", [128, 128], BF16) for c in range(2)]; t_Sbf = [T(), T()]
            vnb = [A(f"vnb{c}", [128, 128], BF16) for c in range(2)]; t_vnb = [T(), T()]
            oacc = A("oacc", [128, 16, 128]); t_oacc = [T() for _ in range(16)]
            oacc_set = [False] * 16
            for c in range(2):
                k.memset("dve", S32[c][:], 0.0, [t_S32[c]])
                k.cp("act", Sbf[c][:], S32[c][:], [t_S32[c]], [t_Sbf[c]])
            f_list = [(0, 32), (0, 33)] + [(0, i) for i in range(16)]
            b_own = [(1, i) for i in range(15, -1, -1)]
            units = [(1, 33), (1, 32)] + [(1, i) for i in range(31, 15, -1)]
            for i in range(18):
                units.append(f_list[i])
                if i < 16:
                    units.append(b_own[i])
            groups = [units[g0:g0 + 4] for g0 in range(0, len(units), 4)]

            def pre_pieces(gi):
                grp = groups[gi]
                so = 4 * (gi % 2)
                pcs = []

                def stA():
                    for s, (c, ti) in enumerate(grp):
                        j = c * 4 + h
                        col = slice(ti * 128, (ti + 1) * 128)
                        k.ts("dve", dgm[s][:], ident, g_g[:, ti, j:j + 1], None, ALU.mult, ALU.bypass,
                             [t_cst, t_gs], [t_dgm[s]])
                        k.mm(PS[s][:, 0:128], ones, dgm[s][:], True, True, [t_cst, t_dgm[s]], [PT[s]])
                        k.mm(PS[s][:, 128:256], gkT[:, col], gkT[:, col], True, True, [t_gkT], [PT[s]])
                        k.tr(PSB[s][:, 512:640], gkT[:, col], identb, [t_gkT, t_cstb], [PT[s]])
                        k.tr(PSB[s][:, 640:768], gvT[:, col], identb, [t_gvT, t_cstb], [PT[s]])

                def stB():
                    for s, (c, ti) in enumerate(grp):
                        j = c * 4 + h
                        u = so + s
                        hasq = ti < 16
                        Mst = L_st if c == 0 else U_st
                        MinT = U_in if c == 0 else L_in
                        k.ts("dve", ad[s][:], PS[s][:, 0:128], g_g[:, ti, j:j + 1], None, ALU.subtract, ALU.bypass,
                             [PT[s], t_gs], [t_ad[s]])
                        k.ts("dve", Fm[s][:], PS[s][:, 0:128], -1.0, g_g[:, ti, j:j + 1], ALU.mult, ALU.add,
                             [PT[s], t_gs], [t_Fm[s]])
                        k.tt("dve", Fm[s][:], Fm[s][:], ad[s][:], ALU.min, [t_Fm[s], t_ad[s]], [t_Fm[s]])
                        k.act(Fm[s][:], Fm[s][:], AF.Exp, [t_Fm[s]], [t_Fm[s]])
                        if hasq:
                            k.act(EG[s][:], PS[s][:, 0:128], AF.Exp, [PT[s]], [t_EG[s]])
                        k.ts("dve", XAB[u][:, 0:128], PSB[s][:, 640:768], g_beta[:, ti, j:j + 1], None, ALU.mult,
                             ALU.bypass, [PT[s], t_gs], [t_XAB[u]])
                        k.ts("dve", XAB[u][:, 128:256], PSB[s][:, 512:640], g_beg[:, ti, j:j + 1], None, ALU.mult,
                             ALU.bypass, [PT[s], t_gs], [t_XAB[u]])
                        k.ts("dve", ktl[u][:], PSB[s][:, 512:640], g_kts[:, ti, j:j + 1], None, ALU.mult, ALU.bypass,
                             [PT[s], t_gs], [t_ktl[u]])
                        k.tt("dve", FL[s][:], Fm[s][:], Mst, ALU.mult, [t_Fm[s], t_cst], [t_FL[s]])
                        if hasq:
                            k.tt("dve", FU[s][:], Fm[s][:], MinT, ALU.mult, [t_Fm[s], t_cst], [t_FU[s]])
                        k.stt(XAB[u][:, 256:384], PS[s][:, 128:256], g_nbeta[:, ti, j:j + 1], FL[s][:], ALU.mult,
                              ALU.mult, [PT[s], t_gs, t_FL[s]], [t_XAB[u]])

                def stC():
                    for s, (c, ti) in enumerate(grp):
                        u = so + s
                        hasq = ti < 16
                        col = slice(ti * 128, (ti + 1) * 128)
                        k.tr(PS[s][:, 384:512], XAB[u][:, 256:384], ident, [t_XAB[u], t_cst], [PT[s]])
                        k.cp("act", XAB[u][:, 384:512], PS[s][:, 384:512], [PT[s]], [t_XAB[u]])
                        if hasq:
                            k.mm(PS[s][:, 256:384], gkT[:, col], gqT[:, col], True, True, [t_gkT, t_gqT], [PT[s]])
                            k.tt("dve", QKm[u][:], PS[s][:, 256:384], FU[s][:], ALU.mult, [PT[s], t_FU[s]], [t_QKm[u]])
                            k.tt("pool", QgT[u][:], gqT[:, col], EG[s][:], ALU.mult, [t_gqT, t_EG[s]], [t_QgT[u]])

                def level(lv):
                    def f():
                        last = lv == 6
                        w_ = 256 if last else 384
                        for s in range(len(grp)):
                            u = so + s
                            k.mm(PS[s][:, 0:w_], XAB[u][:, 384:512], XAB[u][:, 0:w_], True, True, [t_XAB[u]], [PT[s]])
                            if not last:
                                k.mm(PS[s][:, 384:512], XAB[u][:, 256:384], XAB[u][:, 384:512], True, True,
                                     [t_XAB[u]], [PT[s]])
                        for s in range(len(grp)):
                            u = so + s
                            k.tt("dve", XAB[u][:, 0:256], XAB[u][:, 0:256], PS[s][:, 0:256], ALU.add,
                                 [t_XAB[u], PT[s]], [t_XAB[u]])
                            if not last:
                                k.cp("act", XAB[u][:, 256:512], PS[s][:, 256:512], [PT[s]], [t_XAB[u]])
                    return f

                def fin():
                    for s in range(len(grp)):
                        u = so + s
                        k.tr(PS[s][:, 0:128], XAB[u][:, 128:256], ident, [t_XAB[u], t_cst], [PT[s]])
                        k.cp("act", wkT[u][:], PS[s][:, 0:128], [PT[s]], [t_wkT[u]])

                return [stA, stB, stC] + [level(lv) for lv in range(7)] + [fin]

            def scan_pieces(gi):
                grp = groups[gi]
                so = 4 * (gi % 2)
                pcs = []
                for s, (c, ti) in enumerate(grp):
                    u = so + s
                    j = c * 4 + h
                    hasq = ti < 16
                    pb = 4 + c

                    def pa(u=u, c=c, pb=pb):
                        k.mm(PS[pb][:, 0:128], wkT[u][:], Sbf[c][:], True, True, [t_wkT[u], t_Sbf[c]], [PT[pb]])
                        k.tt("dve", vnb[c][:], XAB[u][:, 0:128], PS[pb][:, 0:128], ALU.subtract, [t_XAB[u], PT[pb]],
                             [t_vnb[c]])

                    def pbf(u=u, c=c, pb=pb, ti=ti, j=j, hasq=hasq):
                        if hasq:
                            k.mm(PS[6 + c][:, 0:128], QKm[u][:], vnb[c][:], True, False, [t_QKm[u], t_vnb[c]], [PT[6 + c]])
                            k.mm(PS[6 + c][:, 0:128], QgT[u][:], Sbf[c][:], False, True, [t_QgT[u], t_Sbf[c]], [PT[6 + c]])
                        k.mm(PS[pb][:, 128:256], ktl[u][:], vnb[c][:], True, True, [t_ktl[u], t_vnb[c]], [PT[pb]])
                        k.stt(S32[c][:], S32[c][:], g_egl[:, ti, j:j + 1], PS[pb][:, 128:256], ALU.mult, ALU.add,
                              [t_S32[c], t_gs, PT[pb]], [t_S32[c]])
                        k.cp("act", Sbf[c][:], S32[c][:], [t_S32[c]], [t_Sbf[c]])
                        if hasq:
                            if not oacc_set[ti]:
                                k.cp("act", oacc[:, ti, :], PS[6 + c][:, 0:128], [PT[6 + c]], [t_oacc[ti]])
                                oacc_set[ti] = True
                            else:
                                k.tt("dve", oacc[:, ti, :], oacc[:, ti, :], PS[6 + c][:, 0:128], ALU.add,
                                     [t_oacc[ti], PT[6 + c]], [t_oacc[ti]])
                    pcs += [pa, pbf]
                return pcs

            NG = len(groups)
            for gi in range(NG + 1):
                pre = pre_pieces(gi) if gi < NG else []
                scn = scan_pieces(gi - 1) if gi >= 1 else []
                if noq == "seq":
                    for f_ in scn:
                        f_()
                    for f_ in pre:
                        f_()
                    continue
                n = max(len(pre), len(scn))
                for i in range(n):
                    if i < len(pre):
                        pre[i]()
                    if i < len(scn):
                        scn[i]()
            dump(f"oacc{h}", oacc[:], t_oacc, [128, 16, 128])
            for n_ in range(16):
                k.act(junk[:, 0:128], oacc[:, n_, :], AF.Square, [t_oacc[n_]], [t_junk, t_ss],
                      accum_out=ss[:, 40 + n_:41 + n_])
            rstd_to(rs[:, 40:56], ss[:, 40:56], 1.0 / 128, [t_ss], t_rs)
            for n_ in range(16):
                i = n_ % 2
                cs = slice(n_ * 128, (n_ + 1) * 128)
                k.ts("dve", onb[i][:], oacc[:, n_, :], rs[:, 40 + n_:41 + n_], None, ALU.mult, ALU.bypass,
                     [t_oacc[n_], t_rs], [t_onb[i]])
                k.tr(PSB[6 + i][:, 0:128], onb[i][:], identb, [t_onb[i], t_cstb], [PT[6 + i]])
                k.stt(ybuf[:, cs], PSB[6 + i][:, 0:128], nw[:, 1, h:h + 1], sgzT[:, cs], ALU.mult, ALU.mult,
                      [PT[6 + i], t_nw, t_sgz[n_ // 4]], [t_ybuf])
            dump(f"ygdn{h}", ybuf[:], [t_ybuf], [128, NTOK], BF16)
            k.dma("sp", y_d[4 + h], ybuf[:], reads=[t_ybuf])
            k.barrier()
        if stop_after == f"H{h}":
            return finish(nc, k, out_toks, out_d)

    es_heads.close()
    h2T = P("h2T", [128, 8, NTOK], BF16); t_h2T = [T() for _ in range(16)]
    wts = P("wts", [128, 16, NE]); t_wts = [T() for _ in range(16)]
    with ExitStack() as es:
        A = lambda name, shape, dt=F32: es.enter_context(nc.sbuf_tensor("s_" + name, list(shape), dt))
        wg = A("wg", [128, 8, 2048], BF16); t_wg = T()
        wgv = w_gates_d.rearrange("(k p) n -> p k n", p=128)
        for q4 in range(4):
            k.dma("pool", wg[:, :, q4 * 512:(q4 + 1) * 512], wgv[:, :, q4 * 512:(q4 + 1) * 512], writes=[t_wg])
        wro = A("wro", [128, 4, D], BF16); t_wro = T()
        k.dma("pool", wro[:], w_ro_d.rearrange("(k p) n -> p k n", p=128), writes=[t_wro])
        wgo = A("wgo", [128, 4, D], BF16); t_wgo = T()
        k.dma("pool", wgo[:], w_go_d.rearrange("(k p) n -> p k n", p=128), writes=[t_wgo])
        wo = A("wo", [128, 8, D], BF16); t_wo = T()
        k.dma("pool", wo[:], w_o_d.rearrange("(k p) n -> p k n", p=128), writes=[t_wo])
        wr = A("wr", [128, 8, 64]); t_wr = T()
        k.dma("sp", wr[:], w_r_d.rearrange("(k p) n -> p k n", p=128), writes=[t_wr])
        rb = A("rb", [128, 64]); t_rb = T()
        k.dma("sp", rb[:], rb_d, writes=[t_rb])
        hxb31 = A("hxb3", [128, 8, 512], BF16); hxb = [hxb31, hxb31]; t_hxb31 = T(); t_hxb = [t_hxb31, t_hxb31]
        yb1 = A("yb", [128, 8, 512], BF16); yb = [yb1, yb1]; t_yb1 = T(); t_yb = [t_yb1, t_yb1]
        mT = A("mT", [128, 8, 512], BF16); t_mT = T()
        sg1 = A("sg1", [128, 512]); t_sg1 = T()
        sg2 = A("sg2", [128, 512]); t_sg2 = T()
        u1 = A("u1", [128, 512]); t_u1 = T()
        u2 = A("u2", [128, 512]); t_u2 = T()
        xt = [A(f"xt3{i}", [128, D]) for i in range(2)]; t_xt = [T(), T()]
        x1 = [A(f"x1{i}", [128, D]) for i in range(2)]; t_x1 = [T(), T()]
        xn2 = [A(f"xn3{i}", [128, D]) for i in range(2)]; t_xn2 = [T(), T()]
        den2 = A("den2", [128, 2]); t_den2 = T()
        h2f = A("h2f", [128, 8, 128]); t_h2f = T()
        sc = A("sc", [128, 64]); t_sc = T()
        bs = A("bs", [128, 64]); t_bs = T()
        m8 = A("m8", [128, 8, 8]); t_m8 = T()
        grp_ = A("grp", [128, 8]); t_grp = T()
        g8 = A("g8", [128, 8]); t_g8 = T()
        pen = A("pen", [128, 8]); t_pen = T()
        cand = A("cand", [128, 64]); t_cand = T()
        c8 = A("c8", [128, 8]); t_c8 = T()
        den = A("den", [128, 2]); t_den = T()
        for bi in range(4):
            tok0 = bi * 512
            hb = bi % 2
            k.dma("sp", hxb[hb][:], hx_d[:, :, tok0:tok0 + 512], writes=[t_hxb[hb]])
            k.dma("sp", yb[hb][:], y_d[:, :, tok0:tok0 + 512].rearrange("f p n -> p f n"), writes=[t_yb[hb]])
            for dc in range(8):
                for kc in range(8):
                    k.mm(PS[0][:], wg[:, kc, dc * 128:(dc + 1) * 128], hxb[hb][:, kc, :], kc == 0, kc == 7,
                         [t_wg, t_hxb[hb]], [PT[0]])
                for kc in range(8):
                    k.mm(PS[1][:], wg[:, kc, 1024 + dc * 128:1024 + (dc + 1) * 128], hxb[hb][:, kc, :], kc == 0, kc == 7,
                         [t_wg, t_hxb[hb]], [PT[1]])
                for fc in range(4):
                    k.mm(PS[2][:], wro[:, fc, dc * 128:(dc + 1) * 128], yb[hb][:, fc, :], fc == 0, fc == 3,
                         [t_wro, t_yb[hb]], [PT[2]])
                for fc in range(4):
                    k.mm(PS[3][:], wgo[:, fc, dc * 128:(dc + 1) * 128], yb[hb][:, 4 + fc, :], fc == 0, fc == 3,
                         [t_wgo, t_yb[hb]], [PT[3]])
                k.act(sg1[:], PS[0][:], AF.Sigmoid, [PT[0]], [t_sg1])
                k.act(sg2[:], PS[1][:], AF.Sigmoid, [PT[1]], [t_sg2])
                k.tt("dve", u1[:], PS[2][:], sg1[:], ALU.mult, [PT[2], t_sg1], [t_u1])
                k.tt("dve", u2[:], PS[3][:], sg2[:], ALU.mult, [PT[3], t_sg2], [t_u2])
                k.tt("dve", mT[:, dc, :], u1[:], u2[:], ALU.add, [t_u1, t_u2], [t_mT])
            def p3_s1(j):
                it = bi * 4 + j
                i2 = it % 2
                pbk = 6 if j % 2 == 0 else 1
                for nh in range(2):
                    for dc in range(8):
                        k.mm(PS[4 + nh][:], mT[:, dc, j * 128:(j + 1) * 128], wo[:, dc, nh * 512:(nh + 1) * 512],
                             dc == 0, dc == 7, [t_mT, t_wo], [PT[4 + nh]])
                k.dma("sp", xt[i2][:], xl[it * 128:(it + 1) * 128, :], writes=[t_xt[i2]])
                for nh in range(2):
                    k.act(junk[:, 0:512], PS[4 + nh][:], AF.Square, [PT[4 + nh]], [t_junk, t_den],
                          accum_out=den[:, nh:nh + 1])
                k.tt("dve", ss[:, it:it + 1], den[:, 0:1], den[:, 1:2], ALU.add, [t_den], [t_ss])
                rstd_to(rs[:, it:it + 1], ss[:, it:it + 1], 1.0 / D, [t_ss], t_rs)
                for nh in range(2):
                    hs = slice(nh * 512, (nh + 1) * 512)
                    k.stt(x1[i2][:, hs], PS[4 + nh][:], rs[:, it:it + 1], gbc[0][:, hs], ALU.mult, ALU.mult,
                          [PT[4 + nh], t_rs, t_gbc[0]], [t_x1[i2]])
                k.tt("dve", x1[i2][:], x1[i2][:], xt[i2][:], ALU.add, [t_x1[i2], t_xt[i2]], [t_x1[i2]])
                k.dma("pool", x1_d[it * 128:(it + 1) * 128, :], x1[i2][:], reads=[t_x1[i2]])
                if it == 0:
                    dump("x1_0", x1[i2][:], [t_x1[i2]], [128, D])
                norm_transpose((xn2[i2], t_xn2[i2]), x1[i2][:], t_x1[i2], 16 + it, 4, 5,
                               h2T[:, :, it * 128:(it + 1) * 128], t_h2T[it], pbk, f32out=(h2f, t_h2f), stage=1)

            def p3_s2(j):
                it = bi * 4 + j
                i2 = it % 2
                pbk = 6 if j % 2 == 0 else 1
                norm_transpose((xn2[i2], t_xn2[i2]), x1[i2][:], t_x1[i2], 16 + it, 4, 5,
                               h2T[:, :, it * 128:(it + 1) * 128], t_h2T[it], pbk, f32out=(h2f, t_h2f), stage=2)
                for kc in range(8):
                    k.mm(PS[0][:, 0:64], h2f[:, kc, :], wr[:, kc, :], kc == 0, kc == 7, [t_h2f, t_wr], [PT[0]])
                k.act(sc[:], PS[0][:, 0:64], AF.Exp, [PT[0]], [t_sc], scale=-1.0)
                k.ts("dve", sc[:], sc[:], 1.0, None, ALU.add, ALU.bypass, [t_sc], [t_sc])
                k.op("dve", lambda g: g.reciprocal(out=sc[:], in_=sc[:]), [t_sc], [t_sc])
                k.tt("dve", bs[:], sc[:], rb[:], ALU.add, [t_sc, t_rb], [t_bs])
                for gidx in range(8):
                    k.op("dve", lambda g: g.max(out=m8[:, gidx, :], in_=bs[:, gidx * 8:(gidx + 1) * 8]), [t_bs], [t_m8])
                k.tt("dve", grp_[:], m8[:, :, 0], m8[:, :, 1], ALU.add, [t_m8], [t_grp])
                k.op("dve", lambda g: g.max(out=g8[:], in_=grp_[:]), [t_grp], [t_g8])
                k.ts("dve", pen[:], grp_[:], g8[:, 3:4], 1.0e9, ALU.is_ge, ALU.mult, [t_grp, t_g8], [t_pen])
                k.ts("dve", pen[:], pen[:], -1.0e9, None, ALU.add, ALU.bypass, [t_pen], [t_pen])
                k.tt("dve", cand[:].rearrange("p (g m) -> p g m", m=8), bs[:].rearrange("p (g m) -> p g m", m=8),
                     pen[:].unsqueeze(2).broadcast_to([128, 8, 8]), ALU.add, [t_bs, t_pen], [t_cand])
                k.op("dve", lambda g: g.max(out=c8[:], in_=cand[:]), [t_cand], [t_c8])
                k.ts("dve", cand[:], cand[:], c8[:, 7:8], None, ALU.is_ge, ALU.bypass, [t_cand, t_c8], [t_cand])
                k.tt("dve", cand[:], cand[:], sc[:], ALU.mult, [t_cand, t_sc], [t_cand])
                k.op("dve", lambda g: g.reduce_sum(out=den2[:, 0:1], in_=cand[:], axis=mybir.AxisListType.X),
                     [t_cand], [t_den2])
                k.op("dve", lambda g: g.reciprocal(out=den2[:, 1:2], in_=den2[:, 0:1]), [t_den2], [t_den2])
                k.ts("dve", wts[:, it, 0:64], cand[:], den2[:, 1:2], 2.5, ALU.mult, ALU.mult, [t_cand, t_den2], [t_wts[it]])
                k.memset("dve", wts[:, it, 64:65], 1.0, [t_wts[it]])

            p3_s1(0)
            for j in range(4):
                if j + 1 < 4:
                    p3_s1(j + 1)
                p3_s2(j)
        dump("wts", wts[:], t_wts, [128, 16, NE])
        dump("h2T", h2T[:], t_h2T, [128, 8, NTOK], BF16)
        k.barrier()
    if stop_after == "P3":
        return finish(nc, k, out_toks, out_d)

    with ExitStack() as es:
        A = lambda name, shape, dt=F32: es.enter_context(nc.sbuf_tensor("s_" + name, list(shape), dt))
        oacc = A("moe_acc", [128, 16, D]); t_oacc = [T() for _ in range(16)]
        wgu = [A(f"wgu{i}", [128, 8, 512], BF16) for i in range(2)]; t_wgu = [T(), T()]
        wdn = [A(f"wdn{i}", [128, 2, D], BF16) for i in range(2)]; t_wdn = [T(), T()]
        sgm = [A(f"sgm{i}", [128, 256]) for i in range(2)]; t_sgm = [T(), T()]
        ab = [A(f"ab{i}", [128, 256], BF16) for i in range(2)]; t_ab = [T(), T()]
        aT = [A(f"aT{i}", [128, 256], BF16) for i in range(2)]; t_aT = [T(), T()]
        xt = [A(f"xt5{i}", [128, D]) for i in range(2)]; t_xt = [T(), T()]
        fo = [A(f"fo{i}", [128, D]) for i in range(2)]; t_fo = [T(), T()]
        steps = [(e, it) for e in range(NE) for it in range(16)]
        NSTEP = len(steps)
        loaded = set()

        def load_w(e):
            if e >= NE or e in loaded:
                return
            loaded.add(e)
            eb = e % 2
            k.dma("pool", wgu[eb][:], w_gu_d[e].rearrange("(k p) n -> p k n", p=128), writes=[t_wgu[eb]])
            k.dma("pool", wdn[eb][:], w_dn_d[e].rearrange("(k p) n -> p k n", p=128), writes=[t_wdn[eb]])

        def gu_act(si):
            e, it = steps[si]
            eb, i2 = e % 2, si % 2
            for kc in range(8):
                k.mm(PS[i2][:], h2T[:, kc, it * 128:(it + 1) * 128], wgu[eb][:, kc, :], kc == 0, kc == 7,
                     [t_h2T[it], t_wgu[eb]], [PT[i2]])
            k.act(sgm[i2][:], PS[i2][:, 0:256], AF.Silu, [PT[i2]], [t_sgm[i2]])
            k.stt(ab[i2][:], PS[i2][:, 256:512], wts[:, it, e:e + 1], sgm[i2][:], ALU.mult, ALU.mult,
                  [PT[i2], t_wts[it], t_sgm[i2]], [t_ab[i2]])

        def tr_act(si):
            i2 = si % 2
            ptr = 2 + i2
            for fc in range(2):
                k.tr(PSB[ptr][:, fc * 128:(fc + 1) * 128], ab[i2][:, fc * 128:(fc + 1) * 128], identb,
                     [t_ab[i2], t_cstb], [PT[ptr]])
            k.cp("act", aT[i2][:], PSB[ptr][:, 0:256], [PT[ptr]], [t_aT[i2]])

        def down_acc(si):
            e, it = steps[si]
            eb, i2 = e % 2, si % 2
            pd0 = 4 + 2 * i2
            for nh in range(2):
                for fc in range(2):
                    k.mm(PS[pd0 + nh][:], aT[i2][:, fc * 128:(fc + 1) * 128], wdn[eb][:, fc, nh * 512:(nh + 1) * 512],
                         fc == 0, fc == 1, [t_aT[i2], t_wdn[eb]], [PT[pd0 + nh]])
            for nh in range(2):
                hs = slice(nh * 512, (nh + 1) * 512)
                if e == 0:
                    k.cp("act", oacc[:, it, hs], PS[pd0 + nh][:], [PT[pd0 + nh]], [t_oacc[it]])
                else:
                    k.tt("dve", oacc[:, it, hs], oacc[:, it, hs], PS[pd0 + nh][:], ALU.add,
                         [t_oacc[it], PT[pd0 + nh]], [t_oacc[it]])

        load_w(0)
        load_w(1)
        gu_act(0)
        for si in range(NSTEP + 1):
            if si + 1 < NSTEP:
                gu_act(si + 1)
            if si < NSTEP:
                tr_act(si)
            if si >= 1:
                down_acc(si - 1)
                if steps[si - 1][1] == 15:
                    load_w(steps[si - 1][0] + 2)
        dump("moe0", oacc[:, 0, :], [t_oacc[0]], [128, D])
        for it in range(16):
            i2 = it % 2
            k.dma("sp", xt[i2][:], x1_d[it * 128:(it + 1) * 128, :], writes=[t_xt[i2]])
            k.act(junk[:], oacc[:, it, :], AF.Square, [t_oacc[it]], [t_junk, t_ss], accum_out=ss[:, 32 + it % 8:33 + it % 8])
            rstd_to(rs[:, 32 + it % 8:33 + it % 8], ss[:, 32 + it % 8:33 + it % 8], 1.0 / D, [t_ss], t_rs)
            k.stt(fo[i2][:], oacc[:, it, :], rs[:, 32 + it % 8:33 + it % 8], gbc[1][:], ALU.mult, ALU.mult,
                  [t_oacc[it], t_rs, t_gbc[1]], [t_fo[i2]])
            k.tt("dve", fo[i2][:], fo[i2][:], xt[i2][:], ALU.add, [t_fo[i2], t_xt[i2]], [t_fo[i2]])
            out_toks.append(k.dma("sp", out_d[it * 128:(it + 1) * 128, :], fo[i2][:], reads=[t_fo[i2]]))
    return finish(nc, k, out_toks, out_d)


def finish(nc, k, out_toks, out_d):
    for tok in k.all_tokens():
        if tok[3] == "dma":
            semname, sem, val, owner = tok
            if k.waited["sp"].get(semname, 0) < val:
                nc.sync.wait_ge(sem, val)
                k.waited["sp"][semname] = val
    return nc


def _consts():
    p = np.arange(128, dtype=np.float32)[:, None]
    f = np.arange(128, dtype=np.float32)[None, :]
    c = np.zeros((128, 11, 128), np.float32)
    c[:, 0] = (p == f)
    c[:, 1] = 1.0
    c[:, 2] = (p > f)
    c[:, 3] = (p >= f)
    c[:, 4] = (p < f)
    c[:, 5] = (p <= f)
    c[:, 6] = np.maximum(f - p, 0)
    c[:, 7] = np.maximum(p - f, 0)
    c[:, 8] = f + 1 + 0 * p
    c[:, 9] = 128 - f + 0 * p
    c[:, 10, 0] = p[:, 0]
    c[:, 10, 1] = 127 - p[:, 0]
    c[:, 10, 2] = 128.0
    cb = np.zeros((128, 2, 128), np.float32)
    cb[:, 0] = (p == f)
    cb[:, 1] = 1.0
    return c, cb


def _rope_tables(flip):
    t = np.arange(NLAT)
    tg = (NLAT - 1 - t) if flip else t
    rows = (tg // 64).astype(np.float32)
    cols = (tg % 64).astype(np.float32)
    inv = (np.float32(10000.0) ** (-np.arange(32, dtype=np.float32) / np.float32(32))).astype(np.float32)
    ang = np.concatenate([rows[:, None] * inv, cols[:, None] * inv], axis=-1).astype(np.float32)
    cos = np.cos(ang).astype(np.float32)
    sin = np.sin(ang).astype(np.float32)
    C2 = np.concatenate([cos, cos], axis=1).T
    S2 = np.concatenate([-sin, sin], axis=1).T
    tab = np.zeros((128, 4, NS), np.float32)
    tab[:, 0, :NLAT] = C2 * QSC
    tab[:, 1, :NLAT] = S2 * QSC
    tab[:, 0, NLAT:] = QSC
    tab[:, 2, :NLAT] = C2
    tab[:, 3, :NLAT] = S2
    return tab


def fm(v):
    return np.ascontiguousarray(v.reshape(-1, 128).T)


def prep_inputs(inp, c):
    b, hf = c // 2, c % 2
    flip = hf == 1
    f32 = np.float32
    x = np.asarray(inp["x"][b], f32)
    ctx = np.asarray(inp["ctx"][b], f32)
    if flip:
        x = x[::-1]
        ctx = ctx[::-1]
    d = {}
    d["xl"] = np.ascontiguousarray(x)
    d["ctxl"] = np.ascontiguousarray(ctx)
    cv = np.stack([fm(np.asarray(inp["c"][b], f32)), fm(np.asarray(inp["c_ctx"], f32))], axis=-1)
    d["cvec"] = np.ascontiguousarray(cv)
    d["w_mod"] = np.ascontiguousarray(np.asarray(inp["w_mod"][0], f32))
    d["bmod"] = fm(np.asarray(inp["b_mod"][0], f32))
    d["gains"] = np.ascontiguousarray(np.stack([fm(np.asarray(inp[n][0], f32)) for n in
                                                ("norm_mix_pre", "norm_mix_post", "norm_ffn_pre", "norm_ffn_post")], axis=1))
    cst, cstb = _consts()
    d["cst"] = cst
    d["cstb"] = cstb
    d["rope"] = _rope_tables(flip)
    w_in = np.asarray(inp["w_in"][0], f32)
    Q0 = 2064
    sw = (np.arange(128) + 64) % 128
    heads = []
    for h in range(4):
        hs = slice(h * 128, (h + 1) * 128)
        rk = w_in[:, 0:512][:, hs]
        rv = w_in[:, 512:1024][:, hs]
        gk = w_in[:, 1024:1536][:, hs]
        gv = w_in[:, 1536:2048][:, hs]
        rq = w_in[:, Q0:Q0 + 512][:, hs]
        rg = w_in[:, Q0 + 512:Q0 + 1024][:, hs]
        gq = w_in[:, Q0 + 1024:Q0 + 1536][:, hs]
        gz = w_in[:, Q0 + 1536:Q0 + 2048][:, hs]
        heads.append(np.concatenate([rk, rk[:, sw], rv, gk, gv, rq, rq[:, sw], rg, gq, gz], axis=1))
    d["w_in_h"] = np.ascontiguousarray(np.stack(heads, 0))
    gabc = w_in[:, 2048:2064].reshape(D, 2, 2, 4)
    dirs = [1, 0] if flip else [0, 1]
    d["w_gab"] = np.ascontiguousarray(gabc[:, :, dirs, :].reshape(D, 16))
    d["w_gates"] = np.ascontiguousarray(w_in[:, Q0 + 2048:Q0 + 4096])
    conv = np.asarray(inp["gdn_conv"][0], f32)
    if flip:
        conv = conv[::-1]
    cw = np.zeros((128, 4, 3, 3), f32)
    for h in range(4):
        for fam in range(3):
            cw[:, h, fam, :] = conv[:, fam * 512 + h * 128: fam * 512 + (h + 1) * 128].T
    d["convw"] = cw
    hpv = np.concatenate([np.asarray(inp["ret_log_decay"][0], f32)[dirs].reshape(-1),
                          np.asarray(inp["gdn_a_log"][0], f32)[dirs].reshape(-1),
                          np.asarray(inp["gdn_dt_bias"][0], f32)[dirs].reshape(-1)])
    d["hp"] = np.ascontiguousarray(np.broadcast_to(hpv[None, :], (128, 24)).astype(f32))
    d["nw"] = np.ascontiguousarray(np.stack([fm(np.asarray(inp["ret_gn_w"][0], f32)),
                                             fm(np.asarray(inp["gdn_norm_w"][0], f32))], axis=1))
    d["w_ro"] = np.ascontiguousarray(np.asarray(inp["w_ret_out"][0], f32))
    d["w_go"] = np.ascontiguousarray(np.asarray(inp["w_gdn_out"][0], f32))
    d["w_o"] = np.ascontiguousarray(np.asarray(inp["w_o"][0], f32))
    d["w_r"] = np.ascontiguousarray(np.asarray(inp["w_router"][0], f32))
    d["rb"] = np.ascontiguousarray(np.broadcast_to(np.asarray(inp["router_bias"][0], f32)[None, :], (128, 64)).astype(f32))
    return d


_SHARED = {}


def shared_inputs(inp):
    f32 = np.float32
    wg = np.asarray(inp["w_gate"][0], f32)
    wu = np.asarray(inp["w_up"][0], f32)
    gu = np.empty((NE, D, 512), f32)
    gu[:64, :, :256] = wg
    gu[:64, :, 256:] = wu
    gu[64, :, :256] = np.asarray(inp["w_sh_gate"][0], f32)
    gu[64, :, 256:] = np.asarray(inp["w_sh_up"][0], f32)
    dn = np.empty((NE, 256, D), f32)
    dn[:64] = np.asarray(inp["w_down"][0], f32)
    dn[64] = np.asarray(inp["w_sh_down"][0], f32)
    return {"w_gu": gu, "w_dn": dn}


def kernel(**inputs):
    nc = build()
    sh = shared_inputs(inputs)
    in_maps = []
    for c in range(8):
        d = prep_inputs(inputs, c)
        d.update(sh)
        in_maps.append(d)
    res = run_bass_kernel_spmd(nc, in_maps, core_ids=list(range(8)))
    out = np.empty((4, NLAT, D), np.float32)
    for c in range(8):
        o = np.asarray(res.results[c]["out"], np.float32)
        b, hf = c // 2, c % 2
        if hf == 0:
            out[b, :NTOK] = o
        else:
            out[b, NTOK:] = o[::-1]
    return out
```
